# Optimizing a Trainium2 kernel written in Bass

```python
import math
import jax
import jax.numpy as jnp
from jax import lax
import numpy as np


D_MODEL = 1024
BATCH = 4
SEQ = 4096
DEPTH = 1
DEC_BATCH = 32
DEC_SEQ = 1
PAST_LEN = 16384
PAGE_SIZE = 128

ATTN_WIDTH = D_MODEL // 2
CONV_CH = D_MODEL - ATTN_WIDTH
HEAD_DIM = 64
N_HEADS = ATTN_WIDTH // HEAD_DIM
IN_WIDTH = 3 * ATTN_WIDTH + 2 * CONV_CH
CONV_LEN = 31
MOBA_BLOCK = 256
MOBA_TOPK = 3
QUERY_BLOCK = 64
ROPE_THETA = 10000.0
N_EXPERT_GROUPS = 4
EXPERTS_PER_GROUP = 4
N_EXPERTS = N_EXPERT_GROUPS * EXPERTS_PER_GROUP
EXPERT_TOP_K = 2
D_EXPERT = D_MODEL // 2
RMS_EPS = 1e-6
LN_EPS = 1e-5
NEG_INF = -1e30

kernel_name = 'hymba_moba_conformer_hmoe_step'


def rms_norm(x, g):
    xf = x.astype(jnp.float32)
    y = xf * lax.rsqrt(jnp.mean(xf * xf, axis=-1, keepdims=True) + RMS_EPS)
    return (y * g.astype(jnp.float32)).astype(x.dtype)


def layer_norm(x, g, b):
    xf = x.astype(jnp.float32)
    mu = jnp.mean(xf, axis=-1, keepdims=True)
    var = jnp.mean(jnp.square(xf - mu), axis=-1, keepdims=True)
    y = (xf - mu) * lax.rsqrt(var + LN_EPS)
    return (y * g.astype(jnp.float32) + b.astype(jnp.float32)).astype(x.dtype)


def rope(x, pos):
    half = HEAD_DIM // 2
    inv_freq = jnp.exp(-math.log(ROPE_THETA) * jnp.arange(half, dtype=jnp.float32) / half)
    ang = pos.astype(jnp.float32)[:, None] * inv_freq[None, :]
    cos = jnp.cos(ang)[None, :, None, :]
    sin = jnp.sin(ang)[None, :, None, :]
    xf = x.astype(jnp.float32)
    x1, x2 = xf[..., :half], xf[..., half:]
    return jnp.concatenate([x1 * cos - x2 * sin, x2 * cos + x1 * sin], axis=-1).astype(x.dtype)


def in_projection(x, norm_g, w_in, b_in):
    bsz, slen, _ = x.shape
    z = rms_norm(x, norm_g) @ w_in + b_in
    a, c = ATTN_WIDTH, CONV_CH
    q, k, v, u_val, u_gate = jnp.split(z, [a, 2 * a, 3 * a, 3 * a + c], axis=-1)
    heads = lambda t: t.reshape(bsz, slen, N_HEADS, HEAD_DIM)
    u = u_val * jax.nn.sigmoid(u_gate)
    return heads(q), heads(k), heads(v), u


def conv_tail(u_hist, conv_w, conv_b, ln_g, ln_b):
    c = lax.conv_general_dilated(u_hist, conv_w[:, None, :], window_strides=(1,), padding='VALID',
                                 dimension_numbers=('NWC', 'WIO', 'NWC'),
                                 feature_group_count=u_hist.shape[-1]) + conv_b
    return jax.nn.silu(layer_norm(c, ln_g, ln_b))


def moba_attend(q, q_pos, kb, vb, k_mean):
    bsz, n_blocks = kb.shape[0], kb.shape[1]
    n_sel = min(MOBA_TOPK, n_blocks)
    own = q_pos // MOBA_BLOCK
    gate = jnp.einsum('bqhd,bnhd->bhqn', q.astype(jnp.float32), k_mean)
    fully_past = jnp.arange(n_blocks, dtype=jnp.int32)[None, :] < own[:, None]
    gate = jnp.where(fully_past, gate, NEG_INF)
    _, sel = lax.top_k(gate, n_sel)
    sel_ok = jnp.arange(n_sel, dtype=jnp.int32)[None, :] < own[:, None]
    own_idx = jnp.broadcast_to(own[None, None, :, None], sel.shape[:3] + (1,)).astype(sel.dtype)
    idx = jnp.concatenate([sel, own_idx], axis=-1)
    slot_ok = jnp.concatenate([sel_ok, jnp.ones_like(own[:, None], dtype=bool)], axis=-1)
    b_ix = jnp.arange(bsz)[:, None, None, None]
    h_ix = jnp.arange(N_HEADS)[None, :, None, None]
    kg = kb[b_ix, idx, :, h_ix]
    vg = vb[b_ix, idx, :, h_ix]
    kpos = idx[..., None] * MOBA_BLOCK + jnp.arange(MOBA_BLOCK, dtype=idx.dtype)
    mask = slot_ok[None, None, :, :, None] & (kpos <= q_pos[None, None, :, None, None])
    logits = jnp.einsum('bqhd,bhqsld->bhqsl', q, kg).astype(jnp.float32) * (HEAD_DIM ** -0.5)
    logits = jnp.where(mask, logits, NEG_INF)
    shp = logits.shape
    probs = jax.nn.softmax(logits.reshape(shp[:3] + (-1,)), axis=-1).reshape(shp)
    return jnp.einsum('bhqsl,bhqsld->bqhd', probs.astype(vg.dtype), vg)


def to_blocks(t, n_blocks):
    bsz, length = t.shape[0], t.shape[1]
    t = jnp.pad(t, ((0, 0), (0, n_blocks * MOBA_BLOCK - length), (0, 0), (0, 0)))
    return t.reshape(bsz, n_blocks, MOBA_BLOCK, N_HEADS, HEAD_DIM)


def moba_prompt(q, k, v, pos):
    bsz, slen = q.shape[0], q.shape[1]
    n_blocks = -(-slen // MOBA_BLOCK)
    kb, vb = to_blocks(k, n_blocks), to_blocks(v, n_blocks)
    k_mean = jnp.mean(kb.astype(jnp.float32), axis=2)
    n_q = slen // QUERY_BLOCK
    qc = q.reshape(bsz, n_q, QUERY_BLOCK, N_HEADS, HEAD_DIM).swapaxes(0, 1)
    pc = pos.reshape(n_q, QUERY_BLOCK)
    out = lax.map(lambda a: moba_attend(a[0], a[1], kb, vb, k_mean), (qc, pc))
    return out.swapaxes(0, 1).reshape(bsz, slen, ATTN_WIDTH)


def moba_sample(q, k, v, past_k, past_v, pos):
    bsz, sd = q.shape[0], q.shape[1]
    total = past_k.shape[1] + sd
    n_blocks = -(-total // MOBA_BLOCK)
    pad = jnp.zeros((bsz, n_blocks * MOBA_BLOCK - total, N_HEADS, HEAD_DIM), k.dtype)
    kb = jnp.concatenate([past_k, k, pad], axis=1).reshape(bsz, n_blocks, MOBA_BLOCK, N_HEADS, HEAD_DIM)
    vb = jnp.concatenate([past_v, v, pad.astype(v.dtype)], axis=1).reshape(bsz, n_blocks, MOBA_BLOCK, N_HEADS, HEAD_DIM)
    k_mean = jnp.mean(kb.astype(jnp.float32), axis=2)
    return moba_attend(q, pos, kb, vb, k_mean).reshape(bsz, sd, ATTN_WIDTH)


def hier_moe(h, w_group, b_group, w_router, b_router, w_gate, w_up, w_down):
    bsz, slen, d = h.shape
    t = h.reshape(-1, d)
    n_tok = t.shape[0]
    g_prob = jax.nn.softmax((t @ w_group).astype(jnp.float32) + b_group, axis=-1)
    g_sel = jnp.argmax(g_prob, axis=-1)
    g_w = jnp.take_along_axis(g_prob, g_sel[:, None], axis=-1)
    e_logits = ((t @ w_router).astype(jnp.float32) + b_router).reshape(n_tok, N_EXPERT_GROUPS, EXPERTS_PER_GROUP)
    e_in = jnp.take_along_axis(e_logits, g_sel[:, None, None], axis=1)[:, 0]
    top_p, top_i = lax.top_k(jax.nn.softmax(e_in, axis=-1), EXPERT_TOP_K)
    top_p = top_p / jnp.sum(top_p, axis=-1, keepdims=True)
    expert_id = g_sel[:, None] * EXPERTS_PER_GROUP + top_i
    combine = jnp.sum(jax.nn.one_hot(expert_id, N_EXPERTS, dtype=jnp.float32) * (g_w * top_p)[..., None], axis=1)
    hid = jax.nn.silu(jnp.einsum('td,edf->tef', t, w_gate)) * jnp.einsum('td,edf->tef', t, w_up)
    hid = hid * combine[:, :, None].astype(hid.dtype)
    return jnp.einsum('tef,efd->td', hid, w_down).reshape(bsz, slen, d)


def post_mixer(x, attn, conv, w_out, norm2_g, w_group, b_group, w_router, b_router, w_gate, w_up, w_down):
    x = x + jnp.concatenate([attn, conv], axis=-1) @ w_out
    return x + hier_moe(rms_norm(x, norm2_g), w_group, b_group, w_router, b_router, w_gate, w_up, w_down)


def setup_inputs(seed: int = 0) -> dict:
    key = jax.random.key(seed)
    ks = jax.random.split(key, 24)
    nrm = lambda k, shape, scale: jax.random.normal(k, shape, jnp.float32) * scale
    n_pages = PAST_LEN // PAGE_SIZE
    n_phys = (DEC_BATCH * n_pages * 5 + 3) // 4
    page_table = jax.random.permutation(ks[5], n_phys)[:DEC_BATCH * n_pages].reshape(DEC_BATCH, n_pages).astype(jnp.int32)
    return {
        'x_prompt': nrm(ks[0], (BATCH, SEQ, D_MODEL), 1.0),
        'x_sample': nrm(ks[1], (DEC_BATCH, DEC_SEQ, D_MODEL), 1.0),
        'cache_k': nrm(ks[2], (DEPTH, n_phys, PAGE_SIZE, N_HEADS, HEAD_DIM), 1.0),
        'cache_v': nrm(ks[3], (DEPTH, n_phys, PAGE_SIZE, N_HEADS, HEAD_DIM), 1.0),
        'state_conv': nrm(ks[4], (DEPTH, DEC_BATCH, CONV_LEN - 1, CONV_CH), 0.5),
        'page_table': page_table,
        'norm1_g': 1.0 + nrm(ks[6], (DEPTH, D_MODEL), 0.01),
        'w_in': nrm(ks[7], (DEPTH, D_MODEL, IN_WIDTH), D_MODEL ** -0.5),
        'b_in': nrm(ks[8], (DEPTH, IN_WIDTH), 0.01),
        'conv_w': nrm(ks[9], (DEPTH, CONV_LEN, CONV_CH), CONV_LEN ** -0.5),
        'conv_b': nrm(ks[10], (DEPTH, CONV_CH), 0.01),
        'conv_ln_g': 1.0 + nrm(ks[11], (DEPTH, CONV_CH), 0.01),
        'conv_ln_b': nrm(ks[12], (DEPTH, CONV_CH), 0.01),
        'w_out': nrm(ks[13], (DEPTH, D_MODEL, D_MODEL), D_MODEL ** -0.5),
        'norm2_g': 1.0 + nrm(ks[14], (DEPTH, D_MODEL), 0.01),
        'w_group': nrm(ks[15], (DEPTH, D_MODEL, N_EXPERT_GROUPS), D_MODEL ** -0.5),
        'b_group': nrm(ks[16], (DEPTH, N_EXPERT_GROUPS), 0.01),
        'w_router': nrm(ks[17], (DEPTH, D_MODEL, N_EXPERTS), D_MODEL ** -0.5),
        'b_router': nrm(ks[18], (DEPTH, N_EXPERTS), 0.01),
        'w_gate': nrm(ks[19], (DEPTH, N_EXPERTS, D_MODEL, D_EXPERT), D_MODEL ** -0.5),
        'w_up': nrm(ks[20], (DEPTH, N_EXPERTS, D_MODEL, D_EXPERT), D_MODEL ** -0.5),
        'w_down': nrm(ks[21], (DEPTH, N_EXPERTS, D_EXPERT, D_MODEL), D_EXPERT ** -0.5),
        'norm_f_g': 1.0 + nrm(ks[22], (D_MODEL,), 0.01),
    }


def reference(x_prompt, x_sample, cache_k, cache_v, state_conv, page_table, norm1_g, w_in, b_in,
              conv_w, conv_b, conv_ln_g, conv_ln_b, w_out, norm2_g, w_group, b_group, w_router,
              b_router, w_gate, w_up, w_down, norm_f_g):
    bsz, slen, _ = x_prompt.shape
    dbsz, dlen, _ = x_sample.shape
    past_len = page_table.shape[1] * cache_k.shape[2]
    pos_p = jnp.arange(slen, dtype=jnp.int32)
    pos_s = past_len + jnp.arange(dlen, dtype=jnp.int32)
    xp, xs = x_prompt, x_sample
    kp_all, vp_all, cp_all, ks_all, vs_all, cs_all = [], [], [], [], [], []
    for l in range(DEPTH):
        q, k, v, u = in_projection(xp, norm1_g[l], w_in[l], b_in[l])
        q, k = rope(q, pos_p), rope(k, pos_p)
        attn = moba_prompt(q, k, v, pos_p)
        u_hist = jnp.pad(u, ((0, 0), (CONV_LEN - 1, 0), (0, 0)))
        conv = conv_tail(u_hist, conv_w[l], conv_b[l], conv_ln_g[l], conv_ln_b[l])
        xp = post_mixer(xp, attn, conv, w_out[l], norm2_g[l], w_group[l], b_group[l], w_router[l],
                        b_router[l], w_gate[l], w_up[l], w_down[l])
        kp_all.append(k)
        vp_all.append(v)
        cp_all.append(u_hist[:, u_hist.shape[1] - (CONV_LEN - 1):])
        q, k, v, u = in_projection(xs, norm1_g[l], w_in[l], b_in[l])
        q, k = rope(q, pos_s), rope(k, pos_s)
        past_k = cache_k[l, page_table].reshape(dbsz, past_len, N_HEADS, HEAD_DIM)
        past_v = cache_v[l, page_table].reshape(dbsz, past_len, N_HEADS, HEAD_DIM)
        attn = moba_sample(q, k, v, past_k, past_v, pos_s)
        u_hist = jnp.concatenate([state_conv[l], u], axis=1)
        conv = conv_tail(u_hist, conv_w[l], conv_b[l], conv_ln_g[l], conv_ln_b[l])
        xs = post_mixer(xs, attn, conv, w_out[l], norm2_g[l], w_group[l], b_group[l], w_router[l],
                        b_router[l], w_gate[l], w_up[l], w_down[l])
        ks_all.append(k)
        vs_all.append(v)
        cs_all.append(u_hist[:, u_hist.shape[1] - (CONV_LEN - 1):])
    y_prompt = rms_norm(xp, norm_f_g)
    y_sample = rms_norm(xs, norm_f_g)
    return (y_prompt, y_sample, jnp.stack(kp_all), jnp.stack(vp_all), jnp.stack(cp_all),
            jnp.stack(ks_all), jnp.stack(vs_all), jnp.stack(cs_all))
```

```python
import math
import numpy as np
import ml_dtypes
from contextlib import ExitStack
import concourse.bass as bass
import concourse.mybir as mybir
from concourse.bass_utils import run_bass_kernel_spmd

F32 = mybir.dt.float32
BF16 = mybir.dt.bfloat16
I32 = mybir.dt.int32
U32 = mybir.dt.uint32
AF = mybir.ActivationFunctionType
ALU = mybir.AluOpType
AX = mybir.AxisListType

D = 1024
NT_OTH = 16
NT_OWN = 16
NT = 33
T_OWN = 2048
NEG = -1.0e30


class Buf:
    __slots__ = ("name", "last_w", "readers", "dsem", "dcount")

    def __init__(self, name):
        self.name = name
        self.last_w = None
        self.readers = []
        self.dsem = None
        self.dcount = 0


class Tl:
    def __init__(self, t, buf):
        self.t = t
        self.b = buf

    def __getitem__(self, k):
        return self.t[k]


class FW:
    ENGS = ("pe", "act", "dve", "pool", "sp")

    def __init__(self, nc, stack):
        self.nc = nc
        self.stack = stack
        self.E = {"pe": nc.tensor, "act": nc.scalar, "dve": nc.vector, "pool": nc.gpsimd, "sp": nc.sync}
        self.sem = {}
        self.seq = {e: 0 for e in self.ENGS}
        self.known = {e: {} for e in self.ENGS}
        for e in self.ENGS:
            self.sem[e] = stack.enter_context(nc.semaphore("s_" + e))
        self.semkey = {id(self.sem[e]): e for e in self.ENGS}
        self.dsems = []
        self.free_dsems = []
        self.n = 0

    def buf(self, name=None):
        self.n += 1
        return Buf(name or f"b{self.n}")

    def sb(self, scope, name, shape, dt):
        self.n += 1
        return Tl(scope.enter_context(self.nc.sbuf_tensor(f"sb{self.n}_{name}", shape, dt)), self.buf(name))

    def ps(self, scope, name, shape, dt):
        self.n += 1
        return Tl(scope.enter_context(self.nc.psum_tensor(f"ps{self.n}_{name}", shape, dt)), self.buf(name))

    def _waits(self, eng, reads, writes):
        need = {}
        semobj = {}

        def add(ev):
            if ev is None:
                return
            s, v = ev
            k = id(s)
            semobj[k] = s
            if need.get(k, 0) < v:
                need[k] = v

        for b in reads:
            add(b.last_w)
        for b in writes:
            add(b.last_w)
            for r in b.readers:
                add(r)
        known = self.known[eng]
        for k, v in need.items():
            if known.get(k, 0) >= v:
                continue
            if eng == "pe" and self.semkey.get(k) == "pe":
                continue
            known[k] = v
            self.E[eng].wait_ge(semobj[k], v)

    def _record(self, ev, reads, writes):
        for b in reads:
            b.readers.append(ev)
            if len(b.readers) > 64:
                last = {}
                for (s, v) in b.readers:
                    if last.get(id(s), (None, 0))[1] < v:
                        last[id(s)] = (s, v)
                b.readers = list(last.values())
        for b in writes:
            b.last_w = ev
            b.readers = []

    @staticmethod
    def _bufs(xs):
        return [x.b if isinstance(x, Tl) else x for x in xs]

    def op(self, eng, fn, reads=(), writes=()):
        reads = self._bufs(reads)
        writes = self._bufs(writes)
        self._waits(eng, reads, writes)
        self.seq[eng] += 1
        s = self.sem[eng]
        ev = (s, self.seq[eng])
        fn(self.E[eng]).then_inc(s, 1)
        self._record(ev, reads, writes)
        return ev

    def dma(self, q, fn, reads=(), writes=(), track=None):
        reads = self._bufs(reads)
        writes = self._bufs(writes)
        self._waits(q, reads, writes)
        b = track.b if isinstance(track, Tl) else track
        if b.dsem is None:
            self.n += 1
            b.dsem = self.stack.enter_context(self.nc.semaphore(f"d{self.n}_" + b.name))
            self.dsems.append(b)
        b.dcount += 16
        ev = (b.dsem, b.dcount)
        fn(self.E[q]).then_inc(b.dsem, 16)
        self._record(ev, reads, writes)
        return ev

    def barrier(self):
        for e in self.ENGS:
            known = self.known[e]
            for f in self.ENGS:
                if f == e or self.seq[f] == 0:
                    continue
                k = id(self.sem[f])
                if known.get(k, 0) < self.seq[f]:
                    known[k] = self.seq[f]
                    self.E[e].wait_ge(self.sem[f], self.seq[f])
            for b in self.dsems:
                k = id(b.dsem)
                if known.get(k, 0) < b.dcount:
                    known[k] = b.dcount
                    self.E[e].wait_ge(b.dsem, b.dcount)


def _dsize(dt):
    return 4 if dt in (F32, I32, U32) else 2


class Arena:
    def __init__(self, fw, ap, words):
        self.fw = fw
        self.ap = ap
        self.words = words
        self.free = [(0, words)]
        self.ghosts = []

    def alloc(self, name, shape, dt):
        n = 1
        for d in shape[1:]:
            n *= d
        nbytes = n * _dsize(dt)
        w = (nbytes + 63) // 64 * 16
        for i, (s, e) in enumerate(self.free):
            if e - s >= w:
                break
        else:
            raise RuntimeError(f"arena out of space for {name} ({w * 4} B); free={self.free}")
        self.free[i:i + 1] = [(s + w, e)] if e - s > w else []
        v = self.ap[:, s:s + w]
        if dt != F32:
            v = v.bitcast(dt)
        v = v[0:shape[0], 0:n]
        if len(shape) > 2:
            names = " ".join(f"d{j}" for j in range(len(shape) - 1))
            kw = {f"d{j}": shape[j + 1] for j in range(len(shape) - 2)}
            v = v.rearrange(f"p ({names}) -> p {names}", **kw)
        b = self.fw.buf(name)
        keep = []
        for (gs, ge, evs) in self.ghosts:
            if gs < s + w and ge > s:
                b.readers.extend(evs)
                if gs >= s and ge <= s + w:
                    continue
            keep.append((gs, ge, evs))
        self.ghosts = keep
        tl = Tl(v, b)
        tl.region = (s, s + w)
        return tl

    def release(self, *tls):
        for tl in tls:
            s, e = tl.region
            evs = list(tl.b.readers)
            if tl.b.last_w is not None:
                evs.append(tl.b.last_w)
            last = {}
            for (sm, v) in evs:
                if last.get(id(sm), (None, 0))[1] < v:
                    last[id(sm)] = (sm, v)
            self.ghosts.append((s, e, list(last.values())))
            self.free.append((s, e))
            self.free.sort()
            merged = []
            for (a, b_) in self.free:
                if merged and merged[-1][1] == a:
                    merged[-1] = (merged[-1][0], b_)
                else:
                    merged.append((a, b_))
            self.free = merged


def _rope_tables(pos):
    half = 32
    inv_freq = np.exp(-math.log(10000.0) * np.arange(half, dtype=np.float32) / half).astype(np.float32)
    ang = pos.astype(np.float32)[:, None] * inv_freq[None, :]
    cos = np.cos(ang).astype(np.float32)
    sin = np.sin(ang).astype(np.float32)
    C = np.concatenate([cos, cos], axis=1)
    S = np.concatenate([-sin, sin], axis=1)
    return np.concatenate([C, S], axis=1).astype(np.float32)


def bf(x):
    return np.asarray(x, dtype=np.float32).astype(ml_dtypes.bfloat16)


ARENA_WORDS = 51200
IN_NAMES = []


def build(stop_after=99, dbg=False):
    nc = bass.Bass("TRN2", target_bir_lowering=False)
    IN_NAMES.clear()

    def din(n, s, d=F32):
        IN_NAMES.append(n)
        return nc.dram_tensor(n, list(s), d, kind="ExternalInput").ap()
    dout = lambda n, s, d=F32: nc.dram_tensor(n, list(s), d, kind="ExternalOutput").ap()

    xall = din("xall", [NT * 128, D])
    rope = din("rope", [NT * 128, 128])
    w_in = din("w_in", [D, 2560])
    b_in = din("b_in", [1, 2560])
    g1 = din("g1", [1, D])
    g2 = din("g2", [1, D])
    gf = din("gf", [1, D])
    hflag = din("hflag", [128, 1])
    identb_d = din("identb", [128, 128], BF16)
    identf_d = din("identf", [128, 128])
    esel_d = din("esel", [128, 256], BF16)
    selmask_d = din("selmask", [128, 128])
    ownhot_d = din("ownhot", [128, 128])
    slotind_d = din("slotind", [16, 4096], BF16)
    causal4_d = din("causal4", [128, 2048], BF16)
    convw_d = din("convw", [128, 124])
    convb_d = din("convb", [128, 4])
    lng_d = din("lng", [1, 512])
    lnb_d = din("lnb", [1, 512])
    w_out = din("w_out", [D, D])
    wr_d = din("wr", [D, 20])
    br_d = din("br", [1, 20])
    if stop_after >= 6:
        w_gate = din("w_gate", [16, D, 512])
        w_up = din("w_up", [16, D, 512])
        w_down = din("w_down", [16, 512, D])
    stc_d = din("stc", [32 * 30, 512])
    attn_s_d = din("attn_s", [128, 512])

    o_y = dout("o_y", [T_OWN, D])
    o_ys = dout("o_ys", [128, D])
    o_k = dout("o_k", [T_OWN, 512])
    o_v = dout("o_v", [T_OWN, 512])
    o_convp = dout("o_convp", [30, 512])
    o_ks = dout("o_ks", [128, 512])
    o_vs = dout("o_vs", [128, 512])
    o_convs = dout("o_convs", [32, 30, 512])
    dbgout = {}

    with ExitStack() as st:
        fw = FW(nc, st)
        op, dma = fw.op, fw.dma
        arena_t = st.enter_context(nc.sbuf_tensor("arena", [128, ARENA_WORDS], F32))
        AR = Arena(fw, arena_t[:, :], ARENA_WORDS)
        A = AR.alloc
        banks = [Tl(st.enter_context(nc.psum_tensor(f"bank{i}", [128, 512], F32))[:, :], fw.buf(f"bank{i}")) for i in range(8)]
        bbf16 = lambda bk: bk.t.bitcast(BF16)
        out_b = fw.buf("outs")

        def dbg_dump(name, tl, shape2d, dt, src=None):
            if not dbg:
                return
            d = dout("dbg_" + name, shape2d, dt)
            dma("sp", lambda e: e.dma_start(out=d, in_=tl), reads=[src], writes=[out_b], track=out_b)

        identb = A("identb", [128, 128], BF16)
        identf = A("identf", [128, 128], F32)
        hfl = A("hfl", [128, 1], F32)
        us_f = A("us_f", [128, 512], F32)
        ks_f = A("ks_f", [128, 512], F32)
        vs_f = A("vs_f", [128, 512], F32)
        dma("sp", lambda e: e.dma_start(out=identb[:], in_=identb_d), writes=[identb], track=identb)
        dma("sp", lambda e: e.dma_start(out=identf[:], in_=identf_d), writes=[identf], track=identf)
        dma("sp", lambda e: e.dma_start(out=hfl[:], in_=hflag), writes=[hfl], track=hfl)

        q_rot = A("q_rot", [128, 17, 512], BF16)
        k_rot = A("k_rot", [128, 32, 512], BF16)
        vaug = A("vaug", [128, 32, 8, 65], BF16)
        uT = A("uT", [128, 4, 30 + T_OWN], BF16)
        op("pool", lambda e: e.memset(vaug[:, :, :, 64:65], 1.0), writes=[vaug])

        def rstd_from_ss(SS, eps, scale):
            op("dve", lambda e: e.tensor_scalar(out=SS[:], in0=SS[:], scalar1=scale, scalar2=eps, op0=ALU.mult, op1=ALU.add), reads=[SS], writes=[SS])
            op("act", lambda e: e.activation(out=SS[:], in_=SS[:], func=AF.Sqrt), reads=[SS], writes=[SS])
            op("dve", lambda e: e.reciprocal(out=SS[:], in_=SS[:]), reads=[SS], writes=[SS])

        wbf = [A(f"wbf{k}", [128, 2560], BF16) for k in range(8)]
        stage = [A(f"stg{i}", [128, 1280], F32) for i in range(2)]
        g1b = A("g1b", [128, D], F32)
        bbf = A("bbf", [1, 2560], BF16)
        ones1 = A("ones1", [1, 128], BF16)
        xt = [A(f"xt{i}", [128, D], F32) for i in range(2)]
        rt = [A(f"rt{i}", [128, 128], F32) for i in range(2)]
        sq = A("sq", [128, D], BF16)
        ss = [A(f"ss{i}", [128, 1], F32) for i in range(2)]
        xn = [A(f"xn{i}", [128, D], BF16) for i in range(2)]
        xnT = [A(f"xnT{i}", [128, 8, 128], BF16) for i in range(2)]
        t1 = A("t1", [128, 512], F32)
        t2 = A("t2", [128, 512], F32)
        kf = A("kf", [128, 512], F32)
        vf = A("vf", [128, 512], F32)
        sig = A("sig", [128, 512], F32)
        uf = A("uf", [128, 512], F32)
        ub = A("ub", [128, 512], BF16)
        p1_tiles = wbf + stage + [g1b, bbf, ones1] + xt + rt + [sq] + ss + xn + xnT + [t1, t2, kf, vf, sig, uf, ub]
        pT = banks[0]
        pTv = bbf16(pT).rearrange("p (c n) -> p c n", c=8)
        pg = banks[1:7]

        dma("sp", lambda e: e.dma_start(out=g1b[:], in_=g1.partition_broadcast(128)), writes=[g1b], track=g1b)
        for hh in range(2):
            dma("sp", lambda e, hh=hh: e.dma_start(out=stage[1][0:1, :], in_=b_in[:, hh * 1280:(hh + 1) * 1280]), writes=[stage[1]], track=stage[1])
            op("pool", lambda e, hh=hh: e.tensor_copy(out=bbf[:, hh * 1280:(hh + 1) * 1280], in_=stage[1][0:1, :]), reads=[stage[1]], writes=[bbf])
        op("pool", lambda e: e.memset(ones1[:], 1.0), writes=[ones1])
        for kc in range(8):
            for hh in range(2):
                sg = stage[hh]
                dma("sp", lambda e, sg=sg, kc=kc, hh=hh: e.dma_start(out=sg[:], in_=w_in[kc * 128:(kc + 1) * 128, hh * 1280:(hh + 1) * 1280]), writes=[sg], track=sg)
                if hh == 0:
                    op("pool", lambda e, sg=sg, kc=kc, hh=hh: e.tensor_copy(out=wbf[kc][:, hh * 1280:(hh + 1) * 1280], in_=sg[:]), reads=[sg], writes=[wbf[kc]])
                else:
                    op("act", lambda e, sg=sg, kc=kc, hh=hh: e.activation(out=wbf[kc][:, hh * 1280:(hh + 1) * 1280], in_=sg[:], func=AF.Copy), reads=[sg], writes=[wbf[kc]])

        pgi = [0]

        def nextpg():
            p = pg[pgi[0] % 6]
            pgi[0] += 1
            return p

        def proj(p, xT, c0):
            for kc in range(8):
                op("pe", lambda e, kc=kc: e.matmul(p[:, :], lhsT=xT[:, kc, :], rhs=wbf[kc][:, c0:c0 + 512], start=(kc == 0), stop=False),
                   reads=[xT, wbf[kc]], writes=[p])
            op("pe", lambda e: e.matmul(p[:, :], lhsT=ones1[:], rhs=bbf[:, c0:c0 + 512], start=False, stop=True),
               reads=[ones1, bbf], writes=[p])

        def rope_evac(p, r, out_ap, out_tl):
            pv = p[:, :].rearrange("p (h d) -> p h d", h=8)
            tav = t1[:].rearrange("p (h d) -> p h d", h=8)
            tbv = t2[:].rearrange("p (h d) -> p h d", h=8)
            Cb = r[:, 0:64].unsqueeze(1).broadcast_to([128, 8, 64])
            S1 = r[:, 64:96].unsqueeze(1).broadcast_to([128, 8, 32])
            S2 = r[:, 96:128].unsqueeze(1).broadcast_to([128, 8, 32])
            op("dve", lambda e: e.tensor_tensor(out=tav, in0=pv, in1=Cb, op=ALU.mult), reads=[p, r], writes=[t1])
            op("dve", lambda e: e.tensor_tensor(out=tbv[:, :, 0:32], in0=pv[:, :, 32:64], in1=S1, op=ALU.mult), reads=[p, r], writes=[t2])
            op("dve", lambda e: e.tensor_tensor(out=tbv[:, :, 32:64], in0=pv[:, :, 0:32], in1=S2, op=ALU.mult), reads=[p, r], writes=[t2])
            op("pool", lambda e: e.tensor_tensor(out=out_ap, in0=t1[:], in1=t2[:], op=ALU.add), reads=[t1, t2], writes=[out_tl])

        for t in range(NT):
            i = t % 2
            own = t >= NT_OTH
            smp = t == NT - 1
            need_u = own or t == NT_OTH - 1
            X, R, XN, XT, SS = xt[i], rt[i], xn[i], xnT[i], ss[i]
            dma("sp", lambda e, X=X, t=t: e.dma_start(out=X[:], in_=xall[t * 128:(t + 1) * 128, :]), writes=[X], track=X)
            dma("sp", lambda e, R=R, t=t: e.dma_start(out=R[:], in_=rope[t * 128:(t + 1) * 128, :]), writes=[R], track=R)
            op("act", lambda e, X=X, SS=SS: e.activation(out=sq[:], in_=X[:], func=AF.Square, accum_out=SS[:]), reads=[X], writes=[sq, SS])
            rstd_from_ss(SS, 1e-6, 1.0 / D)
            op("dve", lambda e, X=X, SS=SS, XN=XN: e.scalar_tensor_tensor(out=XN[:], in0=X[:], scalar=SS[:], in1=g1b[:], op0=ALU.mult, op1=ALU.mult),
               reads=[X, SS, g1b], writes=[XN])
            for c in range(8):
                op("pe", lambda e, c=c, XN=XN: e.transpose(out=pTv[:, c, :], in_=XN[:, c * 128:(c + 1) * 128], identity=identb[:]),
                   reads=[XN, identb], writes=[pT])
            op("act", lambda e, XT=XT: e.activation(out=XT[:], in_=pTv, func=AF.Copy), reads=[pT], writes=[XT])
            if own:
                p = nextpg()
                proj(p, XT, 0)
                rope_evac(p, R, q_rot[:, t - NT_OTH, :], q_rot)
            p = nextpg()
            proj(p, XT, 512)
            KF = ks_f if smp else kf
            rope_evac(p, R, KF[:], KF)
            if not smp:
                op("act", lambda e, KF=KF, t=t: e.activation(out=k_rot[:, t, :], in_=KF[:], func=AF.Copy), reads=[KF], writes=[k_rot])
            if own and not smp:
                r0 = (t - NT_OTH) * 128
                dma("pool", lambda e, KF=KF, r0=r0: e.dma_start(out=o_k[r0:r0 + 128, :], in_=KF[:]), reads=[KF], writes=[out_b], track=out_b)
            if smp:
                dma("pool", lambda e: e.dma_start(out=o_ks, in_=ks_f[:]), reads=[ks_f], writes=[out_b], track=out_b)
            p = nextpg()
            proj(p, XT, 1024)
            VF = vs_f if smp else vf
            op("act", lambda e, VF=VF, p=p: e.activation(out=VF[:], in_=p[:, :], func=AF.Copy), reads=[p], writes=[VF])
            if not smp:
                op("pool", lambda e, VF=VF, t=t: e.tensor_copy(out=vaug[:, t, :, 0:64], in_=VF[:].rearrange("p (h d) -> p h d", h=8)), reads=[VF], writes=[vaug])
            if own and not smp:
                r0 = (t - NT_OTH) * 128
                dma("pool", lambda e, VF=VF, r0=r0: e.dma_start(out=o_v[r0:r0 + 128, :], in_=VF[:]), reads=[VF], writes=[out_b], track=out_b)
            if smp:
                dma("pool", lambda e: e.dma_start(out=o_vs, in_=vs_f[:]), reads=[vs_f], writes=[out_b], track=out_b)
            if need_u:
                pa = nextpg()
                proj(pa, XT, 1536)
                pb = nextpg()
                proj(pb, XT, 2048)
                UF = us_f if smp else uf
                op("act", lambda e, pb=pb: e.activation(out=sig[:], in_=pb[:, :], func=AF.Sigmoid), reads=[pb], writes=[sig])
                op("dve", lambda e, pa=pa, UF=UF: e.tensor_tensor(out=UF[:], in0=pa[:, :], in1=sig[:], op=ALU.mult), reads=[pa, sig], writes=[UF])
                if not smp:
                    op("pool", lambda e, UF=UF: e.tensor_copy(out=ub[:], in_=UF[:]), reads=[UF], writes=[ub])
                    for c in range(4):
                        op("pe", lambda e, c=c: e.transpose(out=pTv[:, c, :], in_=ub[:, c * 128:(c + 1) * 128], identity=identb[:]),
                           reads=[ub, identb], writes=[pT])
                    if t == NT_OTH - 1:
                        op("dve", lambda e: e.tensor_scalar(out=uT[:, :, 0:30], in0=pTv[:, 0:4, 98:128], scalar1=hfl[:], scalar2=None, op0=ALU.mult),
                           reads=[pT, hfl], writes=[uT])
                    else:
                        c0 = 30 + (t - NT_OTH) * 128
                        op("act", lambda e, c0=c0: e.activation(out=uT[:, :, c0:c0 + 128], in_=pTv[:, 0:4, :], func=AF.Copy), reads=[pT], writes=[uT])
                    if t == NT - 2:
                        dma("pool", lambda e, UF=UF: e.dma_start(out=o_convp, in_=UF[98:128, :]), reads=[UF], writes=[out_b], track=out_b)
        AR.release(*p1_tiles)
        if stop_after <= 1:
            fw.barrier()
            return nc

        bias_all = A("bias_all", [128, 16, 8, 16], BF16)
        esel = A("esel", [128, 16, 16], BF16)
        selmask = A("selmask", [128, 8, 16], F32)
        ownhot = A("ownhot", [128, 8, 16], F32)
        kmean = A("kmean", [16, 512], BF16)
        KM = A("KM", [128, 4, 32], BF16)
        qTc = [A(f"qTc{i}", [128, 4, 128], BF16) for i in range(2)]
        gate = A("gate", [128, 8, 16], F32)
        m8 = A("m8", [128, 8, 8], F32)
        thr = A("thr", [128, 8], F32)
        sel = A("sel", [128, 8, 16], F32)
        p2_tiles = [esel, selmask, ownhot, kmean, KM] + qTc + [gate, m8, thr, sel]
        dma("sp", lambda e: e.dma_start(out=esel[:].rearrange("p a b -> p (a b)"), in_=esel_d), writes=[esel], track=esel)
        dma("sp", lambda e: e.dma_start(out=selmask[:].rearrange("p a b -> p (a b)"), in_=selmask_d), writes=[selmask], track=selmask)
        dma("sp", lambda e: e.dma_start(out=ownhot[:].rearrange("p a b -> p (a b)"), in_=ownhot_d), writes=[ownhot], track=ownhot)
        ksum = banks[1]
        for t in range(32):
            op("pe", lambda e, t=t: e.matmul(ksum[0:16, :], lhsT=esel[:, t // 2, :], rhs=k_rot[:, t, :], start=(t == 0), stop=(t == 31)),
               reads=[esel, k_rot], writes=[ksum])
        op("dve", lambda e: e.tensor_scalar(out=kmean[:], in0=ksum[0:16, :], scalar1=1.0 / 256, scalar2=None, op0=ALU.mult), reads=[ksum], writes=[kmean])
        op("pool", lambda e: e.memset(KM[:], 0.0), writes=[KM])
        kmT = banks[2]
        kmTv = bbf16(kmT)
        for c in range(4):
            op("pe", lambda e, c=c: e.transpose(out=kmTv[:, c * 16:(c + 1) * 16], in_=kmean[:, c * 128:(c + 1) * 128], identity=identb[0:16, 0:16]),
               reads=[kmean, identb], writes=[kmT])
        kmTv3 = kmTv[:, 0:64].rearrange("p (c s) -> p c s", c=4)
        op("dve", lambda e: e.tensor_copy(out=KM[0:64, :, 0:16], in_=kmTv3[0:64]), reads=[kmT], writes=[KM])
        op("dve", lambda e: e.tensor_copy(out=KM[64:128, :, 16:32], in_=kmTv3[64:128]), reads=[kmT], writes=[KM])
        for qi in range(16):
            jb = qi // 2
            QT = qTc[qi % 2]
            ptq = banks[3 + (qi % 2)]
            ptqv = bbf16(ptq)[:, 0:512].rearrange("p (c n) -> p c n", c=4)
            for c in range(4):
                op("pe", lambda e, c=c, qi=qi, ptqv=ptqv: e.transpose(out=ptqv[:, c, :], in_=q_rot[:, qi, c * 128:(c + 1) * 128], identity=identb[:]),
                   reads=[q_rot, identb], writes=[ptq])
            op("act", lambda e, QT=QT, ptqv=ptqv: e.activation(out=QT[:], in_=ptqv, func=AF.Copy), reads=[ptq], writes=[QT])
            pgt = banks[5 + (qi % 2)]
            for c in range(4):
                op("pe", lambda e, c=c, QT=QT, pgt=pgt: e.matmul(pgt[:, c * 32:(c + 1) * 32], lhsT=QT[:, c, :], rhs=KM[:, c, :], start=True, stop=True),
                   reads=[QT, KM], writes=[pgt])
            op("dve", lambda e, pgt=pgt, jb=jb: e.tensor_tensor(out=gate[:], in0=pgt[:, 0:128].rearrange("p (h s) -> p h s", h=8),
                                                               in1=selmask[:, jb, :].unsqueeze(1).broadcast_to([128, 8, 16]), op=ALU.add),
               reads=[pgt, selmask], writes=[gate])
            for h in range(8):
                op("dve", lambda e, h=h: e.max(out=m8[:, h, :], in_=gate[:, h, :]), reads=[gate], writes=[m8])
            op("dve", lambda e: e.tensor_scalar(out=thr[:], in0=m8[:, :, 2], scalar1=-1.0e29, scalar2=None, op0=ALU.max), reads=[m8], writes=[thr])
            op("dve", lambda e: e.tensor_tensor(out=sel[:], in0=gate[:], in1=thr[:].unsqueeze(2).broadcast_to([128, 8, 16]), op=ALU.is_ge),
               reads=[gate, thr], writes=[sel])
            op("pool", lambda e, jb=jb: e.tensor_tensor(out=sel[:], in0=sel[:], in1=ownhot[:, jb, :].unsqueeze(1).broadcast_to([128, 8, 16]), op=ALU.add),
               reads=[sel, ownhot], writes=[sel])
            op("pool", lambda e, qi=qi: e.tensor_scalar(out=bias_all[:, qi, :, :], in0=sel[:], scalar1=1.0e30, scalar2=-1.0e30, op0=ALU.mult, op1=ALU.add),
               reads=[sel], writes=[bias_all])
        AR.release(*p2_tiles)
        if stop_after <= 2:
            if dbg:
                dbg_dump("bias", bias_all[:].rearrange("p a b c -> p (a b c)"), [128, 2048], BF16, bias_all)
            fw.barrier()
            return nc

        mix = A("mix", [128, 17, 1024], BF16)
        causal4 = A("causal4", [128, 4, 512], BF16)
        kTh = [A(f"kTh{i}", [80, 4096], BF16) for i in range(2)]
        qTh = [A(f"qTh{i}", [80, 2048], BF16) for i in range(2)]
        PT = [A(f"PT{i}", [128, 512], BF16) for i in range(3)]
        rec = A("rec", [128, 4], F32)
        p3_tiles = [causal4] + kTh + qTh + PT + [rec]
        dma("sp", lambda e: e.dma_start(out=causal4[:].rearrange("p a b -> p (a b)"), in_=causal4_d), writes=[causal4], track=causal4)
        for i in range(2):
            dma("sp", lambda e, i=i: e.dma_start(out=kTh[i][64:80, :], in_=slotind_d), writes=[kTh[i]], track=kTh[i])
        atmp = A("atmp", [128, 512], F32)
        dma("sp", lambda e: e.dma_start(out=atmp[:], in_=attn_s_d), writes=[atmp], track=atmp)
        op("pool", lambda e: e.tensor_copy(out=mix[:, 16, 0:512], in_=atmp[:]), reads=[atmp], writes=[mix])
        AR.release(atmp)
        ptr = banks[0]
        ptrv = bbf16(ptr).rearrange("p (c n) -> p c n", c=8)
        pbias = banks[1]
        psS = banks[2:4]
        psO = banks[4:8]
        pti = 0
        for h in range(8):
            KT, QT = kTh[h % 2], qTh[h % 2]
            for tb in range(4):
                for j in range(8):
                    t = tb * 8 + j
                    op("pe", lambda e, j=j, t=t, h=h: e.transpose(out=ptrv[0:64, j, :], in_=k_rot[:, t, h * 64:(h + 1) * 64], identity=identb[:]),
                       reads=[k_rot, identb], writes=[ptr])
                op("dve", lambda e, KT=KT, tb=tb: e.tensor_copy(out=KT[0:64, tb * 1024:(tb + 1) * 1024], in_=bbf16(ptr)[0:64, :]), reads=[ptr], writes=[KT])
            for tb in range(2):
                for j in range(8):
                    t = tb * 8 + j
                    op("pe", lambda e, j=j, t=t, h=h: e.transpose(out=ptrv[0:64, j, :], in_=q_rot[:, t, h * 64:(h + 1) * 64], identity=identb[:]),
                       reads=[q_rot, identb], writes=[ptr])
                op("dve", lambda e, QT=QT, tb=tb: e.tensor_copy(out=QT[0:64, tb * 1024:(tb + 1) * 1024], in_=bbf16(ptr)[0:64, :]), reads=[ptr], writes=[QT])
            for tb in range(4):
                for j in range(4):
                    qi = tb * 4 + j
                    op("pe", lambda e, j=j, qi=qi, h=h: e.matmul(pbias[64:80, j * 128:(j + 1) * 128], lhsT=bias_all[:, qi, h, :], rhs=identb[:], start=True, stop=True),
                       reads=[bias_all, identb], writes=[pbias])
                op("dve", lambda e, QT=QT, tb=tb: e.tensor_copy(out=QT[64:80, tb * 512:(tb + 1) * 512], in_=pbias[64:80, :]), reads=[pbias], writes=[QT])
            for g in range(4):
                nkt = 20 + 4 * g
                for kt in range(nkt):
                    S = psS[kt % 2]
                    P = PT[pti % 3]
                    pti += 1
                    op("pe", lambda e, S=S, kt=kt, g=g, KT=KT, QT=QT: e.matmul(S[:, :], lhsT=KT[:, kt * 128:(kt + 1) * 128], rhs=QT[:, g * 512:(g + 1) * 512], start=True, stop=True),
                       reads=[KT, QT], writes=[S])
                    op("act", lambda e, S=S, P=P: e.activation(out=P[:], in_=S[:, :], func=AF.Exp, scale=0.125), reads=[S], writes=[P])
                    jd = kt - (16 + 4 * g)
                    if jd >= 0:
                        op("pool", lambda e, P=P, jd=jd: e.tensor_tensor(out=P[:], in0=P[:], in1=causal4[:, jd, :], op=ALU.mult), reads=[P, causal4], writes=[P])
                    for qs in range(4):
                        if jd > qs:
                            continue
                        last = (kt == nkt - 1) or (kt == 16 + 4 * g + qs)
                        O = psO[qs]
                        op("pe", lambda e, O=O, P=P, qs=qs, kt=kt, h=h, last=last: e.matmul(O[:, 0:65], lhsT=P[:, qs * 128:(qs + 1) * 128], rhs=vaug[:, kt, h, :], start=(kt == 0), stop=last),
                           reads=[P, vaug], writes=[O])
                for qs in range(4):
                    O = psO[qs]
                    op("dve", lambda e, O=O, qs=qs: e.reciprocal(out=rec[:, qs:qs + 1], in_=O[:, 64:65]), reads=[O], writes=[rec])
                    op("dve", lambda e, O=O, qs=qs, g=g, h=h: e.tensor_scalar(out=mix[:, 4 * g + qs, h * 64:(h + 1) * 64], in0=O[:, 0:64], scalar1=rec[:, qs:qs + 1], scalar2=None, op0=ALU.mult),
                       reads=[O, rec], writes=[mix])
        AR.release(*p3_tiles)
        AR.release(q_rot, k_rot, vaug, bias_all)
        if stop_after <= 3:
            if dbg:
                dbg_dump("mix", mix[:].rearrange("p a b -> p (a b)"), [128, 17 * 1024], BF16, mix)
            fw.barrier()
            return nc

        cw = A("cw", [128, 4, 31], F32)
        cb = A("cb", [128, 4], F32)
        lngb = A("lngb", [128, 512], F32)
        lnbb = A("lnbb", [128, 512], F32)
        dg = A("dg", [128, 4, 31, 128], BF16)
        cT = [A(f"cT{i}", [128, 4, 512], F32) for i in range(2)]
        st6 = A("st6", [128, 6], F32)
        mv = A("mv", [128, 2], F32)
        yn = [A(f"yn{i}", [128, 512], F32) for i in range(2)]
        hsT = A("hsT", [128, 4, 32, 31], F32)
        stc = [A(f"stc{i}", [128, 512], F32) for i in range(2)]
        prod = A("prod", [128, 4, 32, 31], F32)
        p4_tiles = [cw, cb, lngb, lnbb, dg] + cT + [st6, mv] + yn + [hsT, prod] + stc
        dma("sp", lambda e: e.dma_start(out=cw[:].rearrange("p a b -> p (a b)"), in_=convw_d), writes=[cw], track=cw)
        dma("sp", lambda e: e.dma_start(out=cb[:], in_=convb_d), writes=[cb], track=cb)
        dma("sp", lambda e: e.dma_start(out=lngb[:], in_=lng_d.partition_broadcast(128)), writes=[lngb], track=lngb)
        dma("sp", lambda e: e.dma_start(out=lnbb[:], in_=lnb_d.partition_broadcast(128)), writes=[lnbb], track=lnbb)
        for c in range(4):
            for tau in range(31):
                op("pool", lambda e, c=c, tau=tau: e.tensor_scalar(out=dg[:, c, tau, :], in0=identf[:], scalar1=cw[:, c, tau:tau + 1], scalar2=None, op0=ALU.mult),
                   reads=[identf, cw], writes=[dg])

        def ln_silu_tile(CT, col0, ti, bank):
            for c in range(4):
                op("pe", lambda e, c=c: e.transpose(out=bank[:, c * 128:(c + 1) * 128], in_=CT[:, c, col0:col0 + 128], identity=identf[:]),
                   reads=[CT, identf], writes=[bank])
            op("dve", lambda e: e.bn_stats(out=st6[:], in_=bank[:, :]), reads=[bank], writes=[st6])
            op("dve", lambda e: e.bn_aggr(out=mv[:], in_=st6[:]), reads=[st6], writes=[mv])
            op("dve", lambda e: e.tensor_scalar(out=mv[:, 1:2], in0=mv[:, 1:2], scalar1=1e-5, scalar2=None, op0=ALU.add), reads=[mv], writes=[mv])
            op("act", lambda e: e.activation(out=mv[:, 1:2], in_=mv[:, 1:2], func=AF.Sqrt), reads=[mv], writes=[mv])
            op("dve", lambda e: e.reciprocal(out=mv[:, 1:2], in_=mv[:, 1:2]), reads=[mv], writes=[mv])
            Y = yn[ti % 2]
            op("dve", lambda e: e.tensor_scalar(out=Y[:], in0=bank[:, :], scalar1=mv[:, 0:1], scalar2=mv[:, 1:2], op0=ALU.subtract, op1=ALU.mult),
               reads=[bank, mv], writes=[Y])
            op("pool", lambda e: e.tensor_tensor(out=Y[:], in0=Y[:], in1=lngb[:], op=ALU.mult), reads=[Y, lngb], writes=[Y])
            op("pool", lambda e: e.tensor_tensor(out=Y[:], in0=Y[:], in1=lnbb[:], op=ALU.add), reads=[Y, lnbb], writes=[Y])
            op("act", lambda e: e.activation(out=mix[:, ti, 512:1024], in_=Y[:], func=AF.Silu), reads=[Y], writes=[mix])

        for tg in range(4):
            CT = cT[tg % 2]
            for c in range(4):
                pc = banks[c % 2]
                for tau in range(31):
                    op("pe", lambda e, c=c, tau=tau, tg=tg, pc=pc: e.matmul(pc[:, :], lhsT=dg[:, c, tau, :], rhs=uT[:, c, tg * 512 + tau: tg * 512 + tau + 512], start=(tau == 0), stop=(tau == 30)),
                       reads=[dg, uT], writes=[pc])
                op("act", lambda e, c=c, pc=pc, CT=CT: e.activation(out=CT[:, c, :], in_=pc[:, :], func=AF.Identity, bias=cb[:, c:c + 1]), reads=[pc, cb], writes=[CT])
            for tt in range(4):
                ln_silu_tile(CT, tt * 128, tg * 4 + tt, banks[2 + (tt % 2)])
        for r in range(8):
            nrow = 128 if r < 7 else 64
            S_ = stc[r % 2]
            dma("sp", lambda e, S_=S_, r=r, nrow=nrow: e.dma_start(out=S_[0:nrow, :], in_=stc_d[r * 128:r * 128 + nrow, :]), writes=[S_], track=S_)
            bk = banks[4 + (r % 2)]
            for c in range(4):
                op("pe", lambda e, c=c, S_=S_, nrow=nrow, bk=bk: e.transpose(out=bk[:, c * 128:c * 128 + nrow], in_=S_[0:nrow, c * 128:(c + 1) * 128], identity=identf[0:nrow, 0:nrow]),
                   reads=[S_, identf], writes=[bk])
            bkv = bk[:, :].rearrange("p (c n) -> p c n", c=4)
            f0 = r * 128
            f1 = f0 + nrow
            b0 = f0 // 30
            b1 = (f1 - 1) // 30
            for b in range(b0, b1 + 1):
                lo = max(f0, b * 30)
                hi = min(f1, b * 30 + 30)
                op("dve", lambda e, b=b, lo=lo, hi=hi, bkv=bkv, f0=f0: e.tensor_copy(out=hsT[:, :, b, lo - b * 30:hi - b * 30], in_=bkv[:, :, lo - f0:hi - f0]),
                   reads=[bk], writes=[hsT])
        bk = banks[6]
        for c in range(4):
            op("pe", lambda e, c=c: e.transpose(out=bk[:, c * 128:c * 128 + 32], in_=us_f[0:32, c * 128:(c + 1) * 128], identity=identf[0:32, 0:32]),
               reads=[us_f, identf], writes=[bk])
        op("dve", lambda e: e.tensor_copy(out=hsT[:, :, :, 30], in_=bk[:, :].rearrange("p (c n) -> p c n", c=4)[:, :, 0:32]), reads=[bk], writes=[hsT])
        op("pool", lambda e: e.tensor_tensor(out=prod[:], in0=hsT[:], in1=cw[:].unsqueeze(2).broadcast_to([128, 4, 32, 31]), op=ALU.mult), reads=[hsT, cw], writes=[prod])
        CS = cT[0]
        op("pool", lambda e: e.memset(CS[:, :, 0:128], 0.0), writes=[CS])
        op("dve", lambda e: e.tensor_reduce(out=CS[:, :, 0:32], in_=prod[:], axis=AX.X, op=ALU.add), reads=[prod], writes=[CS])
        op("dve", lambda e: e.tensor_tensor(out=CS[:, :, 0:32], in0=CS[:, :, 0:32], in1=cb[:].unsqueeze(2).broadcast_to([128, 4, 32]), op=ALU.add), reads=[CS, cb], writes=[CS])
        ln_silu_tile(CS, 0, 16, banks[7])
        dma("sp", lambda e: e.dma_start(out=o_convs[:, 0:29, :], in_=stc_d.rearrange("(b t) c -> b t c", t=30)[:, 1:30, :]), writes=[out_b], track=out_b)
        dma("sp", lambda e: e.dma_start(out=o_convs[:, 29, :], in_=us_f[0:32, :]), reads=[us_f], writes=[out_b], track=out_b)
        AR.release(*p4_tiles)
        AR.release(uT)
        if stop_after <= 4:
            if dbg:
                dbg_dump("mix", mix[:].rearrange("p a b -> p (a b)"), [128, 17 * 1024], BF16, mix)
            fw.barrier()
            return nc

        acc = A("acc", [128, 17, D], F32)
        x2T = A("x2T", [128, 8, 17 * 128], BF16)
        comb = A("comb", [128, 17, 16], F32)
        wo = A("wo", [128, 8, D], BF16)
        wrb = A("wrb", [128, 8, 20], BF16)
        wrs = A("wrs", [128, 8, 20], F32)
        brb = A("brb", [128, 20], F32)
        g2b = A("g2b", [128, D], F32)
        stg = [A(f"stgG{i}", [128, 1024], F32) for i in range(2)]
        mixT = [A(f"mixT{i}", [128, 8, 128], BF16) for i in range(2)]
        xr = [A(f"xr{i}", [128, D], F32) for i in range(2)]
        x2 = [A(f"x2_{i}", [128, D], BF16) for i in range(2)]
        sqg = A("sqg", [128, D], BF16)
        ssg = [A(f"ssg{i}", [128, 1], F32) for i in range(2)]
        lg = A("lg", [128, 24], F32)
        gm8 = A("gm8", [128, 8], F32)
        ohg = A("ohg", [128, 4], F32)
        eg = A("eg", [128, 4], F32)
        sume = A("sume", [128, 2], F32)
        tmp44 = A("tmp44", [128, 4, 4], F32)
        ein = A("ein", [128, 8], F32)
        em8 = A("em8", [128, 8], F32)
        sel2 = A("sel2", [128, 4], F32)
        ee = A("ee", [128, 4], F32)
        wts = A("wts", [128, 4], F32)
        pG_tiles = [wo, wrb, wrs, brb, g2b] + stg + mixT + xr + x2 + [sqg] + ssg + [lg, gm8, ohg, eg, sume, tmp44, ein, em8, sel2, ee, wts]
        dma("sp", lambda e: e.dma_start(out=g2b[:], in_=g2.partition_broadcast(128)), writes=[g2b], track=g2b)
        dma("sp", lambda e: e.dma_start(out=brb[:], in_=br_d.partition_broadcast(128)), writes=[brb], track=brb)
        dma("sp", lambda e: e.dma_start(out=wrs[:], in_=wr_d.rearrange("(k p) n -> p k n", p=128)), writes=[wrs], track=wrs)
        op("pool", lambda e: e.tensor_copy(out=wrb[:], in_=wrs[:]), reads=[wrs], writes=[wrb])
        for kc in range(8):
            sg = stg[kc % 2]
            dma("sp", lambda e, sg=sg, kc=kc: e.dma_start(out=sg[:], in_=w_out[kc * 128:(kc + 1) * 128, :]), writes=[sg], track=sg)
            if kc % 2 == 0:
                op("pool", lambda e, sg=sg, kc=kc: e.tensor_copy(out=wo[:, kc, :], in_=sg[:]), reads=[sg], writes=[wo])
            else:
                op("act", lambda e, sg=sg, kc=kc: e.activation(out=wo[:, kc, :], in_=sg[:], func=AF.Copy), reads=[sg], writes=[wo])
        op("pool", lambda e: e.memset(ein[:, 4:8], -1.0e30), writes=[ein])
        for ti in range(17):
            i = ti % 2
            MT, XR, X2, SSG = mixT[i], xr[i], x2[i], ssg[i]
            pt_ = banks[0]
            ptv = bbf16(pt_).rearrange("p (c n) -> p c n", c=8)
            dma("sp", lambda e, XR=XR, ti=ti: e.dma_start(out=XR[:], in_=xall[(16 + ti) * 128:(17 + ti) * 128, :]), writes=[XR], track=XR)
            for c in range(8):
                op("pe", lambda e, c=c, ti=ti: e.transpose(out=ptv[:, c, :], in_=mix[:, ti, c * 128:(c + 1) * 128], identity=identb[:]), reads=[mix, identb], writes=[pt_])
            op("act", lambda e, MT=MT: e.activation(out=MT[:], in_=ptv, func=AF.Copy), reads=[pt_], writes=[MT])
            for half in range(2):
                po = banks[2 + half]
                for kc in range(8):
                    op("pe", lambda e, kc=kc, half=half, MT=MT, po=po: e.matmul(po[:, :], lhsT=MT[:, kc, :], rhs=wo[:, kc, half * 512:(half + 1) * 512], start=(kc == 0), stop=(kc == 7)),
                       reads=[MT, wo], writes=[po])
                op("dve", lambda e, half=half, po=po, XR=XR, ti=ti: e.tensor_tensor(out=acc[:, ti, half * 512:(half + 1) * 512], in0=po[:, :], in1=XR[:, half * 512:(half + 1) * 512], op=ALU.add),
                   reads=[po, XR], writes=[acc])
            op("act", lambda e, SSG=SSG, ti=ti: e.activation(out=sqg[:], in_=acc[:, ti, :], func=AF.Square, accum_out=SSG[:]), reads=[acc], writes=[sqg, SSG])
            rstd_from_ss(SSG, 1e-6, 1.0 / D)
            op("dve", lambda e, SSG=SSG, X2=X2, ti=ti: e.scalar_tensor_tensor(out=X2[:], in0=acc[:, ti, :], scalar=SSG[:], in1=g2b[:], op0=ALU.mult, op1=ALU.mult),
               reads=[acc, SSG, g2b], writes=[X2])
            pt2 = banks[1]
            pt2v = bbf16(pt2).rearrange("p (c n) -> p c n", c=8)
            for c in range(8):
                op("pe", lambda e, c=c, X2=X2: e.transpose(out=pt2v[:, c, :], in_=X2[:, c * 128:(c + 1) * 128], identity=identb[:]), reads=[X2, identb], writes=[pt2])
            op("act", lambda e, ti=ti: e.activation(out=x2T[:, :, ti * 128:(ti + 1) * 128], in_=pt2v, func=AF.Copy), reads=[pt2], writes=[x2T])
            pl = banks[4 + i]
            for kc in range(8):
                op("pe", lambda e, kc=kc, ti=ti, pl=pl: e.matmul(pl[:, 0:20], lhsT=x2T[:, kc, ti * 128:(ti + 1) * 128], rhs=wrb[:, kc, :], start=(kc == 0), stop=(kc == 7)),
                   reads=[x2T, wrb], writes=[pl])
            op("dve", lambda e, pl=pl: e.tensor_tensor(out=lg[:, 0:4], in0=pl[:, 0:4], in1=brb[:, 0:4], op=ALU.add), reads=[pl, brb], writes=[lg])
            op("dve", lambda e, pl=pl: e.tensor_tensor(out=lg[:, 8:24], in0=pl[:, 4:20], in1=brb[:, 4:20], op=ALU.add), reads=[pl, brb], writes=[lg])
            op("pool", lambda e: e.memset(lg[:, 4:8], -1.0e30), writes=[lg])
            op("dve", lambda e: e.max(out=gm8[:], in_=lg[:, 0:8]), reads=[lg], writes=[gm8])
            op("dve", lambda e: e.tensor_scalar(out=ohg[:], in0=lg[:, 0:4], scalar1=gm8[:, 0:1], scalar2=None, op0=ALU.is_ge), reads=[lg, gm8], writes=[ohg])
            op("dve", lambda e: e.tensor_scalar(out=gm8[:, 1:2], in0=gm8[:, 0:1], scalar1=-1.0, scalar2=None, op0=ALU.mult), reads=[gm8], writes=[gm8])
            op("act", lambda e: e.activation(out=eg[:], in_=lg[:, 0:4], func=AF.Exp, bias=gm8[:, 1:2], accum_out=sume[:, 0:1]), reads=[lg, gm8], writes=[eg, sume])
            op("dve", lambda e: e.tensor_tensor(out=tmp44[:], in0=lg[:, 8:24].rearrange("p (g x) -> p g x", g=4), in1=ohg[:].unsqueeze(2).broadcast_to([128, 4, 4]), op=ALU.mult),
               reads=[lg, ohg], writes=[tmp44])
            op("dve", lambda e: e.tensor_reduce(out=ein[:, 0:4], in_=tmp44[:].rearrange("p g x -> p x g"), axis=AX.X, op=ALU.add), reads=[tmp44], writes=[ein])
            op("dve", lambda e: e.max(out=em8[:], in_=ein[:]), reads=[ein], writes=[em8])
            op("dve", lambda e: e.tensor_scalar(out=sel2[:], in0=ein[:, 0:4], scalar1=em8[:, 1:2], scalar2=None, op0=ALU.is_ge), reads=[ein, em8], writes=[sel2])
            op("dve", lambda e: e.tensor_scalar(out=em8[:, 2:3], in0=em8[:, 0:1], scalar1=-1.0, scalar2=None, op0=ALU.mult), reads=[em8], writes=[em8])
            op("act", lambda e: e.activation(out=ee[:], in_=ein[:, 0:4], func=AF.Exp, bias=em8[:, 2:3]), reads=[ein, em8], writes=[ee])
            op("dve", lambda e: e.tensor_tensor(out=ee[:], in0=ee[:], in1=sel2[:], op=ALU.mult), reads=[ee, sel2], writes=[ee])
            op("dve", lambda e: e.reduce_sum(out=sume[:, 1:2], in_=ee[:], axis=AX.X), reads=[ee], writes=[sume])
            op("dve", lambda e: e.tensor_tensor(out=sume[:, 0:1], in0=sume[:, 0:1], in1=sume[:, 1:2], op=ALU.mult), reads=[sume], writes=[sume])
            op("dve", lambda e: e.reciprocal(out=sume[:, 0:1], in_=sume[:, 0:1]), reads=[sume], writes=[sume])
            op("dve", lambda e: e.tensor_scalar(out=wts[:], in0=ee[:], scalar1=sume[:, 0:1], scalar2=None, op0=ALU.mult), reads=[ee, sume], writes=[wts])
            op("dve", lambda e, ti=ti: e.tensor_tensor(out=comb[:, ti, :].rearrange("p (g x) -> p g x", g=4), in0=ohg[:].unsqueeze(2).broadcast_to([128, 4, 4]),
                                                      in1=wts[:].unsqueeze(1).broadcast_to([128, 4, 4]), op=ALU.mult),
               reads=[ohg, wts], writes=[comb])
        AR.release(*pG_tiles)
        AR.release(mix)
        if stop_after <= 5:
            if dbg:
                dbg_dump("acc", acc[:].rearrange("p a b -> p (a b)"), [128, 17 * 1024], F32, acc)
                dbg_dump("comb", comb[:].rearrange("p a b -> p (a b)"), [128, 17 * 16], F32, comb)
            fw.barrier()
            return nc

        wgb = [A(f"wgb{i}", [128, 8, 512], BF16) for i in range(2)]
        wub = [A(f"wub{i}", [128, 8, 512], BF16) for i in range(2)]
        wdb = [A(f"wdb{i}", [128, 4, D], BF16) for i in range(2)]
        stI = [A(f"stI{i}", [128, 2048], F32) for i in range(3)]
        sgt = [A(f"sgt{i}", [128, 512], F32) for i in range(2)]
        hT = [A(f"hT{i}", [128, 4, 512], BF16) for i in range(2)]
        pI_tiles = wgb + wub + wdb + stI + sgt + hT
        sti = [0]

        def load_expert(e_):
            i = e_ % 2
            srcs = []
            for hf in range(2):
                srcs.append((w_gate[e_, hf * 512:(hf + 1) * 512, :].rearrange("(k p) n -> p k n", p=128), wgb[i], lambda tl, hf=hf: tl[:, 4 * hf:4 * hf + 4, :], 4))
                srcs.append((w_up[e_, hf * 512:(hf + 1) * 512, :].rearrange("(k p) n -> p k n", p=128), wub[i], lambda tl, hf=hf: tl[:, 4 * hf:4 * hf + 4, :], 4))
            for hf in range(2):
                srcs.append((w_down[e_, hf * 256:(hf + 1) * 256, :].rearrange("(k p) n -> p k n", p=128), wdb[i], lambda tl, hf=hf: tl[:, 2 * hf:2 * hf + 2, :], 2))
            for (src, dst, view, k) in srcs:
                sg = stI[sti[0] % 3]
                sti[0] += 1
                dma("sp", lambda e, sg=sg, src=src, k=k: e.dma_start(out=sg[:].rearrange("p (k n) -> p k n", k=k), in_=src), writes=[sg], track=sg)
                op("pool", lambda e, sg=sg, dst=dst, view=view, k=k: e.tensor_copy(out=view(dst), in_=sg[:].rearrange("p (k n) -> p k n", k=k)), reads=[sg], writes=[dst])

        load_expert(0)
        gi = 0
        for e_ in range(16):
            if stop_after == 6 and e_ >= 2:
                break
            i = e_ % 2
            if e_ + 1 < 16:
                load_expert(e_ + 1)
            for tg in range(5):
                n = 512 if tg < 4 else 128
                tok0 = tg * 512
                H = hT[tg % 2]
                for fc in range(4):
                    pgt_ = banks[(gi % 2) * 2]
                    put_ = banks[(gi % 2) * 2 + 1]
                    gi += 1
                    for kc in range(8):
                        op("pe", lambda e, kc=kc, fc=fc, i=i, n=n, tok0=tok0, pgt_=pgt_: e.matmul(pgt_[:, 0:n], lhsT=wgb[i][:, kc, fc * 128:(fc + 1) * 128], rhs=x2T[:, kc, tok0:tok0 + n], start=(kc == 0), stop=(kc == 7)),
                           reads=[wgb[i], x2T], writes=[pgt_])
                    for kc in range(8):
                        op("pe", lambda e, kc=kc, fc=fc, i=i, n=n, tok0=tok0, put_=put_: e.matmul(put_[:, 0:n], lhsT=wub[i][:, kc, fc * 128:(fc + 1) * 128], rhs=x2T[:, kc, tok0:tok0 + n], start=(kc == 0), stop=(kc == 7)),
                           reads=[wub[i], x2T], writes=[put_])
                    SG = sgt[fc % 2]
                    op("act", lambda e, SG=SG, pgt_=pgt_, n=n: e.activation(out=SG[:, 0:n], in_=pgt_[:, 0:n], func=AF.Silu), reads=[pgt_], writes=[SG])
                    op("dve", lambda e, SG=SG, put_=put_, n=n, H=H, fc=fc: e.tensor_tensor(out=H[:, fc, 0:n], in0=put_[:, 0:n], in1=SG[:, 0:n], op=ALU.mult), reads=[put_, SG], writes=[H])
                for tt in range(n // 128):
                    ti = tg * 4 + tt
                    for half in range(2):
                        po = banks[4 + 2 * (ti % 2) + half]
                        for fc in range(4):
                            op("pe", lambda e, fc=fc, half=half, H=H, tt=tt, i=i, po=po: e.matmul(po[:, :], lhsT=H[:, fc, tt * 128:(tt + 1) * 128], rhs=wdb[i][:, fc, half * 512:(half + 1) * 512], start=(fc == 0), stop=(fc == 3)),
                               reads=[H, wdb[i]], writes=[po])
                        op("dve", lambda e, half=half, po=po, ti=ti, e_=e_: e.scalar_tensor_tensor(out=acc[:, ti, half * 512:(half + 1) * 512], in0=po[:, :], scalar=comb[:, ti, e_:e_ + 1],
                                                                                              in1=acc[:, ti, half * 512:(half + 1) * 512], op0=ALU.mult, op1=ALU.add),
                           reads=[po, comb, acc], writes=[acc])
        AR.release(*pI_tiles)
        AR.release(x2T)

        gfb = A("gfb", [128, D], F32)
        yt = [A(f"yt{i}", [128, D], F32) for i in range(2)]
        sqj = A("sqj", [128, D], BF16)
        ssj = [A(f"ssj{i}", [128, 1], F32) for i in range(2)]
        dma("sp", lambda e: e.dma_start(out=gfb[:], in_=gf.partition_broadcast(128)), writes=[gfb], track=gfb)
        for ti in range(17):
            i = ti % 2
            SSJ, Y = ssj[i], yt[i]
            op("act", lambda e, SSJ=SSJ, ti=ti: e.activation(out=sqj[:], in_=acc[:, ti, :], func=AF.Square, accum_out=SSJ[:]), reads=[acc], writes=[sqj, SSJ])
            rstd_from_ss(SSJ, 1e-6, 1.0 / D)
            op("dve", lambda e, SSJ=SSJ, Y=Y, ti=ti: e.scalar_tensor_tensor(out=Y[:], in0=acc[:, ti, :], scalar=SSJ[:], in1=gfb[:], op0=ALU.mult, op1=ALU.mult),
               reads=[acc, SSJ, gfb], writes=[Y])
            if ti < 16:
                dma("sp", lambda e, Y=Y, ti=ti: e.dma_start(out=o_y[ti * 128:(ti + 1) * 128, :], in_=Y[:]), reads=[Y], writes=[out_b], track=out_b)
            else:
                dma("sp", lambda e, Y=Y: e.dma_start(out=o_ys, in_=Y[:]), reads=[Y], writes=[out_b], track=out_b)
        fw.barrier()
    return nc


def build_sa():
    nc = bass.Bass("TRN2", target_bir_lowering=False)
    din = lambda n, s, d=F32: nc.dram_tensor(n, list(s), d, kind="ExternalInput").ap()
    xs = din("xs", [128, D])
    g1 = din("g1", [1, D])
    wq = din("wq", [D, 192])
    bq = din("bq", [1, 192])
    ropes = din("ropes", [128, 128])
    identf_d = din("identf", [128, 128])
    gsel_d = din("gsel", [96, 64])
    ck = din("ck", [5120, 8192])
    cv = din("cv", [5120, 8192])
    ptT_d = din("ptT", [128, 32], I32)
    pt_d = din("pt", [32, 128], I32)
    o_attn = nc.dram_tensor("o_attn", [32, 64], F32, kind="ExternalOutput").ap()
    scr_q = nc.dram_tensor("scr_q", [32, 64], F32, kind="Internal").ap()
    scr_p = nc.dram_tensor("scr_p", [32, 6], I32, kind="Internal").ap()

    with ExitStack() as st:
        fw = FW(nc, st)
        op, dma = fw.op, fw.dma
        arena_t = st.enter_context(nc.sbuf_tensor("arena", [128, ARENA_WORDS], F32))
        AR = Arena(fw, arena_t[:, :], ARENA_WORDS)
        A = AR.alloc
        banks = [Tl(st.enter_context(nc.psum_tensor(f"bank{i}", [128, 512], F32))[:, :], fw.buf(f"bank{i}")) for i in range(8)]
        out_b = fw.buf("outs")
        sq_b, sp_b = fw.buf("scrq"), fw.buf("scrp")

        identf = A("identf", [128, 128], F32)
        xt = A("xt", [128, D], F32)
        g1b = A("g1b", [128, D], F32)
        sqx = A("sqx", [128, D], F32)
        ssx = A("ssx", [128, 1], F32)
        xn = A("xn", [128, D], F32)
        xnT = A("xnT", [128, 8, 128], F32)
        wsb = A("wsb", [128, 8, 192], F32)
        bqb = A("bqb", [128, 192], F32)
        rp = A("rp", [128, 128], F32)
        z = A("z", [128, 192], F32)
        ta = A("ta", [128, 128], F32)
        tb = A("tb", [128, 128], F32)
        qk = A("qk", [128, 128], F32)
        ptT = A("ptT", [128, 32], I32)
        pti = A("pti", [32, 128], I32)
        ptf = A("ptf", [32, 128], F32)
        gsel = A("gsel", [96, 64], F32)
        for (t_, d_) in ((identf, identf_d), (xt, xs), (rp, ropes), (ptT, ptT_d), (pti, pt_d), (gsel, gsel_d)):
            dma("sp", lambda e, t_=t_, d_=d_: e.dma_start(out=t_[:], in_=d_), writes=[t_], track=t_)
        dma("sp", lambda e: e.dma_start(out=g1b[:], in_=g1.partition_broadcast(128)), writes=[g1b], track=g1b)
        dma("sp", lambda e: e.dma_start(out=bqb[:], in_=bq.partition_broadcast(128)), writes=[bqb], track=bqb)
        dma("sp", lambda e: e.dma_start(out=wsb[:], in_=wq.rearrange("(k p) n -> p k n", p=128)), writes=[wsb], track=wsb)
        op("dve", lambda e: e.tensor_copy(out=ptf[:], in_=pti[:]), reads=[pti], writes=[ptf])

        kbuf = [A(f"kbuf{i}", [128, 8192], F32) for i in range(2)]
        pagesum = A("pagesum", [128, 32, 64], F32)
        for b in range(32):
            KB = kbuf[b % 2]
            dma("pool", lambda e, KB=KB, b=b: e.indirect_dma_start(out=KB[:], out_offset=None, in_=ck, in_offset=bass.IndirectOffsetOnAxis(ap=ptT[:, b:b + 1], axis=0)),
                reads=[ptT], writes=[KB], track=KB)
            op("dve", lambda e, KB=KB, b=b: e.tensor_reduce(out=pagesum[:, b, :], in_=KB[:].rearrange("p (s d) -> p d s", d=64), axis=AX.X, op=ALU.add),
               reads=[KB], writes=[pagesum])

        op("act", lambda e: e.activation(out=sqx[:], in_=xt[:], func=AF.Square, accum_out=ssx[:]), reads=[xt], writes=[sqx, ssx])
        op("dve", lambda e: e.tensor_scalar(out=ssx[:], in0=ssx[:], scalar1=1.0 / D, scalar2=1e-6, op0=ALU.mult, op1=ALU.add), reads=[ssx], writes=[ssx])
        op("act", lambda e: e.activation(out=ssx[:], in_=ssx[:], func=AF.Sqrt), reads=[ssx], writes=[ssx])
        op("dve", lambda e: e.reciprocal(out=ssx[:], in_=ssx[:]), reads=[ssx], writes=[ssx])
        op("dve", lambda e: e.scalar_tensor_tensor(out=xn[:], in0=xt[:], scalar=ssx[:], in1=g1b[:], op0=ALU.mult, op1=ALU.mult), reads=[xt, ssx, g1b], writes=[xn])
        for c in range(8):
            bk = banks[c % 2]
            op("pe", lambda e, c=c, bk=bk: e.transpose(out=bk[:, 0:128], in_=xn[:, c * 128:(c + 1) * 128], identity=identf[:]), reads=[xn, identf], writes=[bk])
            op("dve", lambda e, c=c, bk=bk: e.tensor_copy(out=xnT[:, c, :], in_=bk[:, 0:128]), reads=[bk], writes=[xnT])
        pz = banks[2]
        for c in range(8):
            op("pe", lambda e, c=c: e.matmul(pz[:, 0:192], lhsT=xnT[:, c, :], rhs=wsb[:, c, :], start=(c == 0), stop=(c == 7)), reads=[xnT, wsb], writes=[pz])
        op("dve", lambda e: e.tensor_tensor(out=z[:], in0=pz[:, 0:192], in1=bqb[:], op=ALU.add), reads=[pz, bqb], writes=[z])
        zv = z[:, 0:128].rearrange("p (h d) -> p h d", h=2)
        tav = ta[:].rearrange("p (h d) -> p h d", h=2)
        tbv = tb[:].rearrange("p (h d) -> p h d", h=2)
        op("dve", lambda e: e.tensor_tensor(out=tav, in0=zv, in1=rp[:, 0:64].unsqueeze(1).broadcast_to([128, 2, 64]), op=ALU.mult), reads=[z, rp], writes=[ta])
        op("dve", lambda e: e.tensor_tensor(out=tbv[:, :, 0:32], in0=zv[:, :, 32:64], in1=rp[:, 64:96].unsqueeze(1).broadcast_to([128, 2, 32]), op=ALU.mult), reads=[z, rp], writes=[tb])
        op("dve", lambda e: e.tensor_tensor(out=tbv[:, :, 32:64], in0=zv[:, :, 0:32], in1=rp[:, 96:128].unsqueeze(1).broadcast_to([128, 2, 32]), op=ALU.mult), reads=[z, rp], writes=[tb])
        op("dve", lambda e: e.tensor_tensor(out=qk[:], in0=ta[:], in1=tb[:], op=ALU.add), reads=[ta, tb], writes=[qk])
        qbc = A("qbc", [128, 32, 64], F32)
        dma("sp", lambda e: e.dma_start(out=scr_q, in_=qk[0:32, 0:64]), reads=[qk], writes=[sq_b], track=sq_b)
        dma("sp", lambda e: e.dma_start(out=qbc[:].rearrange("p b d -> p (b d)"), in_=scr_q.rearrange("b d -> (b d)").unsqueeze(0).partition_broadcast(128)),
            reads=[sq_b], writes=[qbc], track=qbc)

        tmpg = A("tmpg", [128, 32, 64], F32)
        gp = A("gp", [128, 32], F32)
        gT = A("gT", [32, 128], F32)
        gate = A("gate", [32, 64], F32)
        m8 = A("m8", [32, 8], F32)
        eq = A("eq", [32, 3, 64], F32)
        tmp4 = A("tmp4", [32, 3, 2, 64], F32)
        psel = A("psel", [32, 6], F32)
        pseli = A("pseli", [32, 6], I32)
        op("dve", lambda e: e.tensor_tensor(out=tmpg[:], in0=pagesum[:], in1=qbc[:], op=ALU.mult), reads=[pagesum, qbc], writes=[tmpg])
        op("dve", lambda e: e.tensor_reduce(out=gp[:], in_=tmpg[:], axis=AX.X, op=ALU.add), reads=[tmpg], writes=[gp])
        pgT = banks[3]
        op("pe", lambda e: e.transpose(out=pgT[0:32, 0:128], in_=gp[:], identity=identf[:]), reads=[gp, identf], writes=[pgT])
        op("dve", lambda e: e.tensor_copy(out=gT[:], in_=pgT[0:32, 0:128]), reads=[pgT], writes=[gT])
        gTv = gT[:].rearrange("b (k t) -> b t k", t=2)
        op("dve", lambda e: e.tensor_tensor(out=gate[:], in0=gTv[:, 0, :], in1=gTv[:, 1, :], op=ALU.add), reads=[gT], writes=[gate])
        op("dve", lambda e: e.max(out=m8[:], in_=gate[:]), reads=[gate], writes=[m8])
        for j in range(3):
            op("dve", lambda e, j=j: e.tensor_scalar(out=eq[:, j, :], in0=gate[:], scalar1=m8[:, j:j + 1], scalar2=None, op0=ALU.is_equal), reads=[gate, m8], writes=[eq])
        ptv = ptf[:].rearrange("b (k t) -> b t k", t=2)
        op("dve", lambda e: e.tensor_tensor(out=tmp4[:], in0=eq[:].unsqueeze(2).broadcast_to([32, 3, 2, 64]), in1=ptv.unsqueeze(1).broadcast_to([32, 3, 2, 64]), op=ALU.mult),
           reads=[eq, ptf], writes=[tmp4])
        op("dve", lambda e: e.tensor_reduce(out=psel[:].rearrange("b (j t) -> b j t", j=3), in_=tmp4[:], axis=AX.X, op=ALU.add), reads=[tmp4], writes=[psel])
        op("dve", lambda e: e.tensor_copy(out=pseli[:], in_=psel[:]), reads=[psel], writes=[pseli])
        dma("sp", lambda e: e.dma_start(out=scr_p, in_=pseli[:]), reads=[pseli], writes=[sp_b], track=sp_b)

        AR.release(kbuf[0], kbuf[1], tmpg, qbc)
        ksel = A("ksel", [96, 8192], F32)
        vsel = A("vsel", [96, 8192], F32)
        prod = A("prod", [96, 8192], F32)
        idx = [A(f"idx{r}", [96, 1], I32) for r in range(2)]
        qrep = [A(f"qrep{r}", [96, 64], F32) for r in range(2)]
        S = A("S", [96, 128], F32)
        Pm = A("Pm", [96, 128], F32)
        pvr = A("pvr", [96, 65], F32)
        pacc = banks[4]
        for r in range(2):
            for s_ in range(6):
                dma("sp", lambda e, r=r, s_=s_: e.dma_start(out=idx[r][s_ * 16:(s_ + 1) * 16, :], in_=scr_p[16 * r:16 * r + 16, s_:s_ + 1], allow_slow_non_contiguous=True), reads=[sp_b], writes=[idx[r]], track=idx[r])
                dma("sp", lambda e, r=r, s_=s_: e.dma_start(out=qrep[r][s_ * 16:(s_ + 1) * 16, :], in_=scr_q[16 * r:16 * r + 16, :]), reads=[sq_b], writes=[qrep[r]], track=qrep[r])
            dma("pool", lambda e, r=r: e.indirect_dma_start(out=ksel[:], out_offset=None, in_=ck, in_offset=bass.IndirectOffsetOnAxis(ap=idx[r][:, 0:1], axis=0)),
                reads=[idx[r]], writes=[ksel], track=ksel)
            dma("pool", lambda e, r=r: e.indirect_dma_start(out=vsel[:], out_offset=None, in_=cv, in_offset=bass.IndirectOffsetOnAxis(ap=idx[r][:, 0:1], axis=0)),
                reads=[idx[r]], writes=[vsel], track=vsel)
            op("dve", lambda e, r=r: e.tensor_tensor(out=prod[:].rearrange("p (s d) -> p s d", d=64), in0=ksel[:].rearrange("p (s d) -> p s d", d=64),
                                                   in1=qrep[r][:].unsqueeze(1).broadcast_to([96, 128, 64]), op=ALU.mult), reads=[ksel, qrep[r]], writes=[prod])
            op("dve", lambda e: e.tensor_reduce(out=S[:], in_=prod[:].rearrange("p (s d) -> p s d", d=64), axis=AX.X, op=ALU.add), reads=[prod], writes=[S])
            op("act", lambda e: e.activation(out=Pm[:], in_=S[:], func=AF.Exp, scale=0.125, accum_out=pvr[:, 64:65]), reads=[S], writes=[Pm, pvr])
            op("dve", lambda e: e.tensor_tensor(out=prod[:].rearrange("p (s d) -> p s d", d=64), in0=vsel[:].rearrange("p (s d) -> p s d", d=64),
                                              in1=Pm[:].unsqueeze(2).broadcast_to([96, 128, 64]), op=ALU.mult), reads=[vsel, Pm], writes=[prod])
            op("dve", lambda e: e.tensor_reduce(out=pvr[:, 0:64], in_=prod[:].rearrange("p (s d) -> p d s", d=64), axis=AX.X, op=ALU.add), reads=[prod], writes=[pvr])
            op("pe", lambda e, r=r: e.matmul(pacc[0:32, 0:65], lhsT=gsel[:, 32 * r:32 * r + 32], rhs=pvr[:], start=(r == 0), stop=(r == 1)), reads=[gsel, pvr], writes=[pacc])
        ls = A("ls", [32, 1], F32)
        tq = A("tq", [32, 64], F32)
        num = A("num", [32, 65], F32)
        res = A("res", [32, 64], F32)
        op("dve", lambda e: e.tensor_tensor(out=tq[:], in0=qk[0:32, 0:64], in1=qk[0:32, 64:128], op=ALU.mult), reads=[qk], writes=[tq])
        op("dve", lambda e: e.reduce_sum(out=ls[:], in_=tq[:], axis=AX.X), reads=[tq], writes=[ls])
        op("act", lambda e: e.activation(out=ls[:], in_=ls[:], func=AF.Exp, scale=0.125), reads=[ls], writes=[ls])
        op("dve", lambda e: e.scalar_tensor_tensor(out=num[:, 0:64], in0=z[0:32, 128:192], scalar=ls[:], in1=pacc[0:32, 0:64], op0=ALU.mult, op1=ALU.add), reads=[z, ls, pacc], writes=[num])
        op("dve", lambda e: e.tensor_tensor(out=num[:, 64:65], in0=pacc[0:32, 64:65], in1=ls[:], op=ALU.add), reads=[pacc, ls], writes=[num])
        op("dve", lambda e: e.reciprocal(out=num[:, 64:65], in_=num[:, 64:65]), reads=[num], writes=[num])
        op("dve", lambda e: e.tensor_scalar(out=res[:], in0=num[:, 0:64], scalar1=num[:, 64:65], scalar2=None, op0=ALU.mult), reads=[num], writes=[res])
        dma("sp", lambda e: e.dma_start(out=o_attn, in_=res[:]), reads=[res], writes=[out_b], track=out_b)
        fw.barrier()
    return nc


def prep_sa(inp):
    f = lambda a: np.ascontiguousarray(np.asarray(a, dtype=np.float32))
    xs = f(inp["x_sample"])
    xsp = np.zeros((128, D), np.float32)
    xsp[:32] = xs[:, 0]
    w_in = f(inp["w_in"])[0]
    b_in = f(inp["b_in"])
    pt = np.ascontiguousarray(np.asarray(inp["page_table"], dtype=np.int32))
    ck = np.asarray(inp["cache_k"])[0]
    cv = np.asarray(inp["cache_v"])[0]
    gs = np.zeros((96, 64), np.float32)
    for r in range(2):
        for s_ in range(6):
            for b in range(16):
                gs[s_ * 16 + b, 32 * r + 16 * r + b] = 1.0
    maps = []
    for c in range(8):
        cols = np.concatenate([np.arange(64) + 64 * c, 512 + np.arange(64) + 64 * c, 1024 + np.arange(64) + 64 * c])
        maps.append(dict(
            xs=xsp, g1=f(inp["norm1_g"]), wq=np.ascontiguousarray(w_in[:, cols]), bq=np.ascontiguousarray(b_in[:, cols]),
            ropes=_rope_tables(np.full(128, 16384)), identf=np.eye(128, dtype=np.float32), gsel=gs,
            ck=np.ascontiguousarray(ck[:, :, c, :]).reshape(5120, 8192), cv=np.ascontiguousarray(cv[:, :, c, :]).reshape(5120, 8192),
            ptT=np.ascontiguousarray(pt.T), pt=pt))
    return maps


def _consts(s):
    c = {}
    c["identb"] = bf(np.eye(128))
    c["identf"] = np.eye(128, dtype=np.float32)
    es = np.zeros((128, 16, 16), np.float32)
    for j in range(16):
        es[:, j, j] = 1.0
    c["esel"] = bf(es.reshape(128, 256))
    sm = np.zeros((128, 8, 16), np.float32)
    oh = np.zeros((128, 8, 16), np.float32)
    for jb in range(8):
        sm[:, jb, 0:8] = 0.0 if s == 1 else NEG
        for sl in range(8):
            sm[:, jb, 8 + sl] = 0.0 if sl < jb else NEG
        oh[:, jb, 8 + jb] = 1.0
    c["selmask"] = sm.reshape(128, 128)
    c["ownhot"] = oh.reshape(128, 128)
    si = np.zeros((16, 4096), np.float32)
    for sl in range(16):
        si[sl, sl * 256:(sl + 1) * 256] = 1.0
    c["slotind"] = bf(si)
    key = np.arange(128)[:, None, None]
    jj = np.arange(4)[None, :, None]
    qq = np.arange(512)[None, None, :]
    c["causal4"] = bf((128 * jj + key <= qq).astype(np.float32).reshape(128, 2048))
    c["hflag"] = np.full((128, 1), float(s), np.float32)
    return c


def prep(inp, ncores=8):
    f = lambda a: np.ascontiguousarray(np.asarray(a, dtype=np.float32))
    xp = f(inp["x_prompt"])
    xs = f(inp["x_sample"])
    xsp = np.zeros((128, D), np.float32)
    xsp[:32] = xs[:, 0]
    cw = f(inp["conv_w"])[0]
    shared = dict(
        w_in=f(inp["w_in"])[0], b_in=f(inp["b_in"]), g1=f(inp["norm1_g"]), g2=f(inp["norm2_g"]),
        gf=f(inp["norm_f_g"]).reshape(1, D),
        convw=np.ascontiguousarray(cw.T.reshape(4, 128, 31).transpose(1, 0, 2)).reshape(128, 124),
        convb=np.ascontiguousarray(f(inp["conv_b"])[0].reshape(4, 128).T),
        lng=f(inp["conv_ln_g"]), lnb=f(inp["conv_ln_b"]),
        w_out=f(inp["w_out"])[0],
        wr=np.ascontiguousarray(np.concatenate([f(inp["w_group"])[0], f(inp["w_router"])[0]], axis=1)),
        br=np.ascontiguousarray(np.concatenate([f(inp["b_group"]), f(inp["b_router"])], axis=1)),
        w_gate=f(inp["w_gate"])[0], w_up=f(inp["w_up"])[0], w_down=f(inp["w_down"])[0],
        stc=f(inp["state_conv"])[0].reshape(32 * 30, 512),
    )
    maps = []
    for c in range(ncores):
        b, s = c // 2, c % 2
        xo = xp[b, 2048 * s:2048 * s + 2048]
        xh = xp[b, 2048 * (1 - s):2048 * (1 - s) + 2048]
        pos = np.concatenate([np.arange(2048) + 2048 * (1 - s), np.arange(2048) + 2048 * s, np.full(128, 16384)])
        m = dict(shared)
        m.update(_consts(s))
        m["xall"] = np.concatenate([xh, xo, xsp], axis=0)
        m["rope"] = _rope_tables(pos)
        maps.append(m)
    return maps


_NC_CACHE = {}


def kernel(**inputs):
    if "nca" not in _NC_CACHE:
        _NC_CACHE["nca"] = build_sa()
    resa = run_bass_kernel_spmd(_NC_CACHE["nca"], prep_sa(inputs), core_ids=list(range(8))).results
    attn_s = np.zeros((128, 512), np.float32)
    for c in range(8):
        attn_s[:32, 64 * c:64 * c + 64] = resa[c]["o_attn"]
    del resa
    maps = prep(inputs, 8)
    for m in maps:
        m["attn_s"] = attn_s
    if "nc" not in _NC_CACHE:
        _NC_CACHE["nc"] = build()
    nc = _NC_CACHE["nc"]
    res = run_bass_kernel_spmd(nc, maps, core_ids=list(range(8))).results
    y_prompt = np.zeros((4, 4096, D), np.float32)
    kp = np.zeros((1, 4, 4096, 8, 64), np.float32)
    vp = np.zeros((1, 4, 4096, 8, 64), np.float32)
    cp = np.zeros((1, 4, 30, 512), np.float32)
    for c in range(8):
        b, s = c // 2, c % 2
        sl = slice(2048 * s, 2048 * s + 2048)
        y_prompt[b, sl] = res[c]["o_y"]
        kp[0, b, sl] = res[c]["o_k"].reshape(2048, 8, 64)
        vp[0, b, sl] = res[c]["o_v"].reshape(2048, 8, 64)
        if s == 1:
            cp[0, b] = res[c]["o_convp"]
    y_sample = np.ascontiguousarray(res[0]["o_ys"][:32]).reshape(32, 1, D)
    ks = np.ascontiguousarray(res[0]["o_ks"][:32]).reshape(1, 32, 1, 8, 64)
    vs = np.ascontiguousarray(res[0]["o_vs"][:32]).reshape(1, 32, 1, 8, 64)
    cs = np.ascontiguousarray(res[0]["o_convs"]).reshape(1, 32, 30, 512)
    return (y_prompt, y_sample, kp, vp, cp, ks, vs, cs)
```

```python
import math
import numpy as np
import ml_dtypes
from contextlib import ExitStack
import concourse.bass as bass
import concourse.mybir as mybir
from concourse.bass_utils import run_bass_kernel_spmd

F32 = mybir.dt.float32
BF16 = mybir.dt.bfloat16
I32 = mybir.dt.int32
U32 = mybir.dt.uint32
AF = mybir.ActivationFunctionType
ALU = mybir.AluOpType
AX = mybir.AxisListType

D = 1024
NT_OTH = 16
NT_OWN = 16
NT = 33
T_OWN = 2048
NEG = -1.0e30


class Buf:
    __slots__ = ("name", "last_w", "readers", "dsem", "dcount")

    def __init__(self, name):
        self.name = name
        self.last_w = None
        self.readers = []
        self.dsem = None
        self.dcount = 0


class Tl:
    def __init__(self, t, buf):
        self.t = t
        self.b = buf

    def __getitem__(self, k):
        return self.t[k]


class FW:
    ENGS = ("pe", "act", "dve", "pool", "sp")

    def __init__(self, nc, stack):
        self.nc = nc
        self.stack = stack
        self.E = {"pe": nc.tensor, "act": nc.scalar, "dve": nc.vector, "pool": nc.gpsimd, "sp": nc.sync}
        self.sem = {}
        self.seq = {e: 0 for e in self.ENGS}
        self.known = {e: {} for e in self.ENGS}
        for e in self.ENGS:
            self.sem[e] = stack.enter_context(nc.semaphore("s_" + e))
        self.semkey = {id(self.sem[e]): e for e in self.ENGS}
        self.dsems = []
        self.free_dsems = []
        self.n = 0

    def buf(self, name=None):
        self.n += 1
        return Buf(name or f"b{self.n}")

    def sb(self, scope, name, shape, dt):
        self.n += 1
        return Tl(scope.enter_context(self.nc.sbuf_tensor(f"sb{self.n}_{name}", shape, dt)), self.buf(name))

    def ps(self, scope, name, shape, dt):
        self.n += 1
        return Tl(scope.enter_context(self.nc.psum_tensor(f"ps{self.n}_{name}", shape, dt)), self.buf(name))

    def _waits(self, eng, reads, writes):
        need = {}
        semobj = {}

        def add(ev):
            if ev is None:
                return
            s, v = ev
            k = id(s)
            semobj[k] = s
            if need.get(k, 0) < v:
                need[k] = v

        for b in reads:
            add(b.last_w)
        for b in writes:
            add(b.last_w)
            for r in b.readers:
                add(r)
        known = self.known[eng]
        for k, v in need.items():
            if known.get(k, 0) >= v:
                continue
            if eng == "pe" and self.semkey.get(k) == "pe":
                continue
            known[k] = v
            self.E[eng].wait_ge(semobj[k], v)

    def _record(self, ev, reads, writes):
        for b in reads:
            b.readers.append(ev)
            if len(b.readers) > 64:
                last = {}
                for (s, v) in b.readers:
                    if last.get(id(s), (None, 0))[1] < v:
                        last[id(s)] = (s, v)
                b.readers = list(last.values())
        for b in writes:
            b.last_w = ev
            b.readers = []

    @staticmethod
    def _bufs(xs):
        return [x.b if isinstance(x, Tl) else x for x in xs]

    def op(self, eng, fn, reads=(), writes=()):
        reads = self._bufs(reads)
        writes = self._bufs(writes)
        self._waits(eng, reads, writes)
        self.seq[eng] += 1
        s = self.sem[eng]
        ev = (s, self.seq[eng])
        fn(self.E[eng]).then_inc(s, 1)
        self._record(ev, reads, writes)
        return ev

    def dma(self, q, fn, reads=(), writes=(), track=None):
        reads = self._bufs(reads)
        writes = self._bufs(writes)
        self._waits(q, reads, writes)
        b = track.b if isinstance(track, Tl) else track
        if b.dsem is None:
            self.n += 1
            b.dsem = self.stack.enter_context(self.nc.semaphore(f"d{self.n}_" + b.name))
            self.dsems.append(b)
        b.dcount += 16
        ev = (b.dsem, b.dcount)
        fn(self.E[q]).then_inc(b.dsem, 16)
        self._record(ev, reads, writes)
        return ev

    def barrier(self):
        for e in self.ENGS:
            known = self.known[e]
            for f in self.ENGS:
                if f == e or self.seq[f] == 0:
                    continue
                k = id(self.sem[f])
                if known.get(k, 0) < self.seq[f]:
                    known[k] = self.seq[f]
                    self.E[e].wait_ge(self.sem[f], self.seq[f])
            for b in self.dsems:
                k = id(b.dsem)
                if known.get(k, 0) < b.dcount:
                    known[k] = b.dcount
                    self.E[e].wait_ge(b.dsem, b.dcount)


def _dsize(dt):
    return 4 if dt in (F32, I32, U32) else 2


class Arena:
    def __init__(self, fw, ap, words):
        self.fw = fw
        self.ap = ap
        self.words = words
        self.free = [(0, words)]
        self.ghosts = []

    def alloc(self, name, shape, dt):
        n = 1
        for d in shape[1:]:
            n *= d
        nbytes = n * _dsize(dt)
        w = (nbytes + 63) // 64 * 16
        for i, (s, e) in enumerate(self.free):
            if e - s >= w:
                break
        else:
            raise RuntimeError(f"arena out of space for {name} ({w * 4} B); free={self.free}")
        self.free[i:i + 1] = [(s + w, e)] if e - s > w else []
        v = self.ap[:, s:s + w]
        if dt != F32:
            v = v.bitcast(dt)
        v = v[0:shape[0], 0:n]
        if len(shape) > 2:
            names = " ".join(f"d{j}" for j in range(len(shape) - 1))
            kw = {f"d{j}": shape[j + 1] for j in range(len(shape) - 2)}
            v = v.rearrange(f"p ({names}) -> p {names}", **kw)
        b = self.fw.buf(name)
        keep = []
        for (gs, ge, evs) in self.ghosts:
            if gs < s + w and ge > s:
                b.readers.extend(evs)
                if gs >= s and ge <= s + w:
                    continue
            keep.append((gs, ge, evs))
        self.ghosts = keep
        tl = Tl(v, b)
        tl.region = (s, s + w)
        return tl

    def release(self, *tls):
        for tl in tls:
            s, e = tl.region
            evs = list(tl.b.readers)
            if tl.b.last_w is not None:
                evs.append(tl.b.last_w)
            last = {}
            for (sm, v) in evs:
                if last.get(id(sm), (None, 0))[1] < v:
                    last[id(sm)] = (sm, v)
            self.ghosts.append((s, e, list(last.values())))
            self.free.append((s, e))
            self.free.sort()
            merged = []
            for (a, b_) in self.free:
                if merged and merged[-1][1] == a:
                    merged[-1] = (merged[-1][0], b_)
                else:
                    merged.append((a, b_))
            self.free = merged


def _rope_tables(pos):
    half = 32
    inv_freq = np.exp(-math.log(10000.0) * np.arange(half, dtype=np.float32) / half).astype(np.float32)
    ang = pos.astype(np.float32)[:, None] * inv_freq[None, :]
    cos = np.cos(ang).astype(np.float32)
    sin = np.sin(ang).astype(np.float32)
    C = np.concatenate([cos, cos], axis=1)
    S = np.concatenate([-sin, sin], axis=1)
    return np.concatenate([C, S], axis=1).astype(np.float32)


def bf(x):
    return np.asarray(x, dtype=np.float32).astype(ml_dtypes.bfloat16)


ARENA_WORDS = 51200
IN_NAMES = []


def build(stop_after=99, dbg=False):
    nc = bass.Bass("TRN2", target_bir_lowering=False)
    IN_NAMES.clear()

    def din(n, s, d=F32):
        IN_NAMES.append(n)
        return nc.dram_tensor(n, list(s), d, kind="ExternalInput").ap()
    dout = lambda n, s, d=F32: nc.dram_tensor(n, list(s), d, kind="ExternalOutput").ap()

    xall = din("xall", [NT * 128, D])
    rope = din("rope", [NT * 128, 128])
    w_in = din("w_in", [D, 2560])
    b_in = din("b_in", [1, 2560])
    g1 = din("g1", [1, D])
    g2 = din("g2", [1, D])
    gf = din("gf", [1, D])
    hflag = din("hflag", [128, 1])
    identb_d = din("identb", [128, 128], BF16)
    identf_d = din("identf", [128, 128])
    esel_d = din("esel", [128, 256], BF16)
    selmask_d = din("selmask", [128, 128])
    ownhot_d = din("ownhot", [128, 128])
    slotind_d = din("slotind", [16, 4096], BF16)
    causal4_d = din("causal4", [128, 2048], BF16)
    convw_d = din("convw", [128, 124])
    convb_d = din("convb", [128, 4])
    lng_d = din("lng", [1, 512])
    lnb_d = din("lnb", [1, 512])
    w_out = din("w_out", [D, D])
    wr_d = din("wr", [D, 20])
    br_d = din("br", [1, 20])
    if stop_after >= 6:
        w_gate = din("w_gate", [16, D, 512])
        w_up = din("w_up", [16, D, 512])
        w_down = din("w_down", [16, 512, D])
    stc_d = din("stc", [32 * 30, 512])
    attn_s_d = din("attn_s", [128, 512])

    o_y = dout("o_y", [T_OWN, D])
    o_ys = dout("o_ys", [128, D])
    o_k = dout("o_k", [T_OWN, 512])
    o_v = dout("o_v", [T_OWN, 512])
    o_convp = dout("o_convp", [30, 512])
    o_ks = dout("o_ks", [128, 512])
    o_vs = dout("o_vs", [128, 512])
    o_convs = dout("o_convs", [32, 30, 512])
    dbgout = {}

    with ExitStack() as st:
        fw = FW(nc, st)
        op, dma = fw.op, fw.dma
        arena_t = st.enter_context(nc.sbuf_tensor("arena", [128, ARENA_WORDS], F32))
        AR = Arena(fw, arena_t[:, :], ARENA_WORDS)
        A = AR.alloc
        banks = [Tl(st.enter_context(nc.psum_tensor(f"bank{i}", [128, 512], F32))[:, :], fw.buf(f"bank{i}")) for i in range(8)]
        bbf16 = lambda bk: bk.t.bitcast(BF16)
        out_b = fw.buf("outs")

        def dbg_dump(name, tl, shape2d, dt, src=None):
            if not dbg:
                return
            d = dout("dbg_" + name, shape2d, dt)
            dma("sp", lambda e: e.dma_start(out=d, in_=tl), reads=[src], writes=[out_b], track=out_b)

        identb = A("identb", [128, 128], BF16)
        identf = A("identf", [128, 128], F32)
        hfl = A("hfl", [128, 1], F32)
        us_f = A("us_f", [128, 512], F32)
        ks_f = A("ks_f", [128, 512], F32)
        vs_f = A("vs_f", [128, 512], F32)
        dma("sp", lambda e: e.dma_start(out=identb[:], in_=identb_d), writes=[identb], track=identb)
        dma("sp", lambda e: e.dma_start(out=identf[:], in_=identf_d), writes=[identf], track=identf)
        dma("sp", lambda e: e.dma_start(out=hfl[:], in_=hflag), writes=[hfl], track=hfl)

        q_rot = A("q_rot", [128, 17, 512], BF16)
        k_rot = A("k_rot", [128, 32, 512], BF16)
        vaug = A("vaug", [128, 32, 8, 65], BF16)
        uT = A("uT", [128, 4, 30 + T_OWN], BF16)
        op("pool", lambda e: e.memset(vaug[:, :, :, 64:65], 1.0), writes=[vaug])

        def rstd_from_ss(SS, eps, scale):
            op("dve", lambda e: e.tensor_scalar(out=SS[:], in0=SS[:], scalar1=scale, scalar2=eps, op0=ALU.mult, op1=ALU.add), reads=[SS], writes=[SS])
            op("act", lambda e: e.activation(out=SS[:], in_=SS[:], func=AF.Sqrt), reads=[SS], writes=[SS])
            op("dve", lambda e: e.reciprocal(out=SS[:], in_=SS[:]), reads=[SS], writes=[SS])

        wbf = [A(f"wbf{k}", [128, 2560], BF16) for k in range(8)]
        stage = [A(f"stg{i}", [128, 1280], F32) for i in range(2)]
        g1b = A("g1b", [128, D], F32)
        bbf = A("bbf", [1, 2560], BF16)
        ones1 = A("ones1", [1, 128], BF16)
        xt = [A(f"xt{i}", [128, D], F32) for i in range(2)]
        rt = [A(f"rt{i}", [128, 128], F32) for i in range(2)]
        sq = A("sq", [128, D], BF16)
        ss = [A(f"ss{i}", [128, 1], F32) for i in range(2)]
        xn = [A(f"xn{i}", [128, D], BF16) for i in range(2)]
        xnT = [A(f"xnT{i}", [128, 8, 128], BF16) for i in range(2)]
        t1 = A("t1", [128, 512], F32)
        t2 = A("t2", [128, 512], F32)
        kf = A("kf", [128, 512], F32)
        vf = A("vf", [128, 512], F32)
        sig = A("sig", [128, 512], F32)
        uf = A("uf", [128, 512], F32)
        ub = A("ub", [128, 512], BF16)
        p1_tiles = wbf + stage + [g1b, bbf, ones1] + xt + rt + [sq] + ss + xn + xnT + [t1, t2, kf, vf, sig, uf, ub]
        pT = banks[0]
        pTv = bbf16(pT).rearrange("p (c n) -> p c n", c=8)
        pg = banks[1:7]

        dma("sp", lambda e: e.dma_start(out=g1b[:], in_=g1.partition_broadcast(128)), writes=[g1b], track=g1b)
        for hh in range(2):
            dma("sp", lambda e, hh=hh: e.dma_start(out=stage[1][0:1, :], in_=b_in[:, hh * 1280:(hh + 1) * 1280]), writes=[stage[1]], track=stage[1])
            op("pool", lambda e, hh=hh: e.tensor_copy(out=bbf[:, hh * 1280:(hh + 1) * 1280], in_=stage[1][0:1, :]), reads=[stage[1]], writes=[bbf])
        op("pool", lambda e: e.memset(ones1[:], 1.0), writes=[ones1])
        for kc in range(8):
            for hh in range(2):
                sg = stage[hh]
                dma("sp", lambda e, sg=sg, kc=kc, hh=hh: e.dma_start(out=sg[:], in_=w_in[kc * 128:(kc + 1) * 128, hh * 1280:(hh + 1) * 1280]), writes=[sg], track=sg)
                if hh == 0:
                    op("pool", lambda e, sg=sg, kc=kc, hh=hh: e.tensor_copy(out=wbf[kc][:, hh * 1280:(hh + 1) * 1280], in_=sg[:]), reads=[sg], writes=[wbf[kc]])
                else:
                    op("act", lambda e, sg=sg, kc=kc, hh=hh: e.activation(out=wbf[kc][:, hh * 1280:(hh + 1) * 1280], in_=sg[:], func=AF.Copy), reads=[sg], writes=[wbf[kc]])

        pgi = [0]

        def nextpg():
            p = pg[pgi[0] % 6]
            pgi[0] += 1
            return p

        def proj(p, xT, c0):
            for kc in range(8):
                op("pe", lambda e, kc=kc: e.matmul(p[:, :], lhsT=xT[:, kc, :], rhs=wbf[kc][:, c0:c0 + 512], start=(kc == 0), stop=False),
                   reads=[xT, wbf[kc]], writes=[p])
            op("pe", lambda e: e.matmul(p[:, :], lhsT=ones1[:], rhs=bbf[:, c0:c0 + 512], start=False, stop=True),
               reads=[ones1, bbf], writes=[p])

        def rope_evac(p, r, out_ap, out_tl):
            pv = p[:, :].rearrange("p (h d) -> p h d", h=8)
            tav = t1[:].rearrange("p (h d) -> p h d", h=8)
            tbv = t2[:].rearrange("p (h d) -> p h d", h=8)
            Cb = r[:, 0:64].unsqueeze(1).broadcast_to([128, 8, 64])
            S1 = r[:, 64:96].unsqueeze(1).broadcast_to([128, 8, 32])
            S2 = r[:, 96:128].unsqueeze(1).broadcast_to([128, 8, 32])
            op("dve", lambda e: e.tensor_tensor(out=tav, in0=pv, in1=Cb, op=ALU.mult), reads=[p, r], writes=[t1])
            op("dve", lambda e: e.tensor_tensor(out=tbv[:, :, 0:32], in0=pv[:, :, 32:64], in1=S1, op=ALU.mult), reads=[p, r], writes=[t2])
            op("dve", lambda e: e.tensor_tensor(out=tbv[:, :, 32:64], in0=pv[:, :, 0:32], in1=S2, op=ALU.mult), reads=[p, r], writes=[t2])
            op("pool", lambda e: e.tensor_tensor(out=out_ap, in0=t1[:], in1=t2[:], op=ALU.add), reads=[t1, t2], writes=[out_tl])

        def stageA(t):
            i = t % 2
            own = t >= NT_OTH
            smp = t == NT - 1
            need_u = own or t == NT_OTH - 1
            X, R, XN, XT, SS = xt[i], rt[i], xn[i], xnT[i], ss[i]
            dma("sp", lambda e, X=X, t=t: e.dma_start(out=X[:], in_=xall[t * 128:(t + 1) * 128, :]), writes=[X], track=X)
            dma("sp", lambda e, R=R, t=t: e.dma_start(out=R[:], in_=rope[t * 128:(t + 1) * 128, :]), writes=[R], track=R)
            op("act", lambda e, X=X, SS=SS: e.activation(out=sq[:], in_=X[:], func=AF.Square, accum_out=SS[:]), reads=[X], writes=[sq, SS])
            rstd_from_ss(SS, 1e-6, 1.0 / D)
            op("dve", lambda e, X=X, SS=SS, XN=XN: e.scalar_tensor_tensor(out=XN[:], in0=X[:], scalar=SS[:], in1=g1b[:], op0=ALU.mult, op1=ALU.mult),
               reads=[X, SS, g1b], writes=[XN])
            for c in range(8):
                op("pe", lambda e, c=c, XN=XN: e.transpose(out=pTv[:, c, :], in_=XN[:, c * 128:(c + 1) * 128], identity=identb[:]),
                   reads=[XN, identb], writes=[pT])
            op("act", lambda e, XT=XT: e.activation(out=XT[:], in_=pTv, func=AF.Copy), reads=[pT], writes=[XT])
        def stageB(t):
            i = t % 2
            own = t >= NT_OTH
            smp = t == NT - 1
            need_u = own or t == NT_OTH - 1
            X, R, XN, XT, SS = xt[i], rt[i], xn[i], xnT[i], ss[i]
            if own:
                p = nextpg()
                proj(p, XT, 0)
                rope_evac(p, R, q_rot[:, t - NT_OTH, :], q_rot)
            p = nextpg()
            proj(p, XT, 512)
            KF = ks_f if smp else kf
            rope_evac(p, R, KF[:], KF)
            if not smp:
                op("act", lambda e, KF=KF, t=t: e.activation(out=k_rot[:, t, :], in_=KF[:], func=AF.Copy), reads=[KF], writes=[k_rot])
            if own and not smp:
                r0 = (t - NT_OTH) * 128
                dma("sp", lambda e, KF=KF, r0=r0: e.dma_start(out=o_k[r0:r0 + 128, :], in_=KF[:]), reads=[KF], writes=[], track=KF)
            if smp:
                dma("sp", lambda e: e.dma_start(out=o_ks, in_=ks_f[:]), reads=[ks_f], writes=[], track=ks_f)
            p = nextpg()
            proj(p, XT, 1024)
            VF = vs_f if smp else vf
            op("act", lambda e, VF=VF, p=p: e.activation(out=VF[:], in_=p[:, :], func=AF.Copy), reads=[p], writes=[VF])
            if not smp:
                op("pool", lambda e, VF=VF, t=t: e.tensor_copy(out=vaug[:, t, :, 0:64], in_=VF[:].rearrange("p (h d) -> p h d", h=8)), reads=[VF], writes=[vaug])
            if own and not smp:
                r0 = (t - NT_OTH) * 128
                dma("sp", lambda e, VF=VF, r0=r0: e.dma_start(out=o_v[r0:r0 + 128, :], in_=VF[:]), reads=[VF], writes=[], track=VF)
            if smp:
                dma("sp", lambda e: e.dma_start(out=o_vs, in_=vs_f[:]), reads=[vs_f], writes=[], track=vs_f)
            if need_u:
                pa = nextpg()
                proj(pa, XT, 1536)
                pb = nextpg()
                proj(pb, XT, 2048)
                UF = us_f if smp else uf
                op("act", lambda e, pb=pb: e.activation(out=sig[:], in_=pb[:, :], func=AF.Sigmoid), reads=[pb], writes=[sig])
                op("dve", lambda e, pa=pa, UF=UF: e.tensor_tensor(out=UF[:], in0=pa[:, :], in1=sig[:], op=ALU.mult), reads=[pa, sig], writes=[UF])
                if not smp:
                    op("pool", lambda e, UF=UF: e.tensor_copy(out=ub[:], in_=UF[:]), reads=[UF], writes=[ub])
                    for c in range(4):
                        op("pe", lambda e, c=c: e.transpose(out=pTv[:, c, :], in_=ub[:, c * 128:(c + 1) * 128], identity=identb[:]),
                           reads=[ub, identb], writes=[pT])
                    if t == NT_OTH - 1:
                        op("dve", lambda e: e.tensor_scalar(out=uT[:, :, 0:30], in0=pTv[:, 0:4, 98:128], scalar1=hfl[:], scalar2=None, op0=ALU.mult),
                           reads=[pT, hfl], writes=[uT])
                    else:
                        c0 = 30 + (t - NT_OTH) * 128
                        op("act", lambda e, c0=c0: e.activation(out=uT[:, :, c0:c0 + 128], in_=pTv[:, 0:4, :], func=AF.Copy), reads=[pT], writes=[uT])
                    if t == NT - 2:
                        dma("sp", lambda e, UF=UF: e.dma_start(out=o_convp, in_=UF[98:128, :]), reads=[UF], writes=[], track=UF)
        stageA(0)
        for t in range(NT):
            if t + 1 < NT:
                stageA(t + 1)
            stageB(t)
        AR.release(*p1_tiles)
        if stop_after <= 1:
            fw.barrier()
            return nc

        bias_all = A("bias_all", [128, 16, 8, 16], BF16)
        esel = A("esel", [128, 16, 16], BF16)
        selmask = A("selmask", [128, 8, 16], F32)
        ownhot = A("ownhot", [128, 8, 16], F32)
        kmean = A("kmean", [16, 512], BF16)
        KM = A("KM", [128, 4, 32], BF16)
        qTc = [A(f"qTc{i}", [128, 4, 128], BF16) for i in range(2)]
        gate = A("gate", [128, 8, 16], F32)
        m8 = A("m8", [128, 8, 8], F32)
        thr = A("thr", [128, 8], F32)
        sel = A("sel", [128, 8, 16], F32)
        p2_tiles = [esel, selmask, ownhot, kmean, KM] + qTc + [gate, m8, thr, sel]
        dma("sp", lambda e: e.dma_start(out=esel[:].rearrange("p a b -> p (a b)"), in_=esel_d), writes=[esel], track=esel)
        dma("sp", lambda e: e.dma_start(out=selmask[:].rearrange("p a b -> p (a b)"), in_=selmask_d), writes=[selmask], track=selmask)
        dma("sp", lambda e: e.dma_start(out=ownhot[:].rearrange("p a b -> p (a b)"), in_=ownhot_d), writes=[ownhot], track=ownhot)
        ksum = banks[1]
        for t in range(32):
            op("pe", lambda e, t=t: e.matmul(ksum[0:16, :], lhsT=esel[:, t // 2, :], rhs=k_rot[:, t, :], start=(t == 0), stop=(t == 31)),
               reads=[esel, k_rot], writes=[ksum])
        op("dve", lambda e: e.tensor_scalar(out=kmean[:], in0=ksum[0:16, :], scalar1=1.0 / 256, scalar2=None, op0=ALU.mult), reads=[ksum], writes=[kmean])
        op("pool", lambda e: e.memset(KM[:], 0.0), writes=[KM])
        kmT = banks[2]
        kmTv = bbf16(kmT)
        for c in range(4):
            op("pe", lambda e, c=c: e.transpose(out=kmTv[:, c * 16:(c + 1) * 16], in_=kmean[:, c * 128:(c + 1) * 128], identity=identb[0:16, 0:16]),
               reads=[kmean, identb], writes=[kmT])
        kmTv3 = kmTv[:, 0:64].rearrange("p (c s) -> p c s", c=4)
        op("dve", lambda e: e.tensor_copy(out=KM[0:64, :, 0:16], in_=kmTv3[0:64]), reads=[kmT], writes=[KM])
        op("dve", lambda e: e.tensor_copy(out=KM[64:128, :, 16:32], in_=kmTv3[64:128]), reads=[kmT], writes=[KM])
        for qi in range(16):
            jb = qi // 2
            QT = qTc[qi % 2]
            ptq = banks[3 + (qi % 2)]
            ptqv = bbf16(ptq)[:, 0:512].rearrange("p (c n) -> p c n", c=4)
            for c in range(4):
                op("pe", lambda e, c=c, qi=qi, ptqv=ptqv: e.transpose(out=ptqv[:, c, :], in_=q_rot[:, qi, c * 128:(c + 1) * 128], identity=identb[:]),
                   reads=[q_rot, identb], writes=[ptq])
            op("act", lambda e, QT=QT, ptqv=ptqv: e.activation(out=QT[:], in_=ptqv, func=AF.Copy), reads=[ptq], writes=[QT])
            pgt = banks[5 + (qi % 2)]
            for c in range(4):
                op("pe", lambda e, c=c, QT=QT, pgt=pgt: e.matmul(pgt[:, c * 32:(c + 1) * 32], lhsT=QT[:, c, :], rhs=KM[:, c, :], start=True, stop=True),
                   reads=[QT, KM], writes=[pgt])
            op("dve", lambda e, pgt=pgt, jb=jb: e.tensor_tensor(out=gate[:], in0=pgt[:, 0:128].rearrange("p (h s) -> p h s", h=8),
                                                               in1=selmask[:, jb, :].unsqueeze(1).broadcast_to([128, 8, 16]), op=ALU.add),
               reads=[pgt, selmask], writes=[gate])
            for h in range(8):
                op("dve", lambda e, h=h: e.max(out=m8[:, h, :], in_=gate[:, h, :]), reads=[gate], writes=[m8])
            op("dve", lambda e: e.tensor_scalar(out=thr[:], in0=m8[:, :, 2], scalar1=-1.0e29, scalar2=None, op0=ALU.max), reads=[m8], writes=[thr])
            op("dve", lambda e: e.tensor_tensor(out=sel[:], in0=gate[:], in1=thr[:].unsqueeze(2).broadcast_to([128, 8, 16]), op=ALU.is_ge),
               reads=[gate, thr], writes=[sel])
            op("pool", lambda e, jb=jb: e.tensor_tensor(out=sel[:], in0=sel[:], in1=ownhot[:, jb, :].unsqueeze(1).broadcast_to([128, 8, 16]), op=ALU.add),
               reads=[sel, ownhot], writes=[sel])
            op("pool", lambda e, qi=qi: e.tensor_scalar(out=bias_all[:, qi, :, :], in0=sel[:], scalar1=1.0e30, scalar2=-1.0e30, op0=ALU.mult, op1=ALU.add),
               reads=[sel], writes=[bias_all])
        AR.release(*p2_tiles)
        if stop_after <= 2:
            if dbg:
                dbg_dump("bias", bias_all[:].rearrange("p a b c -> p (a b c)"), [128, 2048], BF16, bias_all)
            fw.barrier()
            return nc

        mix = A("mix", [128, 17, 1024], BF16)
        causal4 = A("causal4", [128, 4, 512], BF16)
        kTh = [A(f"kTh{i}", [80, 4096], BF16) for i in range(2)]
        qTh = [A(f"qTh{i}", [80, 2048], BF16) for i in range(2)]
        PT = [A(f"PT{i}", [128, 512], BF16) for i in range(4)]
        rec = A("rec", [128, 4], F32)
        p3_tiles = [causal4] + kTh + qTh + PT + [rec]
        dma("sp", lambda e: e.dma_start(out=causal4[:].rearrange("p a b -> p (a b)"), in_=causal4_d), writes=[causal4], track=causal4)
        for i in range(2):
            dma("sp", lambda e, i=i: e.dma_start(out=kTh[i][64:80, :], in_=slotind_d), writes=[kTh[i]], track=kTh[i])
        atmp = A("atmp", [128, 512], F32)
        dma("sp", lambda e: e.dma_start(out=atmp[:], in_=attn_s_d), writes=[atmp], track=atmp)
        op("pool", lambda e: e.tensor_copy(out=mix[:, 16, 0:512], in_=atmp[:]), reads=[atmp], writes=[mix])
        AR.release(atmp)
        ptr = banks[0]
        ptrv = bbf16(ptr).rearrange("p (c n) -> p c n", c=8)
        pbias = banks[0]
        psS = banks[1:4]
        psO = banks[4:8]
        def head_build_steps(h):
            KT, QT = kTh[h % 2], qTh[h % 2]
            steps = []
            for tb in range(4):
                def st_k(tb=tb):
                    for j in range(8):
                        t = tb * 8 + j
                        op("pe", lambda e, j=j, t=t: e.transpose(out=ptrv[0:64, j, :], in_=k_rot[:, t, h * 64:(h + 1) * 64], identity=identb[:]),
                           reads=[k_rot, identb], writes=[ptr])
                    op("dve", lambda e: e.tensor_copy(out=KT[0:64, tb * 1024:(tb + 1) * 1024], in_=bbf16(ptr)[0:64, :]), reads=[ptr], writes=[KT])
                steps.append(st_k)
            for tb in range(2):
                def st_q(tb=tb):
                    for j in range(8):
                        t = tb * 8 + j
                        op("pe", lambda e, j=j, t=t: e.transpose(out=ptrv[0:64, j, :], in_=q_rot[:, t, h * 64:(h + 1) * 64], identity=identb[:]),
                           reads=[q_rot, identb], writes=[ptr])
                    op("dve", lambda e: e.tensor_copy(out=QT[0:64, tb * 1024:(tb + 1) * 1024], in_=bbf16(ptr)[0:64, :]), reads=[ptr], writes=[QT])
                steps.append(st_q)
            for tb in range(4):
                def st_b(tb=tb):
                    for j in range(4):
                        qi = tb * 4 + j
                        op("pe", lambda e, j=j, qi=qi: e.matmul(pbias[64:80, j * 128:(j + 1) * 128], lhsT=bias_all[:, qi, h, :], rhs=identb[:], start=True, stop=True),
                           reads=[bias_all, identb], writes=[pbias])
                    op("dve", lambda e: e.tensor_copy(out=QT[64:80, tb * 512:(tb + 1) * 512], in_=pbias[64:80, :]), reads=[pbias], writes=[QT])
                steps.append(st_b)
            return steps

        for stp in head_build_steps(0):
            stp()
        jcount = [0]
        for h in range(8):
            KT, QT = kTh[h % 2], qTh[h % 2]
            nxt = head_build_steps(h + 1) if h + 1 < 8 else []
            jobs = [(g, kt) for g in range(4) for kt in range(20 + 4 * g)]

            def emit_qk(ji, jn):
                g, kt = jobs[ji]
                S = psS[jn % 3]
                op("pe", lambda e: e.matmul(S[:, :], lhsT=KT[:, kt * 128:(kt + 1) * 128], rhs=QT[:, g * 512:(g + 1) * 512], start=True, stop=True),
                   reads=[KT, QT], writes=[S])

            def emit_rest(ji, jn):
                g, kt = jobs[ji]
                nkt = 20 + 4 * g
                S = psS[jn % 3]
                P = PT[jn % 4]
                op("act", lambda e: e.activation(out=P[:], in_=S[:, :], func=AF.Exp, scale=0.125), reads=[S], writes=[P])
                jd = kt - (16 + 4 * g)
                if jd >= 0:
                    op("pool", lambda e: e.tensor_tensor(out=P[:], in0=P[:], in1=causal4[:, jd, :], op=ALU.mult), reads=[P, causal4], writes=[P])
                for qs in range(4):
                    if jd > qs:
                        continue
                    last = (kt == nkt - 1) or (kt == 16 + 4 * g + qs)
                    O = psO[qs]
                    op("pe", lambda e, O=O, qs=qs, last=last: e.matmul(O[:, 0:65], lhsT=P[:, qs * 128:(qs + 1) * 128], rhs=vaug[:, kt, h, :], start=(kt == 0), stop=last),
                       reads=[P, vaug], writes=[O])
                if kt == nkt - 1:
                    for qs in range(4):
                        O = psO[qs]
                        op("dve", lambda e, O=O, qs=qs: e.reciprocal(out=rec[:, qs:qs + 1], in_=O[:, 64:65]), reads=[O], writes=[rec])
                        op("dve", lambda e, O=O, qs=qs: e.tensor_scalar(out=mix[:, 4 * g + qs, h * 64:(h + 1) * 64], in0=O[:, 0:64], scalar1=rec[:, qs:qs + 1], scalar2=None, op0=ALU.mult),
                           reads=[O, rec], writes=[mix])

            emit_qk(0, jcount[0])
            emit_qk(1, jcount[0] + 1)
            for ji in range(len(jobs)):
                if ji + 2 < len(jobs):
                    emit_qk(ji + 2, jcount[0] + 2)
                emit_rest(ji, jcount[0])
                jcount[0] += 1
                if nxt and ji % 8 == 4:
                    nxt.pop(0)()
            while nxt:
                nxt.pop(0)()
        AR.release(*p3_tiles)
        AR.release(q_rot, k_rot, vaug, bias_all)
        if stop_after <= 3:
            if dbg:
                dbg_dump("mix", mix[:].rearrange("p a b -> p (a b)"), [128, 17 * 1024], BF16, mix)
            fw.barrier()
            return nc

        cw = A("cw", [128, 4, 31], F32)
        cb = A("cb", [128, 4], F32)
        lngb = A("lngb", [128, 512], F32)
        lnbb = A("lnbb", [128, 512], F32)
        dg = A("dg", [128, 4, 31, 128], BF16)
        cT = [A(f"cT{i}", [128, 4, 512], F32) for i in range(2)]
        st6 = A("st6", [128, 6], F32)
        mv = A("mv", [128, 2], F32)
        yn = [A(f"yn{i}", [128, 512], F32) for i in range(2)]
        hsT = A("hsT", [128, 4, 32, 31], F32)
        stc = [A(f"stc{i}", [128, 512], F32) for i in range(2)]
        prod = A("prod", [128, 4, 32, 31], F32)
        p4_tiles = [cw, cb, lngb, lnbb, dg] + cT + [st6, mv] + yn + [hsT, prod] + stc
        dma("sp", lambda e: e.dma_start(out=cw[:].rearrange("p a b -> p (a b)"), in_=convw_d), writes=[cw], track=cw)
        dma("sp", lambda e: e.dma_start(out=cb[:], in_=convb_d), writes=[cb], track=cb)
        dma("sp", lambda e: e.dma_start(out=lngb[:], in_=lng_d.partition_broadcast(128)), writes=[lngb], track=lngb)
        dma("sp", lambda e: e.dma_start(out=lnbb[:], in_=lnb_d.partition_broadcast(128)), writes=[lnbb], track=lnbb)
        for c in range(4):
            for tau in range(31):
                op("pool", lambda e, c=c, tau=tau: e.tensor_scalar(out=dg[:, c, tau, :], in0=identf[:], scalar1=cw[:, c, tau:tau + 1], scalar2=None, op0=ALU.mult),
                   reads=[identf, cw], writes=[dg])

        def ln_silu_tile(CT, col0, ti, bank):
            for c in range(4):
                op("pe", lambda e, c=c: e.transpose(out=bank[:, c * 128:(c + 1) * 128], in_=CT[:, c, col0:col0 + 128], identity=identf[:]),
                   reads=[CT, identf], writes=[bank])
            op("dve", lambda e: e.bn_stats(out=st6[:], in_=bank[:, :]), reads=[bank], writes=[st6])
            op("dve", lambda e: e.bn_aggr(out=mv[:], in_=st6[:]), reads=[st6], writes=[mv])
            op("dve", lambda e: e.tensor_scalar(out=mv[:, 1:2], in0=mv[:, 1:2], scalar1=1e-5, scalar2=None, op0=ALU.add), reads=[mv], writes=[mv])
            op("act", lambda e: e.activation(out=mv[:, 1:2], in_=mv[:, 1:2], func=AF.Sqrt), reads=[mv], writes=[mv])
            op("dve", lambda e: e.reciprocal(out=mv[:, 1:2], in_=mv[:, 1:2]), reads=[mv], writes=[mv])
            Y = yn[ti % 2]
            op("dve", lambda e: e.tensor_scalar(out=Y[:], in0=bank[:, :], scalar1=mv[:, 0:1], scalar2=mv[:, 1:2], op0=ALU.subtract, op1=ALU.mult),
               reads=[bank, mv], writes=[Y])
            op("pool", lambda e: e.tensor_tensor(out=Y[:], in0=Y[:], in1=lngb[:], op=ALU.mult), reads=[Y, lngb], writes=[Y])
            op("pool", lambda e: e.tensor_tensor(out=Y[:], in0=Y[:], in1=lnbb[:], op=ALU.add), reads=[Y, lnbb], writes=[Y])
            op("act", lambda e: e.activation(out=mix[:, ti, 512:1024], in_=Y[:], func=AF.Silu), reads=[Y], writes=[mix])

        for tg in range(4):
            CT = cT[tg % 2]
            for c in range(4):
                pc = banks[c % 2]
                for tau in range(31):
                    op("pe", lambda e, c=c, tau=tau, tg=tg, pc=pc: e.matmul(pc[:, :], lhsT=dg[:, c, tau, :], rhs=uT[:, c, tg * 512 + tau: tg * 512 + tau + 512], start=(tau == 0), stop=(tau == 30)),
                       reads=[dg, uT], writes=[pc])
                op("act", lambda e, c=c, pc=pc, CT=CT: e.activation(out=CT[:, c, :], in_=pc[:, :], func=AF.Identity, bias=cb[:, c:c + 1]), reads=[pc, cb], writes=[CT])
            for tt in range(4):
                ln_silu_tile(CT, tt * 128, tg * 4 + tt, banks[2 + (tt % 2)])
        for r in range(8):
            nrow = 128 if r < 7 else 64
            S_ = stc[r % 2]
            dma("sp", lambda e, S_=S_, r=r, nrow=nrow: e.dma_start(out=S_[0:nrow, :], in_=stc_d[r * 128:r * 128 + nrow, :]), writes=[S_], track=S_)
            bk = banks[4 + (r % 2)]
            for c in range(4):
                op("pe", lambda e, c=c, S_=S_, nrow=nrow, bk=bk: e.transpose(out=bk[:, c * 128:c * 128 + nrow], in_=S_[0:nrow, c * 128:(c + 1) * 128], identity=identf[0:nrow, 0:nrow]),
                   reads=[S_, identf], writes=[bk])
            bkv = bk[:, :].rearrange("p (c n) -> p c n", c=4)
            f0 = r * 128
            f1 = f0 + nrow
            b0 = f0 // 30
            b1 = (f1 - 1) // 30
            for b in range(b0, b1 + 1):
                lo = max(f0, b * 30)
                hi = min(f1, b * 30 + 30)
                op("dve", lambda e, b=b, lo=lo, hi=hi, bkv=bkv, f0=f0: e.tensor_copy(out=hsT[:, :, b, lo - b * 30:hi - b * 30], in_=bkv[:, :, lo - f0:hi - f0]),
                   reads=[bk], writes=[hsT])
        bk = banks[6]
        for c in range(4):
            op("pe", lambda e, c=c: e.transpose(out=bk[:, c * 128:c * 128 + 32], in_=us_f[0:32, c * 128:(c + 1) * 128], identity=identf[0:32, 0:32]),
               reads=[us_f, identf], writes=[bk])
        op("dve", lambda e: e.tensor_copy(out=hsT[:, :, :, 30], in_=bk[:, :].rearrange("p (c n) -> p c n", c=4)[:, :, 0:32]), reads=[bk], writes=[hsT])
        op("pool", lambda e: e.tensor_tensor(out=prod[:], in0=hsT[:], in1=cw[:].unsqueeze(2).broadcast_to([128, 4, 32, 31]), op=ALU.mult), reads=[hsT, cw], writes=[prod])
        CS = cT[0]
        op("pool", lambda e: e.memset(CS[:, :, 0:128], 0.0), writes=[CS])
        op("dve", lambda e: e.tensor_reduce(out=CS[:, :, 0:32], in_=prod[:], axis=AX.X, op=ALU.add), reads=[prod], writes=[CS])
        op("dve", lambda e: e.tensor_tensor(out=CS[:, :, 0:32], in0=CS[:, :, 0:32], in1=cb[:].unsqueeze(2).broadcast_to([128, 4, 32]), op=ALU.add), reads=[CS, cb], writes=[CS])
        ln_silu_tile(CS, 0, 16, banks[7])
        dma("sp", lambda e: e.dma_start(out=o_convs[:, 0:29, :], in_=stc_d.rearrange("(b t) c -> b t c", t=30)[:, 1:30, :]), writes=[out_b], track=out_b)
        dma("sp", lambda e: e.dma_start(out=o_convs[:, 29, :], in_=us_f[0:32, :]), reads=[us_f], writes=[out_b], track=out_b)
        AR.release(*p4_tiles)
        AR.release(uT)
        if stop_after <= 4:
            if dbg:
                dbg_dump("mix", mix[:].rearrange("p a b -> p (a b)"), [128, 17 * 1024], BF16, mix)
            fw.barrier()
            return nc

        acc = A("acc", [128, 17, D], F32)
        x2T = A("x2T", [128, 8, 17 * 128], BF16)
        comb = A("comb", [128, 17, 16], F32)
        wo = A("wo", [128, 8, D], BF16)
        wrb = A("wrb", [128, 8, 20], BF16)
        wrs = A("wrs", [128, 8, 20], F32)
        brb = A("brb", [128, 20], F32)
        g2b = A("g2b", [128, D], F32)
        stg = [A(f"stgG{i}", [128, 1024], F32) for i in range(2)]
        mixT = [A(f"mixT{i}", [128, 8, 128], BF16) for i in range(2)]
        xr = [A(f"xr{i}", [128, D], F32) for i in range(2)]
        x2 = [A(f"x2_{i}", [128, D], BF16) for i in range(2)]
        sqg = A("sqg", [128, D], BF16)
        ssg = [A(f"ssg{i}", [128, 1], F32) for i in range(2)]
        lg = A("lg", [128, 24], F32)
        gm8 = A("gm8", [128, 8], F32)
        ohg = A("ohg", [128, 4], F32)
        eg = A("eg", [128, 4], F32)
        sume = A("sume", [128, 2], F32)
        tmp44 = A("tmp44", [128, 4, 4], F32)
        ein = A("ein", [128, 8], F32)
        em8 = A("em8", [128, 8], F32)
        sel2 = A("sel2", [128, 4], F32)
        ee = A("ee", [128, 4], F32)
        wts = A("wts", [128, 4], F32)
        pG_tiles = [wo, wrb, wrs, brb, g2b] + stg + mixT + xr + x2 + [sqg] + ssg + [lg, gm8, ohg, eg, sume, tmp44, ein, em8, sel2, ee, wts]
        dma("sp", lambda e: e.dma_start(out=g2b[:], in_=g2.partition_broadcast(128)), writes=[g2b], track=g2b)
        dma("sp", lambda e: e.dma_start(out=brb[:], in_=br_d.partition_broadcast(128)), writes=[brb], track=brb)
        dma("sp", lambda e: e.dma_start(out=wrs[:], in_=wr_d.rearrange("(k p) n -> p k n", p=128)), writes=[wrs], track=wrs)
        op("pool", lambda e: e.tensor_copy(out=wrb[:], in_=wrs[:]), reads=[wrs], writes=[wrb])
        for kc in range(8):
            sg = stg[kc % 2]
            dma("sp", lambda e, sg=sg, kc=kc: e.dma_start(out=sg[:], in_=w_out[kc * 128:(kc + 1) * 128, :]), writes=[sg], track=sg)
            if kc % 2 == 0:
                op("pool", lambda e, sg=sg, kc=kc: e.tensor_copy(out=wo[:, kc, :], in_=sg[:]), reads=[sg], writes=[wo])
            else:
                op("act", lambda e, sg=sg, kc=kc: e.activation(out=wo[:, kc, :], in_=sg[:], func=AF.Copy), reads=[sg], writes=[wo])
        op("pool", lambda e: e.memset(ein[:, 4:8], -1.0e30), writes=[ein])
        for ti in range(17):
            i = ti % 2
            MT, XR, X2, SSG = mixT[i], xr[i], x2[i], ssg[i]
            pt_ = banks[0]
            ptv = bbf16(pt_).rearrange("p (c n) -> p c n", c=8)
            dma("sp", lambda e, XR=XR, ti=ti: e.dma_start(out=XR[:], in_=xall[(16 + ti) * 128:(17 + ti) * 128, :]), writes=[XR], track=XR)
            for c in range(8):
                op("pe", lambda e, c=c, ti=ti: e.transpose(out=ptv[:, c, :], in_=mix[:, ti, c * 128:(c + 1) * 128], identity=identb[:]), reads=[mix, identb], writes=[pt_])
            op("act", lambda e, MT=MT: e.activation(out=MT[:], in_=ptv, func=AF.Copy), reads=[pt_], writes=[MT])
            for half in range(2):
                po = banks[2 + half]
                for kc in range(8):
                    op("pe", lambda e, kc=kc, half=half, MT=MT, po=po: e.matmul(po[:, :], lhsT=MT[:, kc, :], rhs=wo[:, kc, half * 512:(half + 1) * 512], start=(kc == 0), stop=(kc == 7)),
                       reads=[MT, wo], writes=[po])
                op("dve", lambda e, half=half, po=po, XR=XR, ti=ti: e.tensor_tensor(out=acc[:, ti, half * 512:(half + 1) * 512], in0=po[:, :], in1=XR[:, half * 512:(half + 1) * 512], op=ALU.add),
                   reads=[po, XR], writes=[acc])
            op("act", lambda e, SSG=SSG, ti=ti: e.activation(out=sqg[:], in_=acc[:, ti, :], func=AF.Square, accum_out=SSG[:]), reads=[acc], writes=[sqg, SSG])
            rstd_from_ss(SSG, 1e-6, 1.0 / D)
            op("dve", lambda e, SSG=SSG, X2=X2, ti=ti: e.scalar_tensor_tensor(out=X2[:], in0=acc[:, ti, :], scalar=SSG[:], in1=g2b[:], op0=ALU.mult, op1=ALU.mult),
               reads=[acc, SSG, g2b], writes=[X2])
            pt2 = banks[1]
            pt2v = bbf16(pt2).rearrange("p (c n) -> p c n", c=8)
            for c in range(8):
                op("pe", lambda e, c=c, X2=X2: e.transpose(out=pt2v[:, c, :], in_=X2[:, c * 128:(c + 1) * 128], identity=identb[:]), reads=[X2, identb], writes=[pt2])
            op("act", lambda e, ti=ti: e.activation(out=x2T[:, :, ti * 128:(ti + 1) * 128], in_=pt2v, func=AF.Copy), reads=[pt2], writes=[x2T])
            pl = banks[4 + i]
            for kc in range(8):
                op("pe", lambda e, kc=kc, ti=ti, pl=pl: e.matmul(pl[:, 0:20], lhsT=x2T[:, kc, ti * 128:(ti + 1) * 128], rhs=wrb[:, kc, :], start=(kc == 0), stop=(kc == 7)),
                   reads=[x2T, wrb], writes=[pl])
            op("dve", lambda e, pl=pl: e.tensor_tensor(out=lg[:, 0:4], in0=pl[:, 0:4], in1=brb[:, 0:4], op=ALU.add), reads=[pl, brb], writes=[lg])
            op("dve", lambda e, pl=pl: e.tensor_tensor(out=lg[:, 8:24], in0=pl[:, 4:20], in1=brb[:, 4:20], op=ALU.add), reads=[pl, brb], writes=[lg])
            op("pool", lambda e: e.memset(lg[:, 4:8], -1.0e30), writes=[lg])
            op("dve", lambda e: e.max(out=gm8[:], in_=lg[:, 0:8]), reads=[lg], writes=[gm8])
            op("dve", lambda e: e.tensor_scalar(out=ohg[:], in0=lg[:, 0:4], scalar1=gm8[:, 0:1], scalar2=None, op0=ALU.is_ge), reads=[lg, gm8], writes=[ohg])
            op("dve", lambda e: e.tensor_scalar(out=gm8[:, 1:2], in0=gm8[:, 0:1], scalar1=-1.0, scalar2=None, op0=ALU.mult), reads=[gm8], writes=[gm8])
            op("act", lambda e: e.activation(out=eg[:], in_=lg[:, 0:4], func=AF.Exp, bias=gm8[:, 1:2], accum_out=sume[:, 0:1]), reads=[lg, gm8], writes=[eg, sume])
            op("dve", lambda e: e.tensor_tensor(out=tmp44[:], in0=lg[:, 8:24].rearrange("p (g x) -> p g x", g=4), in1=ohg[:].unsqueeze(2).broadcast_to([128, 4, 4]), op=ALU.mult),
               reads=[lg, ohg], writes=[tmp44])
            op("dve", lambda e: e.tensor_reduce(out=ein[:, 0:4], in_=tmp44[:].rearrange("p g x -> p x g"), axis=AX.X, op=ALU.add), reads=[tmp44], writes=[ein])
            op("dve", lambda e: e.max(out=em8[:], in_=ein[:]), reads=[ein], writes=[em8])
            op("dve", lambda e: e.tensor_scalar(out=sel2[:], in0=ein[:, 0:4], scalar1=em8[:, 1:2], scalar2=None, op0=ALU.is_ge), reads=[ein, em8], writes=[sel2])
            op("dve", lambda e: e.tensor_scalar(out=em8[:, 2:3], in0=em8[:, 0:1], scalar1=-1.0, scalar2=None, op0=ALU.mult), reads=[em8], writes=[em8])
            op("act", lambda e: e.activation(out=ee[:], in_=ein[:, 0:4], func=AF.Exp, bias=em8[:, 2:3]), reads=[ein, em8], writes=[ee])
            op("dve", lambda e: e.tensor_tensor(out=ee[:], in0=ee[:], in1=sel2[:], op=ALU.mult), reads=[ee, sel2], writes=[ee])
            op("dve", lambda e: e.reduce_sum(out=sume[:, 1:2], in_=ee[:], axis=AX.X), reads=[ee], writes=[sume])
            op("dve", lambda e: e.tensor_tensor(out=sume[:, 0:1], in0=sume[:, 0:1], in1=sume[:, 1:2], op=ALU.mult), reads=[sume], writes=[sume])
            op("dve", lambda e: e.reciprocal(out=sume[:, 0:1], in_=sume[:, 0:1]), reads=[sume], writes=[sume])
            op("dve", lambda e: e.tensor_scalar(out=wts[:], in0=ee[:], scalar1=sume[:, 0:1], scalar2=None, op0=ALU.mult), reads=[ee, sume], writes=[wts])
            op("dve", lambda e, ti=ti: e.tensor_tensor(out=comb[:, ti, :].rearrange("p (g x) -> p g x", g=4), in0=ohg[:].unsqueeze(2).broadcast_to([128, 4, 4]),
                                                      in1=wts[:].unsqueeze(1).broadcast_to([128, 4, 4]), op=ALU.mult),
               reads=[ohg, wts], writes=[comb])
        AR.release(*pG_tiles)
        AR.release(mix)
        if stop_after <= 5:
            if dbg:
                dbg_dump("acc", acc[:].rearrange("p a b -> p (a b)"), [128, 17 * 1024], F32, acc)
                dbg_dump("comb", comb[:].rearrange("p a b -> p (a b)"), [128, 17 * 16], F32, comb)
            fw.barrier()
            return nc

        wgb = [A(f"wgb{i}", [128, 8, 512], BF16) for i in range(2)]
        wub = [A(f"wub{i}", [128, 8, 512], BF16) for i in range(2)]
        wdb = [A(f"wdb{i}", [128, 4, D], BF16) for i in range(2)]
        stI = [A(f"stI{i}", [128, 2048], F32) for i in range(3)]
        sgt = [A(f"sgt{i}", [128, 512], F32) for i in range(2)]
        hT = [A(f"hT{i}", [128, 4, 512], BF16) for i in range(2)]
        pI_tiles = wgb + wub + wdb + stI + sgt + hT
        sti = [0]

        def load_expert(e_):
            i = e_ % 2
            srcs = []
            for hf in range(2):
                srcs.append((w_gate[e_, hf * 512:(hf + 1) * 512, :].rearrange("(k p) n -> p k n", p=128), wgb[i], lambda tl, hf=hf: tl[:, 4 * hf:4 * hf + 4, :], 4))
                srcs.append((w_up[e_, hf * 512:(hf + 1) * 512, :].rearrange("(k p) n -> p k n", p=128), wub[i], lambda tl, hf=hf: tl[:, 4 * hf:4 * hf + 4, :], 4))
            for hf in range(2):
                srcs.append((w_down[e_, hf * 256:(hf + 1) * 256, :].rearrange("(k p) n -> p k n", p=128), wdb[i], lambda tl, hf=hf: tl[:, 2 * hf:2 * hf + 2, :], 2))
            for (src, dst, view, k) in srcs:
                sg = stI[sti[0] % 3]
                sti[0] += 1
                dma("sp", lambda e, sg=sg, src=src, k=k: e.dma_start(out=sg[:].rearrange("p (k n) -> p k n", k=k), in_=src), writes=[sg], track=sg)
                op("pool", lambda e, sg=sg, dst=dst, view=view, k=k: e.tensor_copy(out=view(dst), in_=sg[:].rearrange("p (k n) -> p k n", k=k)), reads=[sg], writes=[dst])

        load_expert(0)
        gi = 0
        for e_ in range(16):
            if stop_after == 6 and e_ >= 2:
                break
            i = e_ % 2
            if e_ + 1 < 16:
                load_expert(e_ + 1)
            for tg in range(5):
                n = 512 if tg < 4 else 128
                tok0 = tg * 512
                H = hT[tg % 2]
                for fc in range(4):
                    pgt_ = banks[(gi % 2) * 2]
                    put_ = banks[(gi % 2) * 2 + 1]
                    gi += 1
                    for kc in range(8):
                        op("pe", lambda e, kc=kc, fc=fc, i=i, n=n, tok0=tok0, pgt_=pgt_: e.matmul(pgt_[:, 0:n], lhsT=wgb[i][:, kc, fc * 128:(fc + 1) * 128], rhs=x2T[:, kc, tok0:tok0 + n], start=(kc == 0), stop=(kc == 7)),
                           reads=[wgb[i], x2T], writes=[pgt_])
                    for kc in range(8):
                        op("pe", lambda e, kc=kc, fc=fc, i=i, n=n, tok0=tok0, put_=put_: e.matmul(put_[:, 0:n], lhsT=wub[i][:, kc, fc * 128:(fc + 1) * 128], rhs=x2T[:, kc, tok0:tok0 + n], start=(kc == 0), stop=(kc == 7)),
                           reads=[wub[i], x2T], writes=[put_])
                    SG = sgt[fc % 2]
                    op("act", lambda e, SG=SG, pgt_=pgt_, n=n: e.activation(out=SG[:, 0:n], in_=pgt_[:, 0:n], func=AF.Silu), reads=[pgt_], writes=[SG])
                    op("dve", lambda e, SG=SG, put_=put_, n=n, H=H, fc=fc: e.tensor_tensor(out=H[:, fc, 0:n], in0=put_[:, 0:n], in1=SG[:, 0:n], op=ALU.mult), reads=[put_, SG], writes=[H])
                for tt in range(n // 128):
                    ti = tg * 4 + tt
                    for half in range(2):
                        po = banks[4 + 2 * (ti % 2) + half]
                        for fc in range(4):
                            op("pe", lambda e, fc=fc, half=half, H=H, tt=tt, i=i, po=po: e.matmul(po[:, :], lhsT=H[:, fc, tt * 128:(tt + 1) * 128], rhs=wdb[i][:, fc, half * 512:(half + 1) * 512], start=(fc == 0), stop=(fc == 3)),
                               reads=[H, wdb[i]], writes=[po])
                        op("dve", lambda e, half=half, po=po, ti=ti, e_=e_: e.scalar_tensor_tensor(out=acc[:, ti, half * 512:(half + 1) * 512], in0=po[:, :], scalar=comb[:, ti, e_:e_ + 1],
                                                                                              in1=acc[:, ti, half * 512:(half + 1) * 512], op0=ALU.mult, op1=ALU.add),
                           reads=[po, comb, acc], writes=[acc])
        AR.release(*pI_tiles)
        AR.release(x2T)

        gfb = A("gfb", [128, D], F32)
        yt = [A(f"yt{i}", [128, D], F32) for i in range(2)]
        sqj = A("sqj", [128, D], BF16)
        ssj = [A(f"ssj{i}", [128, 1], F32) for i in range(2)]
        dma("sp", lambda e: e.dma_start(out=gfb[:], in_=gf.partition_broadcast(128)), writes=[gfb], track=gfb)
        for ti in range(17):
            i = ti % 2
            SSJ, Y = ssj[i], yt[i]
            op("act", lambda e, SSJ=SSJ, ti=ti: e.activation(out=sqj[:], in_=acc[:, ti, :], func=AF.Square, accum_out=SSJ[:]), reads=[acc], writes=[sqj, SSJ])
            rstd_from_ss(SSJ, 1e-6, 1.0 / D)
            op("dve", lambda e, SSJ=SSJ, Y=Y, ti=ti: e.scalar_tensor_tensor(out=Y[:], in0=acc[:, ti, :], scalar=SSJ[:], in1=gfb[:], op0=ALU.mult, op1=ALU.mult),
               reads=[acc, SSJ, gfb], writes=[Y])
            if ti < 16:
                dma("sp", lambda e, Y=Y, ti=ti: e.dma_start(out=o_y[ti * 128:(ti + 1) * 128, :], in_=Y[:]), reads=[Y], writes=[], track=Y)
            else:
                dma("sp", lambda e, Y=Y: e.dma_start(out=o_ys, in_=Y[:]), reads=[Y], writes=[], track=Y)
        fw.barrier()
    return nc


def build_sa():
    nc = bass.Bass("TRN2", target_bir_lowering=False)
    din = lambda n, s, d=F32: nc.dram_tensor(n, list(s), d, kind="ExternalInput").ap()
    xs = din("xs", [128, D])
    g1 = din("g1", [1, D])
    wq = din("wq", [D, 192])
    bq = din("bq", [1, 192])
    ropes = din("ropes", [128, 128])
    identf_d = din("identf", [128, 128])
    gsel_d = din("gsel", [96, 64])
    ck = din("ck", [5120, 8192])
    cv = din("cv", [5120, 8192])
    ptT_d = din("ptT", [128, 32], I32)
    pt_d = din("pt", [32, 128], I32)
    o_attn = nc.dram_tensor("o_attn", [32, 64], F32, kind="ExternalOutput").ap()
    scr_q = nc.dram_tensor("scr_q", [32, 64], F32, kind="Internal").ap()
    scr_p = nc.dram_tensor("scr_p", [32, 6], I32, kind="Internal").ap()

    with ExitStack() as st:
        fw = FW(nc, st)
        op, dma = fw.op, fw.dma
        arena_t = st.enter_context(nc.sbuf_tensor("arena", [128, ARENA_WORDS], F32))
        AR = Arena(fw, arena_t[:, :], ARENA_WORDS)
        A = AR.alloc
        banks = [Tl(st.enter_context(nc.psum_tensor(f"bank{i}", [128, 512], F32))[:, :], fw.buf(f"bank{i}")) for i in range(8)]
        out_b = fw.buf("outs")
        sq_b, sp_b = fw.buf("scrq"), fw.buf("scrp")

        identf = A("identf", [128, 128], F32)
        xt = A("xt", [128, D], F32)
        g1b = A("g1b", [128, D], F32)
        sqx = A("sqx", [128, D], F32)
        ssx = A("ssx", [128, 1], F32)
        xn = A("xn", [128, D], F32)
        xnT = A("xnT", [128, 8, 128], F32)
        wsb = A("wsb", [128, 8, 192], F32)
        bqb = A("bqb", [128, 192], F32)
        rp = A("rp", [128, 128], F32)
        z = A("z", [128, 192], F32)
        ta = A("ta", [128, 128], F32)
        tb = A("tb", [128, 128], F32)
        qk = A("qk", [128, 128], F32)
        ptT = A("ptT", [128, 32], I32)
        pti = A("pti", [32, 128], I32)
        ptf = A("ptf", [32, 128], F32)
        gsel = A("gsel", [96, 64], F32)
        for (t_, d_) in ((identf, identf_d), (xt, xs), (rp, ropes), (ptT, ptT_d), (pti, pt_d), (gsel, gsel_d)):
            dma("sp", lambda e, t_=t_, d_=d_: e.dma_start(out=t_[:], in_=d_), writes=[t_], track=t_)
        dma("sp", lambda e: e.dma_start(out=g1b[:], in_=g1.partition_broadcast(128)), writes=[g1b], track=g1b)
        dma("sp", lambda e: e.dma_start(out=bqb[:], in_=bq.partition_broadcast(128)), writes=[bqb], track=bqb)
        dma("sp", lambda e: e.dma_start(out=wsb[:], in_=wq.rearrange("(k p) n -> p k n", p=128)), writes=[wsb], track=wsb)
        op("dve", lambda e: e.tensor_copy(out=ptf[:], in_=pti[:]), reads=[pti], writes=[ptf])

        kbuf = [A(f"kbuf{i}", [128, 8192], F32) for i in range(2)]
        pagesum = A("pagesum", [128, 32, 64], F32)
        for b in range(32):
            KB = kbuf[b % 2]
            dma("pool", lambda e, KB=KB, b=b: e.indirect_dma_start(out=KB[:], out_offset=None, in_=ck, in_offset=bass.IndirectOffsetOnAxis(ap=ptT[:, b:b + 1], axis=0)),
                reads=[ptT], writes=[KB], track=KB)
            op("dve", lambda e, KB=KB, b=b: e.tensor_reduce(out=pagesum[:, b, :], in_=KB[:].rearrange("p (s d) -> p d s", d=64), axis=AX.X, op=ALU.add),
               reads=[KB], writes=[pagesum])

        op("act", lambda e: e.activation(out=sqx[:], in_=xt[:], func=AF.Square, accum_out=ssx[:]), reads=[xt], writes=[sqx, ssx])
        op("dve", lambda e: e.tensor_scalar(out=ssx[:], in0=ssx[:], scalar1=1.0 / D, scalar2=1e-6, op0=ALU.mult, op1=ALU.add), reads=[ssx], writes=[ssx])
        op("act", lambda e: e.activation(out=ssx[:], in_=ssx[:], func=AF.Sqrt), reads=[ssx], writes=[ssx])
        op("dve", lambda e: e.reciprocal(out=ssx[:], in_=ssx[:]), reads=[ssx], writes=[ssx])
        op("dve", lambda e: e.scalar_tensor_tensor(out=xn[:], in0=xt[:], scalar=ssx[:], in1=g1b[:], op0=ALU.mult, op1=ALU.mult), reads=[xt, ssx, g1b], writes=[xn])
        for c in range(8):
            bk = banks[c % 2]
            op("pe", lambda e, c=c, bk=bk: e.transpose(out=bk[:, 0:128], in_=xn[:, c * 128:(c + 1) * 128], identity=identf[:]), reads=[xn, identf], writes=[bk])
            op("dve", lambda e, c=c, bk=bk: e.tensor_copy(out=xnT[:, c, :], in_=bk[:, 0:128]), reads=[bk], writes=[xnT])
        pz = banks[2]
        for c in range(8):
            op("pe", lambda e, c=c: e.matmul(pz[:, 0:192], lhsT=xnT[:, c, :], rhs=wsb[:, c, :], start=(c == 0), stop=(c == 7)), reads=[xnT, wsb], writes=[pz])
        op("dve", lambda e: e.tensor_tensor(out=z[:], in0=pz[:, 0:192], in1=bqb[:], op=ALU.add), reads=[pz, bqb], writes=[z])
        zv = z[:, 0:128].rearrange("p (h d) -> p h d", h=2)
        tav = ta[:].rearrange("p (h d) -> p h d", h=2)
        tbv = tb[:].rearrange("p (h d) -> p h d", h=2)
        op("dve", lambda e: e.tensor_tensor(out=tav, in0=zv, in1=rp[:, 0:64].unsqueeze(1).broadcast_to([128, 2, 64]), op=ALU.mult), reads=[z, rp], writes=[ta])
        op("dve", lambda e: e.tensor_tensor(out=tbv[:, :, 0:32], in0=zv[:, :, 32:64], in1=rp[:, 64:96].unsqueeze(1).broadcast_to([128, 2, 32]), op=ALU.mult), reads=[z, rp], writes=[tb])
        op("dve", lambda e: e.tensor_tensor(out=tbv[:, :, 32:64], in0=zv[:, :, 0:32], in1=rp[:, 96:128].unsqueeze(1).broadcast_to([128, 2, 32]), op=ALU.mult), reads=[z, rp], writes=[tb])
        op("dve", lambda e: e.tensor_tensor(out=qk[:], in0=ta[:], in1=tb[:], op=ALU.add), reads=[ta, tb], writes=[qk])
        qbc = A("qbc", [128, 32, 64], F32)
        dma("sp", lambda e: e.dma_start(out=scr_q, in_=qk[0:32, 0:64]), reads=[qk], writes=[sq_b], track=sq_b)
        dma("sp", lambda e: e.dma_start(out=qbc[:].rearrange("p b d -> p (b d)"), in_=scr_q.rearrange("b d -> (b d)").unsqueeze(0).partition_broadcast(128)),
            reads=[sq_b], writes=[qbc], track=qbc)

        tmpg = A("tmpg", [128, 32, 64], F32)
        gp = A("gp", [128, 32], F32)
        gT = A("gT", [32, 128], F32)
        gate = A("gate", [32, 64], F32)
        m8 = A("m8", [32, 8], F32)
        eq = A("eq", [32, 3, 64], F32)
        tmp4 = A("tmp4", [32, 3, 2, 64], F32)
        psel = A("psel", [32, 6], F32)
        pseli = A("pseli", [32, 6], I32)
        op("dve", lambda e: e.tensor_tensor(out=tmpg[:], in0=pagesum[:], in1=qbc[:], op=ALU.mult), reads=[pagesum, qbc], writes=[tmpg])
        op("dve", lambda e: e.tensor_reduce(out=gp[:], in_=tmpg[:], axis=AX.X, op=ALU.add), reads=[tmpg], writes=[gp])
        pgT = banks[3]
        op("pe", lambda e: e.transpose(out=pgT[0:32, 0:128], in_=gp[:], identity=identf[:]), reads=[gp, identf], writes=[pgT])
        op("dve", lambda e: e.tensor_copy(out=gT[:], in_=pgT[0:32, 0:128]), reads=[pgT], writes=[gT])
        gTv = gT[:].rearrange("b (k t) -> b t k", t=2)
        op("dve", lambda e: e.tensor_tensor(out=gate[:], in0=gTv[:, 0, :], in1=gTv[:, 1, :], op=ALU.add), reads=[gT], writes=[gate])
        op("dve", lambda e: e.max(out=m8[:], in_=gate[:]), reads=[gate], writes=[m8])
        for j in range(3):
            op("dve", lambda e, j=j: e.tensor_scalar(out=eq[:, j, :], in0=gate[:], scalar1=m8[:, j:j + 1], scalar2=None, op0=ALU.is_equal), reads=[gate, m8], writes=[eq])
        ptv = ptf[:].rearrange("b (k t) -> b t k", t=2)
        op("dve", lambda e: e.tensor_tensor(out=tmp4[:], in0=eq[:].unsqueeze(2).broadcast_to([32, 3, 2, 64]), in1=ptv.unsqueeze(1).broadcast_to([32, 3, 2, 64]), op=ALU.mult),
           reads=[eq, ptf], writes=[tmp4])
        op("dve", lambda e: e.tensor_reduce(out=psel[:].rearrange("b (j t) -> b j t", j=3), in_=tmp4[:], axis=AX.X, op=ALU.add), reads=[tmp4], writes=[psel])
        op("dve", lambda e: e.tensor_copy(out=pseli[:], in_=psel[:]), reads=[psel], writes=[pseli])
        dma("sp", lambda e: e.dma_start(out=scr_p, in_=pseli[:]), reads=[pseli], writes=[sp_b], track=sp_b)

        AR.release(kbuf[0], kbuf[1], tmpg, qbc)
        ksel = A("ksel", [96, 8192], F32)
        vsel = A("vsel", [96, 8192], F32)
        prod = A("prod", [96, 8192], F32)
        idx = [A(f"idx{r}", [96, 1], I32) for r in range(2)]
        qrep = [A(f"qrep{r}", [96, 64], F32) for r in range(2)]
        S = A("S", [96, 128], F32)
        Pm = A("Pm", [96, 128], F32)
        pvr = A("pvr", [96, 65], F32)
        pacc = banks[4]
        for r in range(2):
            for s_ in range(6):
                dma("sp", lambda e, r=r, s_=s_: e.dma_start(out=idx[r][s_ * 16:(s_ + 1) * 16, :], in_=scr_p[16 * r:16 * r + 16, s_:s_ + 1], allow_slow_non_contiguous=True), reads=[sp_b], writes=[idx[r]], track=idx[r])
                dma("sp", lambda e, r=r, s_=s_: e.dma_start(out=qrep[r][s_ * 16:(s_ + 1) * 16, :], in_=scr_q[16 * r:16 * r + 16, :]), reads=[sq_b], writes=[qrep[r]], track=qrep[r])
            dma("pool", lambda e, r=r: e.indirect_dma_start(out=ksel[:], out_offset=None, in_=ck, in_offset=bass.IndirectOffsetOnAxis(ap=idx[r][:, 0:1], axis=0)),
                reads=[idx[r]], writes=[ksel], track=ksel)
            dma("pool", lambda e, r=r: e.indirect_dma_start(out=vsel[:], out_offset=None, in_=cv, in_offset=bass.IndirectOffsetOnAxis(ap=idx[r][:, 0:1], axis=0)),
                reads=[idx[r]], writes=[vsel], track=vsel)
            op("dve", lambda e, r=r: e.tensor_tensor(out=prod[:].rearrange("p (s d) -> p s d", d=64), in0=ksel[:].rearrange("p (s d) -> p s d", d=64),
                                                   in1=qrep[r][:].unsqueeze(1).broadcast_to([96, 128, 64]), op=ALU.mult), reads=[ksel, qrep[r]], writes=[prod])
            op("dve", lambda e: e.tensor_reduce(out=S[:], in_=prod[:].rearrange("p (s d) -> p s d", d=64), axis=AX.X, op=ALU.add), reads=[prod], writes=[S])
            op("act", lambda e: e.activation(out=Pm[:], in_=S[:], func=AF.Exp, scale=0.125, accum_out=pvr[:, 64:65]), reads=[S], writes=[Pm, pvr])
            op("dve", lambda e: e.tensor_tensor(out=prod[:].rearrange("p (s d) -> p s d", d=64), in0=vsel[:].rearrange("p (s d) -> p s d", d=64),
                                              in1=Pm[:].unsqueeze(2).broadcast_to([96, 128, 64]), op=ALU.mult), reads=[vsel, Pm], writes=[prod])
            op("dve", lambda e: e.tensor_reduce(out=pvr[:, 0:64], in_=prod[:].rearrange("p (s d) -> p d s", d=64), axis=AX.X, op=ALU.add), reads=[prod], writes=[pvr])
            op("pe", lambda e, r=r: e.matmul(pacc[0:32, 0:65], lhsT=gsel[:, 32 * r:32 * r + 32], rhs=pvr[:], start=(r == 0), stop=(r == 1)), reads=[gsel, pvr], writes=[pacc])
        ls = A("ls", [32, 1], F32)
        tq = A("tq", [32, 64], F32)
        num = A("num", [32, 65], F32)
        res = A("res", [32, 64], F32)
        op("dve", lambda e: e.tensor_tensor(out=tq[:], in0=qk[0:32, 0:64], in1=qk[0:32, 64:128], op=ALU.mult), reads=[qk], writes=[tq])
        op("dve", lambda e: e.reduce_sum(out=ls[:], in_=tq[:], axis=AX.X), reads=[tq], writes=[ls])
        op("act", lambda e: e.activation(out=ls[:], in_=ls[:], func=AF.Exp, scale=0.125), reads=[ls], writes=[ls])
        op("dve", lambda e: e.scalar_tensor_tensor(out=num[:, 0:64], in0=z[0:32, 128:192], scalar=ls[:], in1=pacc[0:32, 0:64], op0=ALU.mult, op1=ALU.add), reads=[z, ls, pacc], writes=[num])
        op("dve", lambda e: e.tensor_tensor(out=num[:, 64:65], in0=pacc[0:32, 64:65], in1=ls[:], op=ALU.add), reads=[pacc, ls], writes=[num])
        op("dve", lambda e: e.reciprocal(out=num[:, 64:65], in_=num[:, 64:65]), reads=[num], writes=[num])
        op("dve", lambda e: e.tensor_scalar(out=res[:], in0=num[:, 0:64], scalar1=num[:, 64:65], scalar2=None, op0=ALU.mult), reads=[num], writes=[res])
        dma("sp", lambda e: e.dma_start(out=o_attn, in_=res[:]), reads=[res], writes=[out_b], track=out_b)
        fw.barrier()
    return nc


def prep_sa(inp):
    f = lambda a: np.ascontiguousarray(np.asarray(a, dtype=np.float32))
    xs = f(inp["x_sample"])
    xsp = np.zeros((128, D), np.float32)
    xsp[:32] = xs[:, 0]
    w_in = f(inp["w_in"])[0]
    b_in = f(inp["b_in"])
    pt = np.ascontiguousarray(np.asarray(inp["page_table"], dtype=np.int32))
    ck = np.asarray(inp["cache_k"])[0]
    cv = np.asarray(inp["cache_v"])[0]
    gs = np.zeros((96, 64), np.float32)
    for r in range(2):
        for s_ in range(6):
            for b in range(16):
                gs[s_ * 16 + b, 32 * r + 16 * r + b] = 1.0
    maps = []
    for c in range(8):
        cols = np.concatenate([np.arange(64) + 64 * c, 512 + np.arange(64) + 64 * c, 1024 + np.arange(64) + 64 * c])
        maps.append(dict(
            xs=xsp, g1=f(inp["norm1_g"]), wq=np.ascontiguousarray(w_in[:, cols]), bq=np.ascontiguousarray(b_in[:, cols]),
            ropes=_rope_tables(np.full(128, 16384)), identf=np.eye(128, dtype=np.float32), gsel=gs,
            ck=np.ascontiguousarray(ck[:, :, c, :]).reshape(5120, 8192), cv=np.ascontiguousarray(cv[:, :, c, :]).reshape(5120, 8192),
            ptT=np.ascontiguousarray(pt.T), pt=pt))
    return maps


def _consts(s):
    c = {}
    c["identb"] = bf(np.eye(128))
    c["identf"] = np.eye(128, dtype=np.float32)
    es = np.zeros((128, 16, 16), np.float32)
    for j in range(16):
        es[:, j, j] = 1.0
    c["esel"] = bf(es.reshape(128, 256))
    sm = np.zeros((128, 8, 16), np.float32)
    oh = np.zeros((128, 8, 16), np.float32)
    for jb in range(8):
        sm[:, jb, 0:8] = 0.0 if s == 1 else NEG
        for sl in range(8):
            sm[:, jb, 8 + sl] = 0.0 if sl < jb else NEG
        oh[:, jb, 8 + jb] = 1.0
    c["selmask"] = sm.reshape(128, 128)
    c["ownhot"] = oh.reshape(128, 128)
    si = np.zeros((16, 4096), np.float32)
    for sl in range(16):
        si[sl, sl * 256:(sl + 1) * 256] = 1.0
    c["slotind"] = bf(si)
    key = np.arange(128)[:, None, None]
    jj = np.arange(4)[None, :, None]
    qq = np.arange(512)[None, None, :]
    c["causal4"] = bf((128 * jj + key <= qq).astype(np.float32).reshape(128, 2048))
    c["hflag"] = np.full((128, 1), float(s), np.float32)
    return c


def prep(inp, ncores=8):
    f = lambda a: np.ascontiguousarray(np.asarray(a, dtype=np.float32))
    xp = f(inp["x_prompt"])
    xs = f(inp["x_sample"])
    xsp = np.zeros((128, D), np.float32)
    xsp[:32] = xs[:, 0]
    cw = f(inp["conv_w"])[0]
    shared = dict(
        w_in=f(inp["w_in"])[0], b_in=f(inp["b_in"]), g1=f(inp["norm1_g"]), g2=f(inp["norm2_g"]),
        gf=f(inp["norm_f_g"]).reshape(1, D),
        convw=np.ascontiguousarray(cw.T.reshape(4, 128, 31).transpose(1, 0, 2)).reshape(128, 124),
        convb=np.ascontiguousarray(f(inp["conv_b"])[0].reshape(4, 128).T),
        lng=f(inp["conv_ln_g"]), lnb=f(inp["conv_ln_b"]),
        w_out=f(inp["w_out"])[0],
        wr=np.ascontiguousarray(np.concatenate([f(inp["w_group"])[0], f(inp["w_router"])[0]], axis=1)),
        br=np.ascontiguousarray(np.concatenate([f(inp["b_group"]), f(inp["b_router"])], axis=1)),
        w_gate=f(inp["w_gate"])[0], w_up=f(inp["w_up"])[0], w_down=f(inp["w_down"])[0],
        stc=f(inp["state_conv"])[0].reshape(32 * 30, 512),
    )
    maps = []
    for c in range(ncores):
        b, s = c // 2, c % 2
        xo = xp[b, 2048 * s:2048 * s + 2048]
        xh = xp[b, 2048 * (1 - s):2048 * (1 - s) + 2048]
        pos = np.concatenate([np.arange(2048) + 2048 * (1 - s), np.arange(2048) + 2048 * s, np.full(128, 16384)])
        m = dict(shared)
        m.update(_consts(s))
        m["xall"] = np.concatenate([xh, xo, xsp], axis=0)
        m["rope"] = _rope_tables(pos)
        maps.append(m)
    return maps


_NC_CACHE = {}


def kernel(**inputs):
    if "nca" not in _NC_CACHE:
        _NC_CACHE["nca"] = build_sa()
    resa = run_bass_kernel_spmd(_NC_CACHE["nca"], prep_sa(inputs), core_ids=list(range(8))).results
    attn_s = np.zeros((128, 512), np.float32)
    for c in range(8):
        attn_s[:32, 64 * c:64 * c + 64] = resa[c]["o_attn"]
    del resa
    maps = prep(inputs, 8)
    for m in maps:
        m["attn_s"] = attn_s
    if "nc" not in _NC_CACHE:
        _NC_CACHE["nc"] = build()
    nc = _NC_CACHE["nc"]
    res = run_bass_kernel_spmd(nc, maps, core_ids=list(range(8))).results
    y_prompt = np.zeros((4, 4096, D), np.float32)
    kp = np.zeros((1, 4, 4096, 8, 64), np.float32)
    vp = np.zeros((1, 4, 4096, 8, 64), np.float32)
    cp = np.zeros((1, 4, 30, 512), np.float32)
    for c in range(8):
        b, s = c // 2, c % 2
        sl = slice(2048 * s, 2048 * s + 2048)
        y_prompt[b, sl] = res[c]["o_y"]
        kp[0, b, sl] = res[c]["o_k"].reshape(2048, 8, 64)
        vp[0, b, sl] = res[c]["o_v"].reshape(2048, 8, 64)
        if s == 1:
            cp[0, b] = res[c]["o_convp"]
    y_sample = np.ascontiguousarray(res[0]["o_ys"][:32]).reshape(32, 1, D)
    ks = np.ascontiguousarray(res[0]["o_ks"][:32]).reshape(1, 32, 1, 8, 64)
    vs = np.ascontiguousarray(res[0]["o_vs"][:32]).reshape(1, 32, 1, 8, 64)
    cs = np.ascontiguousarray(res[0]["o_convs"]).reshape(1, 32, 30, 512)
    return (y_prompt, y_sample, kp, vp, cp, ks, vs, cs)
```

```python
import math
import numpy as np
import ml_dtypes
from contextlib import ExitStack
import concourse.bass as bass
import concourse.mybir as mybir
from concourse.bass_utils import run_bass_kernel_spmd

F32 = mybir.dt.float32
BF16 = mybir.dt.bfloat16
I32 = mybir.dt.int32
U32 = mybir.dt.uint32
AF = mybir.ActivationFunctionType
ALU = mybir.AluOpType
AX = mybir.AxisListType

D = 1024
NT_OTH = 16
NT_OWN = 16
NT = 33
T_OWN = 2048
NEG = -1.0e30


class Buf:
    __slots__ = ("name", "last_w", "readers", "dsem", "dcount")

    def __init__(self, name):
        self.name = name
        self.last_w = None
        self.readers = []
        self.dsem = None
        self.dcount = 0


class Tl:
    def __init__(self, t, buf):
        self.t = t
        self.b = buf

    def __getitem__(self, k):
        return self.t[k]


class FW:
    ENGS = ("pe", "act", "dve", "pool", "sp")

    def __init__(self, nc, stack):
        self.nc = nc
        self.stack = stack
        self.E = {"pe": nc.tensor, "act": nc.scalar, "dve": nc.vector, "pool": nc.gpsimd, "sp": nc.sync}
        self.sem = {}
        self.seq = {e: 0 for e in self.ENGS}
        self.known = {e: {} for e in self.ENGS}
        for e in self.ENGS:
            self.sem[e] = stack.enter_context(nc.semaphore("s_" + e))
        self.semkey = {id(self.sem[e]): e for e in self.ENGS}
        self.dsems = []
        self.free_dsems = []
        self.n = 0

    def buf(self, name=None):
        self.n += 1
        return Buf(name or f"b{self.n}")

    def sb(self, scope, name, shape, dt):
        self.n += 1
        return Tl(scope.enter_context(self.nc.sbuf_tensor(f"sb{self.n}_{name}", shape, dt)), self.buf(name))

    def ps(self, scope, name, shape, dt):
        self.n += 1
        return Tl(scope.enter_context(self.nc.psum_tensor(f"ps{self.n}_{name}", shape, dt)), self.buf(name))

    def _waits(self, eng, reads, writes):
        need = {}
        semobj = {}

        def add(ev):
            if ev is None:
                return
            s, v = ev
            k = id(s)
            semobj[k] = s
            if need.get(k, 0) < v:
                need[k] = v

        for b in reads:
            add(b.last_w)
        for b in writes:
            add(b.last_w)
            for r in b.readers:
                add(r)
        known = self.known[eng]
        for k, v in need.items():
            if known.get(k, 0) >= v:
                continue
            if eng == "pe" and self.semkey.get(k) == "pe":
                continue
            known[k] = v
            self.E[eng].wait_ge(semobj[k], v)

    def _record(self, ev, reads, writes):
        for b in reads:
            b.readers.append(ev)
            if len(b.readers) > 64:
                last = {}
                for (s, v) in b.readers:
                    if last.get(id(s), (None, 0))[1] < v:
                        last[id(s)] = (s, v)
                b.readers = list(last.values())
        for b in writes:
            b.last_w = ev
            b.readers = []

    @staticmethod
    def _bufs(xs):
        return [x.b if isinstance(x, Tl) else x for x in xs]

    def op(self, eng, fn, reads=(), writes=()):
        reads = self._bufs(reads)
        writes = self._bufs(writes)
        self._waits(eng, reads, writes)
        self.seq[eng] += 1
        s = self.sem[eng]
        ev = (s, self.seq[eng])
        fn(self.E[eng]).then_inc(s, 1)
        self._record(ev, reads, writes)
        return ev

    def dma(self, q, fn, reads=(), writes=(), track=None):
        reads = self._bufs(reads)
        writes = self._bufs(writes)
        self._waits(q, reads, writes)
        b = track.b if isinstance(track, Tl) else track
        if b.dsem is None:
            self.n += 1
            b.dsem = self.stack.enter_context(self.nc.semaphore(f"d{self.n}_" + b.name))
            self.dsems.append(b)
        b.dcount += 16
        ev = (b.dsem, b.dcount)
        fn(self.E[q]).then_inc(b.dsem, 16)
        self._record(ev, reads, writes)
        return ev

    def barrier(self):
        for e in self.ENGS:
            known = self.known[e]
            for f in self.ENGS:
                if f == e or self.seq[f] == 0:
                    continue
                k = id(self.sem[f])
                if known.get(k, 0) < self.seq[f]:
                    known[k] = self.seq[f]
                    self.E[e].wait_ge(self.sem[f], self.seq[f])
            for b in self.dsems:
                k = id(b.dsem)
                if known.get(k, 0) < b.dcount:
                    known[k] = b.dcount
                    self.E[e].wait_ge(b.dsem, b.dcount)


def _dsize(dt):
    return 4 if dt in (F32, I32, U32) else 2


class Arena:
    def __init__(self, fw, ap, words):
        self.fw = fw
        self.ap = ap
        self.words = words
        self.free = [(0, words)]
        self.ghosts = []

    def alloc(self, name, shape, dt):
        n = 1
        for d in shape[1:]:
            n *= d
        nbytes = n * _dsize(dt)
        w = (nbytes + 63) // 64 * 16
        for i, (s, e) in enumerate(self.free):
            if e - s >= w:
                break
        else:
            raise RuntimeError(f"arena out of space for {name} ({w * 4} B); free={self.free}")
        self.free[i:i + 1] = [(s + w, e)] if e - s > w else []
        v = self.ap[:, s:s + w]
        if dt != F32:
            v = v.bitcast(dt)
        v = v[0:shape[0], 0:n]
        if len(shape) > 2:
            names = " ".join(f"d{j}" for j in range(len(shape) - 1))
            kw = {f"d{j}": shape[j + 1] for j in range(len(shape) - 2)}
            v = v.rearrange(f"p ({names}) -> p {names}", **kw)
        b = self.fw.buf(name)
        keep = []
        for (gs, ge, evs) in self.ghosts:
            if gs < s + w and ge > s:
                b.readers.extend(evs)
                if gs >= s and ge <= s + w:
                    continue
            keep.append((gs, ge, evs))
        self.ghosts = keep
        tl = Tl(v, b)
        tl.region = (s, s + w)
        return tl

    def release(self, *tls):
        for tl in tls:
            s, e = tl.region
            evs = list(tl.b.readers)
            if tl.b.last_w is not None:
                evs.append(tl.b.last_w)
            last = {}
            for (sm, v) in evs:
                if last.get(id(sm), (None, 0))[1] < v:
                    last[id(sm)] = (sm, v)
            self.ghosts.append((s, e, list(last.values())))
            self.free.append((s, e))
            self.free.sort()
            merged = []
            for (a, b_) in self.free:
                if merged and merged[-1][1] == a:
                    merged[-1] = (merged[-1][0], b_)
                else:
                    merged.append((a, b_))
            self.free = merged


def _rope_tables(pos):
    half = 32
    inv_freq = np.exp(-math.log(10000.0) * np.arange(half, dtype=np.float32) / half).astype(np.float32)
    ang = pos.astype(np.float32)[:, None] * inv_freq[None, :]
    cos = np.cos(ang).astype(np.float32)
    sin = np.sin(ang).astype(np.float32)
    C = np.concatenate([cos, cos], axis=1)
    S = np.concatenate([-sin, sin], axis=1)
    return np.concatenate([C, S], axis=1).astype(np.float32)


def bf(x):
    return np.asarray(x, dtype=np.float32).astype(ml_dtypes.bfloat16)


ARENA_WORDS = 51200
IN_NAMES = []


def build(stop_after=99, dbg=False):
    nc = bass.Bass("TRN2", target_bir_lowering=False)
    IN_NAMES.clear()

    def din(n, s, d=F32):
        IN_NAMES.append(n)
        return nc.dram_tensor(n, list(s), d, kind="ExternalInput").ap()
    dout = lambda n, s, d=F32: nc.dram_tensor(n, list(s), d, kind="ExternalOutput").ap()

    xall = din("xall", [NT * 128, D])
    rope = din("rope", [NT * 128, 128])
    w_in = din("w_in", [D, 2560])
    b_in = din("b_in", [1, 2560])
    g1 = din("g1", [1, D])
    g2 = din("g2", [1, D])
    gf = din("gf", [1, D])
    hflag = din("hflag", [128, 1])
    identb_d = din("identb", [128, 128], BF16)
    identf_d = din("identf", [128, 128])
    esel_d = din("esel", [128, 256], BF16)
    selmask_d = din("selmask", [128, 128])
    ownhot_d = din("ownhot", [128, 128])
    slotind_d = din("slotind", [16, 4096], BF16)
    causal4_d = din("causal4", [128, 2048], BF16)
    convw_d = din("convw", [128, 124])
    convb_d = din("convb", [128, 4])
    lng_d = din("lng", [1, 512])
    lnb_d = din("lnb", [1, 512])
    w_out = din("w_out", [D, D])
    wr_d = din("wr", [D, 20])
    br_d = din("br", [1, 20])
    if stop_after >= 6:
        w_gate = din("w_gate", [16, D, 512])
        w_up = din("w_up", [16, D, 512])
        w_down = din("w_down", [16, 512, D])
    stc_d = din("stc", [32 * 30, 512])
    attn_s_d = din("attn_s", [128, 512])

    o_y = dout("o_y", [T_OWN, D])
    o_ys = dout("o_ys", [128, D])
    o_k = dout("o_k", [T_OWN, 512])
    o_v = dout("o_v", [T_OWN, 512])
    o_convp = dout("o_convp", [30, 512])
    o_ks = dout("o_ks", [128, 512])
    o_vs = dout("o_vs", [128, 512])
    o_convs = dout("o_convs", [32, 30, 512])
    dbgout = {}

    with ExitStack() as st:
        fw = FW(nc, st)
        op, dma = fw.op, fw.dma
        arena_t = st.enter_context(nc.sbuf_tensor("arena", [128, ARENA_WORDS], F32))
        AR = Arena(fw, arena_t[:, :], ARENA_WORDS)
        A = AR.alloc
        banks = [Tl(st.enter_context(nc.psum_tensor(f"bank{i}", [128, 512], F32))[:, :], fw.buf(f"bank{i}")) for i in range(8)]
        bbf16 = lambda bk: bk.t.bitcast(BF16)
        out_b = fw.buf("outs")

        def dbg_dump(name, tl, shape2d, dt, src=None):
            if not dbg:
                return
            d = dout("dbg_" + name, shape2d, dt)
            dma("sp", lambda e: e.dma_start(out=d, in_=tl), reads=[src], writes=[out_b], track=out_b)

        identb = A("identb", [128, 128], BF16)
        identf = A("identf", [128, 128], F32)
        hfl = A("hfl", [128, 1], F32)
        us_f = A("us_f", [128, 512], F32)
        ks_f = A("ks_f", [128, 512], F32)
        vs_f = A("vs_f", [128, 512], F32)
        dma("sp", lambda e: e.dma_start(out=identb[:], in_=identb_d), writes=[identb], track=identb)
        dma("sp", lambda e: e.dma_start(out=identf[:], in_=identf_d), writes=[identf], track=identf)
        dma("sp", lambda e: e.dma_start(out=hfl[:], in_=hflag), writes=[hfl], track=hfl)

        q_rot = A("q_rot", [128, 17, 512], BF16)
        k_rot = A("k_rot", [128, 32, 512], BF16)
        vaug = A("vaug", [128, 32, 8, 65], BF16)
        uT = A("uT", [128, 4, 30 + T_OWN], BF16)
        op("pool", lambda e: e.memset(vaug[:, :, :, 64:65], 1.0), writes=[vaug])

        def rstd_from_ss(SS, eps, scale):
            op("dve", lambda e: e.tensor_scalar(out=SS[:], in0=SS[:], scalar1=scale, scalar2=eps, op0=ALU.mult, op1=ALU.add), reads=[SS], writes=[SS])
            op("act", lambda e: e.activation(out=SS[:], in_=SS[:], func=AF.Sqrt), reads=[SS], writes=[SS])
            op("dve", lambda e: e.reciprocal(out=SS[:], in_=SS[:]), reads=[SS], writes=[SS])

        wbf = [A(f"wbf{k}", [128, 2560], BF16) for k in range(8)]
        stage = [A(f"stg{i}", [128, 1280], F32) for i in range(2)]
        g1b = A("g1b", [128, D], F32)
        bbf = A("bbf", [1, 2560], BF16)
        ones1 = A("ones1", [1, 128], BF16)
        xt = [A(f"xt{i}", [128, D], F32) for i in range(2)]
        rt = [A(f"rt{i}", [128, 128], F32) for i in range(2)]
        sq = A("sq", [128, D], BF16)
        ss = [A(f"ss{i}", [128, 1], F32) for i in range(2)]
        xn = [A(f"xn{i}", [128, D], BF16) for i in range(2)]
        xnT = [A(f"xnT{i}", [128, 8, 128], BF16) for i in range(2)]
        t1 = A("t1", [128, 512], F32)
        t2 = A("t2", [128, 512], F32)
        kf = A("kf", [128, 512], F32)
        vf = A("vf", [128, 512], F32)
        sig = A("sig", [128, 512], F32)
        uf = A("uf", [128, 512], F32)
        ub = A("ub", [128, 512], BF16)
        p1_tiles = wbf + stage + [g1b, bbf, ones1] + xt + rt + [sq] + ss + xn + xnT + [t1, t2, kf, vf, sig, uf, ub]
        pT = banks[0]
        pTv = bbf16(pT).rearrange("p (c n) -> p c n", c=8)
        pg = banks[1:7]

        dma("sp", lambda e: e.dma_start(out=g1b[:], in_=g1.partition_broadcast(128)), writes=[g1b], track=g1b)
        for hh in range(2):
            dma("sp", lambda e, hh=hh: e.dma_start(out=stage[1][0:1, :], in_=b_in[:, hh * 1280:(hh + 1) * 1280]), writes=[stage[1]], track=stage[1])
            op("pool", lambda e, hh=hh: e.tensor_copy(out=bbf[:, hh * 1280:(hh + 1) * 1280], in_=stage[1][0:1, :]), reads=[stage[1]], writes=[bbf])
        op("pool", lambda e: e.memset(ones1[:], 1.0), writes=[ones1])
        for kc in range(8):
            for hh in range(2):
                sg = stage[hh]
                dma("sp", lambda e, sg=sg, kc=kc, hh=hh: e.dma_start(out=sg[:], in_=w_in[kc * 128:(kc + 1) * 128, hh * 1280:(hh + 1) * 1280]), writes=[sg], track=sg)
                if hh == 0:
                    op("pool", lambda e, sg=sg, kc=kc, hh=hh: e.tensor_copy(out=wbf[kc][:, hh * 1280:(hh + 1) * 1280], in_=sg[:]), reads=[sg], writes=[wbf[kc]])
                else:
                    op("act", lambda e, sg=sg, kc=kc, hh=hh: e.activation(out=wbf[kc][:, hh * 1280:(hh + 1) * 1280], in_=sg[:], func=AF.Copy), reads=[sg], writes=[wbf[kc]])

        pgi = [0]

        def nextpg():
            p = pg[pgi[0] % 6]
            pgi[0] += 1
            return p

        def proj(p, xT, c0):
            for kc in range(8):
                op("pe", lambda e, kc=kc: e.matmul(p[:, :], lhsT=xT[:, kc, :], rhs=wbf[kc][:, c0:c0 + 512], start=(kc == 0), stop=False),
                   reads=[xT, wbf[kc]], writes=[p])
            op("pe", lambda e: e.matmul(p[:, :], lhsT=ones1[:], rhs=bbf[:, c0:c0 + 512], start=False, stop=True),
               reads=[ones1, bbf], writes=[p])

        def rope_evac(p, r, out_ap, out_tl):
            pv = p[:, :].rearrange("p (h d) -> p h d", h=8)
            tav = t1[:].rearrange("p (h d) -> p h d", h=8)
            tbv = t2[:].rearrange("p (h d) -> p h d", h=8)
            Cb = r[:, 0:64].unsqueeze(1).broadcast_to([128, 8, 64])
            S1 = r[:, 64:96].unsqueeze(1).broadcast_to([128, 8, 32])
            S2 = r[:, 96:128].unsqueeze(1).broadcast_to([128, 8, 32])
            op("dve", lambda e: e.tensor_tensor(out=tav, in0=pv, in1=Cb, op=ALU.mult), reads=[p, r], writes=[t1])
            op("dve", lambda e: e.tensor_tensor(out=tbv[:, :, 0:32], in0=pv[:, :, 32:64], in1=S1, op=ALU.mult), reads=[p, r], writes=[t2])
            op("dve", lambda e: e.tensor_tensor(out=tbv[:, :, 32:64], in0=pv[:, :, 0:32], in1=S2, op=ALU.mult), reads=[p, r], writes=[t2])
            op("pool", lambda e: e.tensor_tensor(out=out_ap, in0=t1[:], in1=t2[:], op=ALU.add), reads=[t1, t2], writes=[out_tl])

        def stageA(t):
            i = t % 2
            own = t >= NT_OTH
            smp = t == NT - 1
            need_u = own or t == NT_OTH - 1
            X, R, XN, XT, SS = xt[i], rt[i], xn[i], xnT[i], ss[i]
            dma("sp", lambda e, X=X, t=t: e.dma_start(out=X[:], in_=xall[t * 128:(t + 1) * 128, :]), writes=[X], track=X)
            dma("sp", lambda e, R=R, t=t: e.dma_start(out=R[:], in_=rope[t * 128:(t + 1) * 128, :]), writes=[R], track=R)
            op("act", lambda e, X=X, SS=SS: e.activation(out=sq[:], in_=X[:], func=AF.Square, accum_out=SS[:]), reads=[X], writes=[sq, SS])
            rstd_from_ss(SS, 1e-6, 1.0 / D)
            op("dve", lambda e, X=X, SS=SS, XN=XN: e.scalar_tensor_tensor(out=XN[:], in0=X[:], scalar=SS[:], in1=g1b[:], op0=ALU.mult, op1=ALU.mult),
               reads=[X, SS, g1b], writes=[XN])
            for c in range(8):
                op("pe", lambda e, c=c, XN=XN: e.transpose(out=pTv[:, c, :], in_=XN[:, c * 128:(c + 1) * 128], identity=identb[:]),
                   reads=[XN, identb], writes=[pT])
            op("act", lambda e, XT=XT: e.activation(out=XT[:], in_=pTv, func=AF.Copy), reads=[pT], writes=[XT])
        def stageB(t):
            i = t % 2
            own = t >= NT_OTH
            smp = t == NT - 1
            need_u = own or t == NT_OTH - 1
            X, R, XN, XT, SS = xt[i], rt[i], xn[i], xnT[i], ss[i]
            if own:
                p = nextpg()
                proj(p, XT, 0)
                rope_evac(p, R, q_rot[:, t - NT_OTH, :], q_rot)
            p = nextpg()
            proj(p, XT, 512)
            KF = ks_f if smp else kf
            rope_evac(p, R, KF[:], KF)
            if not smp:
                op("act", lambda e, KF=KF, t=t: e.activation(out=k_rot[:, t, :], in_=KF[:], func=AF.Copy), reads=[KF], writes=[k_rot])
            if own and not smp:
                r0 = (t - NT_OTH) * 128
                dma("sp", lambda e, KF=KF, r0=r0: e.dma_start(out=o_k[r0:r0 + 128, :], in_=KF[:]), reads=[KF], writes=[], track=KF)
            if smp:
                dma("sp", lambda e: e.dma_start(out=o_ks, in_=ks_f[:]), reads=[ks_f], writes=[], track=ks_f)
            p = nextpg()
            proj(p, XT, 1024)
            VF = vs_f if smp else vf
            op("act", lambda e, VF=VF, p=p: e.activation(out=VF[:], in_=p[:, :], func=AF.Copy), reads=[p], writes=[VF])
            if not smp:
                op("pool", lambda e, VF=VF, t=t: e.tensor_copy(out=vaug[:, t, :, 0:64], in_=VF[:].rearrange("p (h d) -> p h d", h=8)), reads=[VF], writes=[vaug])
            if own and not smp:
                r0 = (t - NT_OTH) * 128
                dma("sp", lambda e, VF=VF, r0=r0: e.dma_start(out=o_v[r0:r0 + 128, :], in_=VF[:]), reads=[VF], writes=[], track=VF)
            if smp:
                dma("sp", lambda e: e.dma_start(out=o_vs, in_=vs_f[:]), reads=[vs_f], writes=[], track=vs_f)
            if need_u:
                pa = nextpg()
                proj(pa, XT, 1536)
                pb = nextpg()
                proj(pb, XT, 2048)
                UF = us_f if smp else uf
                op("act", lambda e, pb=pb: e.activation(out=sig[:], in_=pb[:, :], func=AF.Sigmoid), reads=[pb], writes=[sig])
                op("dve", lambda e, pa=pa, UF=UF: e.tensor_tensor(out=UF[:], in0=pa[:, :], in1=sig[:], op=ALU.mult), reads=[pa, sig], writes=[UF])
                if not smp:
                    op("pool", lambda e, UF=UF: e.tensor_copy(out=ub[:], in_=UF[:]), reads=[UF], writes=[ub])
                    for c in range(4):
                        op("pe", lambda e, c=c: e.transpose(out=pTv[:, c, :], in_=ub[:, c * 128:(c + 1) * 128], identity=identb[:]),
                           reads=[ub, identb], writes=[pT])
                    if t == NT_OTH - 1:
                        op("dve", lambda e: e.tensor_scalar(out=uT[:, :, 0:30], in0=pTv[:, 0:4, 98:128], scalar1=hfl[:], scalar2=None, op0=ALU.mult),
                           reads=[pT, hfl], writes=[uT])
                    else:
                        c0 = 30 + (t - NT_OTH) * 128
                        op("act", lambda e, c0=c0: e.activation(out=uT[:, :, c0:c0 + 128], in_=pTv[:, 0:4, :], func=AF.Copy), reads=[pT], writes=[uT])
                    if t == NT - 2:
                        dma("sp", lambda e, UF=UF: e.dma_start(out=o_convp, in_=UF[98:128, :]), reads=[UF], writes=[], track=UF)
        stageA(0)
        for t in range(NT):
            if t + 1 < NT:
                stageA(t + 1)
            stageB(t)
        AR.release(*p1_tiles)
        if stop_after <= 1:
            fw.barrier()
            return nc

        bias_all = A("bias_all", [128, 16, 8, 16], BF16)
        esel = A("esel", [128, 16, 16], BF16)
        selmask = A("selmask", [128, 8, 16], F32)
        ownhot = A("ownhot", [128, 8, 16], F32)
        kmean = A("kmean", [16, 512], BF16)
        KM = A("KM", [128, 4, 32], BF16)
        qTc = [A(f"qTc{i}", [128, 4, 128], BF16) for i in range(2)]
        gate = A("gate", [128, 8, 16], F32)
        m8 = A("m8", [128, 8, 8], F32)
        thr = A("thr", [128, 8], F32)
        sel = A("sel", [128, 8, 16], F32)
        p2_tiles = [esel, selmask, ownhot, kmean, KM] + qTc + [gate, m8, thr, sel]
        dma("sp", lambda e: e.dma_start(out=esel[:].rearrange("p a b -> p (a b)"), in_=esel_d), writes=[esel], track=esel)
        dma("sp", lambda e: e.dma_start(out=selmask[:].rearrange("p a b -> p (a b)"), in_=selmask_d), writes=[selmask], track=selmask)
        dma("sp", lambda e: e.dma_start(out=ownhot[:].rearrange("p a b -> p (a b)"), in_=ownhot_d), writes=[ownhot], track=ownhot)
        ksum = banks[1]
        for t in range(32):
            op("pe", lambda e, t=t: e.matmul(ksum[0:16, :], lhsT=esel[:, t // 2, :], rhs=k_rot[:, t, :], start=(t == 0), stop=(t == 31)),
               reads=[esel, k_rot], writes=[ksum])
        op("dve", lambda e: e.tensor_scalar(out=kmean[:], in0=ksum[0:16, :], scalar1=1.0 / 256, scalar2=None, op0=ALU.mult), reads=[ksum], writes=[kmean])
        op("pool", lambda e: e.memset(KM[:], 0.0), writes=[KM])
        kmT = banks[2]
        kmTv = bbf16(kmT)
        for c in range(4):
            op("pe", lambda e, c=c: e.transpose(out=kmTv[:, c * 16:(c + 1) * 16], in_=kmean[:, c * 128:(c + 1) * 128], identity=identb[0:16, 0:16]),
               reads=[kmean, identb], writes=[kmT])
        kmTv3 = kmTv[:, 0:64].rearrange("p (c s) -> p c s", c=4)
        op("dve", lambda e: e.tensor_copy(out=KM[0:64, :, 0:16], in_=kmTv3[0:64]), reads=[kmT], writes=[KM])
        op("dve", lambda e: e.tensor_copy(out=KM[64:128, :, 16:32], in_=kmTv3[64:128]), reads=[kmT], writes=[KM])
        for qi in range(16):
            jb = qi // 2
            QT = qTc[qi % 2]
            ptq = banks[3 + (qi % 2)]
            ptqv = bbf16(ptq)[:, 0:512].rearrange("p (c n) -> p c n", c=4)
            for c in range(4):
                op("pe", lambda e, c=c, qi=qi, ptqv=ptqv: e.transpose(out=ptqv[:, c, :], in_=q_rot[:, qi, c * 128:(c + 1) * 128], identity=identb[:]),
                   reads=[q_rot, identb], writes=[ptq])
            op("act", lambda e, QT=QT, ptqv=ptqv: e.activation(out=QT[:], in_=ptqv, func=AF.Copy), reads=[ptq], writes=[QT])
            pgt = banks[5 + (qi % 2)]
            for c in range(4):
                op("pe", lambda e, c=c, QT=QT, pgt=pgt: e.matmul(pgt[:, c * 32:(c + 1) * 32], lhsT=QT[:, c, :], rhs=KM[:, c, :], start=True, stop=True),
                   reads=[QT, KM], writes=[pgt])
            op("dve", lambda e, pgt=pgt, jb=jb: e.tensor_tensor(out=gate[:], in0=pgt[:, 0:128].rearrange("p (h s) -> p h s", h=8),
                                                               in1=selmask[:, jb, :].unsqueeze(1).broadcast_to([128, 8, 16]), op=ALU.add),
               reads=[pgt, selmask], writes=[gate])
            for h in range(8):
                op("dve", lambda e, h=h: e.max(out=m8[:, h, :], in_=gate[:, h, :]), reads=[gate], writes=[m8])
            op("dve", lambda e: e.tensor_scalar(out=thr[:], in0=m8[:, :, 2], scalar1=-1.0e29, scalar2=None, op0=ALU.max), reads=[m8], writes=[thr])
            op("dve", lambda e: e.tensor_tensor(out=sel[:], in0=gate[:], in1=thr[:].unsqueeze(2).broadcast_to([128, 8, 16]), op=ALU.is_ge),
               reads=[gate, thr], writes=[sel])
            op("pool", lambda e, jb=jb: e.tensor_tensor(out=sel[:], in0=sel[:], in1=ownhot[:, jb, :].unsqueeze(1).broadcast_to([128, 8, 16]), op=ALU.add),
               reads=[sel, ownhot], writes=[sel])
            op("pool", lambda e, qi=qi: e.tensor_scalar(out=bias_all[:, qi, :, :], in0=sel[:], scalar1=1.0e30, scalar2=-1.0e30, op0=ALU.mult, op1=ALU.add),
               reads=[sel], writes=[bias_all])
        AR.release(*p2_tiles)
        if stop_after <= 2:
            if dbg:
                dbg_dump("bias", bias_all[:].rearrange("p a b c -> p (a b c)"), [128, 2048], BF16, bias_all)
            fw.barrier()
            return nc

        mix = A("mix", [128, 17, 1024], BF16)
        causal4 = A("causal4", [128, 4, 512], BF16)
        kTh = [A(f"kTh{i}", [80, 4096], BF16) for i in range(2)]
        qTh = [A(f"qTh{i}", [80, 2048], BF16) for i in range(2)]
        PT = [A(f"PT{i}", [128, 512], BF16) for i in range(4)]
        rec = A("rec", [128, 4], F32)
        p3_tiles = [causal4] + kTh + qTh + PT + [rec]
        dma("sp", lambda e: e.dma_start(out=causal4[:].rearrange("p a b -> p (a b)"), in_=causal4_d), writes=[causal4], track=causal4)
        for i in range(2):
            dma("sp", lambda e, i=i: e.dma_start(out=kTh[i][64:80, :], in_=slotind_d), writes=[kTh[i]], track=kTh[i])
        atmp = A("atmp", [128, 512], F32)
        dma("sp", lambda e: e.dma_start(out=atmp[:], in_=attn_s_d), writes=[atmp], track=atmp)
        op("pool", lambda e: e.tensor_copy(out=mix[:, 16, 0:512], in_=atmp[:]), reads=[atmp], writes=[mix])
        AR.release(atmp)
        ptr = banks[0]
        ptrv = bbf16(ptr).rearrange("p (c n) -> p c n", c=8)
        pbias = banks[0]
        psS = banks[1:4]
        psO = banks[4:8]
        def head_build_steps(h):
            KT, QT = kTh[h % 2], qTh[h % 2]
            steps = []
            for tb in range(4):
                def st_k(tb=tb):
                    for j in range(8):
                        t = tb * 8 + j
                        op("pe", lambda e, j=j, t=t: e.transpose(out=ptrv[0:64, j, :], in_=k_rot[:, t, h * 64:(h + 1) * 64], identity=identb[:]),
                           reads=[k_rot, identb], writes=[ptr])
                    op("dve", lambda e: e.tensor_copy(out=KT[0:64, tb * 1024:(tb + 1) * 1024], in_=bbf16(ptr)[0:64, :]), reads=[ptr], writes=[KT])
                steps.append(st_k)
            for tb in range(2):
                def st_q(tb=tb):
                    for j in range(8):
                        t = tb * 8 + j
                        op("pe", lambda e, j=j, t=t: e.transpose(out=ptrv[0:64, j, :], in_=q_rot[:, t, h * 64:(h + 1) * 64], identity=identb[:]),
                           reads=[q_rot, identb], writes=[ptr])
                    op("dve", lambda e: e.tensor_copy(out=QT[0:64, tb * 1024:(tb + 1) * 1024], in_=bbf16(ptr)[0:64, :]), reads=[ptr], writes=[QT])
                steps.append(st_q)
            for tb in range(4):
                def st_b(tb=tb):
                    for j in range(4):
                        qi = tb * 4 + j
                        op("pe", lambda e, j=j, qi=qi: e.matmul(pbias[64:80, j * 128:(j + 1) * 128], lhsT=bias_all[:, qi, h, :], rhs=identb[:], start=True, stop=True),
                           reads=[bias_all, identb], writes=[pbias])
                    op("dve", lambda e: e.tensor_copy(out=QT[64:80, tb * 512:(tb + 1) * 512], in_=pbias[64:80, :]), reads=[pbias], writes=[QT])
                steps.append(st_b)
            return steps

        for stp in head_build_steps(0):
            stp()
        jcount = [0]
        for h in range(8):
            KT, QT = kTh[h % 2], qTh[h % 2]
            nxt = head_build_steps(h + 1) if h + 1 < 8 else []
            jobs = [(g, kt) for g in range(4) for kt in range(20 + 4 * g)]

            def emit_qk(ji, jn):
                g, kt = jobs[ji]
                S = psS[jn % 3]
                op("pe", lambda e: e.matmul(S[:, :], lhsT=KT[:, kt * 128:(kt + 1) * 128], rhs=QT[:, g * 512:(g + 1) * 512], start=True, stop=True),
                   reads=[KT, QT], writes=[S])

            def emit_rest(ji, jn):
                g, kt = jobs[ji]
                nkt = 20 + 4 * g
                S = psS[jn % 3]
                P = PT[jn % 4]
                op("act", lambda e: e.activation(out=P[:], in_=S[:, :], func=AF.Exp, scale=0.125), reads=[S], writes=[P])
                jd = kt - (16 + 4 * g)
                if jd >= 0:
                    op("pool", lambda e: e.tensor_tensor(out=P[:], in0=P[:], in1=causal4[:, jd, :], op=ALU.mult), reads=[P, causal4], writes=[P])
                for qs in range(4):
                    if jd > qs:
                        continue
                    last = (kt == nkt - 1) or (kt == 16 + 4 * g + qs)
                    O = psO[qs]
                    op("pe", lambda e, O=O, qs=qs, last=last: e.matmul(O[:, 0:65], lhsT=P[:, qs * 128:(qs + 1) * 128], rhs=vaug[:, kt, h, :], start=(kt == 0), stop=last),
                       reads=[P, vaug], writes=[O])
                if kt == nkt - 1:
                    for qs in range(4):
                        O = psO[qs]
                        op("dve", lambda e, O=O, qs=qs: e.reciprocal(out=rec[:, qs:qs + 1], in_=O[:, 64:65]), reads=[O], writes=[rec])
                        op("dve", lambda e, O=O, qs=qs: e.tensor_scalar(out=mix[:, 4 * g + qs, h * 64:(h + 1) * 64], in0=O[:, 0:64], scalar1=rec[:, qs:qs + 1], scalar2=None, op0=ALU.mult),
                           reads=[O, rec], writes=[mix])

            emit_qk(0, jcount[0])
            emit_qk(1, jcount[0] + 1)
            for ji in range(len(jobs)):
                if ji + 2 < len(jobs):
                    emit_qk(ji + 2, jcount[0] + 2)
                emit_rest(ji, jcount[0])
                jcount[0] += 1
                if nxt and ji % 8 == 4:
                    nxt.pop(0)()
            while nxt:
                nxt.pop(0)()
        AR.release(*p3_tiles)
        AR.release(q_rot, k_rot, vaug, bias_all)
        if stop_after <= 3:
            if dbg:
                dbg_dump("mix", mix[:].rearrange("p a b -> p (a b)"), [128, 17 * 1024], BF16, mix)
            fw.barrier()
            return nc

        cw = A("cw", [128, 4, 31], F32)
        cb = A("cb", [128, 4], F32)
        lngb = A("lngb", [128, 512], F32)
        lnbb = A("lnbb", [128, 512], F32)
        dg = A("dg", [128, 4, 31, 128], BF16)
        cT = [A(f"cT{i}", [128, 4, 512], F32) for i in range(2)]
        st6 = A("st6", [128, 6], F32)
        mv = A("mv", [128, 2], F32)
        yn = [A(f"yn{i}", [128, 512], F32) for i in range(2)]
        hsT = A("hsT", [128, 4, 32, 31], F32)
        stc = [A(f"stc{i}", [128, 512], F32) for i in range(2)]
        prod = A("prod", [128, 4, 32, 31], F32)
        p4_tiles = [cw, cb, lngb, lnbb, dg] + cT + [st6, mv] + yn + [hsT, prod] + stc
        dma("sp", lambda e: e.dma_start(out=cw[:].rearrange("p a b -> p (a b)"), in_=convw_d), writes=[cw], track=cw)
        dma("sp", lambda e: e.dma_start(out=cb[:], in_=convb_d), writes=[cb], track=cb)
        dma("sp", lambda e: e.dma_start(out=lngb[:], in_=lng_d.partition_broadcast(128)), writes=[lngb], track=lngb)
        dma("sp", lambda e: e.dma_start(out=lnbb[:], in_=lnb_d.partition_broadcast(128)), writes=[lnbb], track=lnbb)
        for c in range(4):
            for tau in range(31):
                op("pool", lambda e, c=c, tau=tau: e.tensor_scalar(out=dg[:, c, tau, :], in0=identf[:], scalar1=cw[:, c, tau:tau + 1], scalar2=None, op0=ALU.mult),
                   reads=[identf, cw], writes=[dg])

        def ln_silu_tile(CT, col0, ti, bank):
            for c in range(4):
                op("pe", lambda e, c=c: e.transpose(out=bank[:, c * 128:(c + 1) * 128], in_=CT[:, c, col0:col0 + 128], identity=identf[:]),
                   reads=[CT, identf], writes=[bank])
            op("dve", lambda e: e.bn_stats(out=st6[:], in_=bank[:, :]), reads=[bank], writes=[st6])
            op("dve", lambda e: e.bn_aggr(out=mv[:], in_=st6[:]), reads=[st6], writes=[mv])
            op("dve", lambda e: e.tensor_scalar(out=mv[:, 1:2], in0=mv[:, 1:2], scalar1=1e-5, scalar2=None, op0=ALU.add), reads=[mv], writes=[mv])
            op("act", lambda e: e.activation(out=mv[:, 1:2], in_=mv[:, 1:2], func=AF.Sqrt), reads=[mv], writes=[mv])
            op("dve", lambda e: e.reciprocal(out=mv[:, 1:2], in_=mv[:, 1:2]), reads=[mv], writes=[mv])
            Y = yn[ti % 2]
            op("dve", lambda e: e.tensor_scalar(out=Y[:], in0=bank[:, :], scalar1=mv[:, 0:1], scalar2=mv[:, 1:2], op0=ALU.subtract, op1=ALU.mult),
               reads=[bank, mv], writes=[Y])
            op("pool", lambda e: e.tensor_tensor(out=Y[:], in0=Y[:], in1=lngb[:], op=ALU.mult), reads=[Y, lngb], writes=[Y])
            op("pool", lambda e: e.tensor_tensor(out=Y[:], in0=Y[:], in1=lnbb[:], op=ALU.add), reads=[Y, lnbb], writes=[Y])
            op("act", lambda e: e.activation(out=mix[:, ti, 512:1024], in_=Y[:], func=AF.Silu), reads=[Y], writes=[mix])

        for tg in range(4):
            CT = cT[tg % 2]
            for c in range(4):
                pc = banks[c % 2]
                for tau in range(31):
                    op("pe", lambda e, c=c, tau=tau, tg=tg, pc=pc: e.matmul(pc[:, :], lhsT=dg[:, c, tau, :], rhs=uT[:, c, tg * 512 + tau: tg * 512 + tau + 512], start=(tau == 0), stop=(tau == 30)),
                       reads=[dg, uT], writes=[pc])
                op("act", lambda e, c=c, pc=pc, CT=CT: e.activation(out=CT[:, c, :], in_=pc[:, :], func=AF.Identity, bias=cb[:, c:c + 1]), reads=[pc, cb], writes=[CT])
            for tt in range(4):
                ln_silu_tile(CT, tt * 128, tg * 4 + tt, banks[2 + (tt % 2)])
        for r in range(8):
            nrow = 128 if r < 7 else 64
            S_ = stc[r % 2]
            dma("sp", lambda e, S_=S_, r=r, nrow=nrow: e.dma_start(out=S_[0:nrow, :], in_=stc_d[r * 128:r * 128 + nrow, :]), writes=[S_], track=S_)
            bk = banks[4 + (r % 2)]
            for c in range(4):
                op("pe", lambda e, c=c, S_=S_, nrow=nrow, bk=bk: e.transpose(out=bk[:, c * 128:c * 128 + nrow], in_=S_[0:nrow, c * 128:(c + 1) * 128], identity=identf[0:nrow, 0:nrow]),
                   reads=[S_, identf], writes=[bk])
            bkv = bk[:, :].rearrange("p (c n) -> p c n", c=4)
            f0 = r * 128
            f1 = f0 + nrow
            b0 = f0 // 30
            b1 = (f1 - 1) // 30
            for b in range(b0, b1 + 1):
                lo = max(f0, b * 30)
                hi = min(f1, b * 30 + 30)
                op("dve", lambda e, b=b, lo=lo, hi=hi, bkv=bkv, f0=f0: e.tensor_copy(out=hsT[:, :, b, lo - b * 30:hi - b * 30], in_=bkv[:, :, lo - f0:hi - f0]),
                   reads=[bk], writes=[hsT])
        bk = banks[6]
        for c in range(4):
            op("pe", lambda e, c=c: e.transpose(out=bk[:, c * 128:c * 128 + 32], in_=us_f[0:32, c * 128:(c + 1) * 128], identity=identf[0:32, 0:32]),
               reads=[us_f, identf], writes=[bk])
        op("dve", lambda e: e.tensor_copy(out=hsT[:, :, :, 30], in_=bk[:, :].rearrange("p (c n) -> p c n", c=4)[:, :, 0:32]), reads=[bk], writes=[hsT])
        op("pool", lambda e: e.tensor_tensor(out=prod[:], in0=hsT[:], in1=cw[:].unsqueeze(2).broadcast_to([128, 4, 32, 31]), op=ALU.mult), reads=[hsT, cw], writes=[prod])
        CS = cT[0]
        op("pool", lambda e: e.memset(CS[:, :, 0:128], 0.0), writes=[CS])
        op("dve", lambda e: e.tensor_reduce(out=CS[:, :, 0:32], in_=prod[:], axis=AX.X, op=ALU.add), reads=[prod], writes=[CS])
        op("dve", lambda e: e.tensor_tensor(out=CS[:, :, 0:32], in0=CS[:, :, 0:32], in1=cb[:].unsqueeze(2).broadcast_to([128, 4, 32]), op=ALU.add), reads=[CS, cb], writes=[CS])
        ln_silu_tile(CS, 0, 16, banks[7])
        dma("sp", lambda e: e.dma_start(out=o_convs[:, 0:29, :], in_=stc_d.rearrange("(b t) c -> b t c", t=30)[:, 1:30, :]), writes=[out_b], track=out_b)
        dma("sp", lambda e: e.dma_start(out=o_convs[:, 29, :], in_=us_f[0:32, :]), reads=[us_f], writes=[out_b], track=out_b)
        AR.release(*p4_tiles)
        AR.release(uT)
        if stop_after <= 4:
            if dbg:
                dbg_dump("mix", mix[:].rearrange("p a b -> p (a b)"), [128, 17 * 1024], BF16, mix)
            fw.barrier()
            return nc

        acc = A("acc", [128, 17, D], F32)
        x2T = A("x2T", [128, 8, 17 * 128], BF16)
        comb = A("comb", [128, 17, 16], F32)
        wo = A("wo", [128, 8, D], BF16)
        wrb = A("wrb", [128, 8, 20], BF16)
        wrs = A("wrs", [128, 8, 20], F32)
        brb = A("brb", [128, 20], F32)
        g2b = A("g2b", [128, D], F32)
        stg = [A(f"stgG{i}", [128, 1024], F32) for i in range(2)]
        mixT = [A(f"mixT{i}", [128, 8, 128], BF16) for i in range(2)]
        xr = [A(f"xr{i}", [128, D], F32) for i in range(2)]
        x2 = [A(f"x2_{i}", [128, D], BF16) for i in range(2)]
        sqg = A("sqg", [128, D], BF16)
        ssg = [A(f"ssg{i}", [128, 1], F32) for i in range(2)]
        lg = A("lg", [128, 24], F32)
        gm8 = A("gm8", [128, 8], F32)
        ohg = A("ohg", [128, 4], F32)
        eg = A("eg", [128, 4], F32)
        sume = A("sume", [128, 2], F32)
        tmp44 = A("tmp44", [128, 4, 4], F32)
        ein = A("ein", [128, 8], F32)
        em8 = A("em8", [128, 8], F32)
        sel2 = A("sel2", [128, 4], F32)
        ee = A("ee", [128, 4], F32)
        wts = A("wts", [128, 4], F32)
        pG_tiles = [wo, wrb, wrs, brb, g2b] + stg + mixT + xr + x2 + [sqg] + ssg + [lg, gm8, ohg, eg, sume, tmp44, ein, em8, sel2, ee, wts]
        dma("sp", lambda e: e.dma_start(out=g2b[:], in_=g2.partition_broadcast(128)), writes=[g2b], track=g2b)
        dma("sp", lambda e: e.dma_start(out=brb[:], in_=br_d.partition_broadcast(128)), writes=[brb], track=brb)
        dma("sp", lambda e: e.dma_start(out=wrs[:], in_=wr_d.rearrange("(k p) n -> p k n", p=128)), writes=[wrs], track=wrs)
        op("pool", lambda e: e.tensor_copy(out=wrb[:], in_=wrs[:]), reads=[wrs], writes=[wrb])
        for kc in range(8):
            sg = stg[kc % 2]
            dma("sp", lambda e, sg=sg, kc=kc: e.dma_start(out=sg[:], in_=w_out[kc * 128:(kc + 1) * 128, :]), writes=[sg], track=sg)
            if kc % 2 == 0:
                op("pool", lambda e, sg=sg, kc=kc: e.tensor_copy(out=wo[:, kc, :], in_=sg[:]), reads=[sg], writes=[wo])
            else:
                op("act", lambda e, sg=sg, kc=kc: e.activation(out=wo[:, kc, :], in_=sg[:], func=AF.Copy), reads=[sg], writes=[wo])
        op("pool", lambda e: e.memset(ein[:, 4:8], -1.0e30), writes=[ein])
        def stageGA(ti):
            i = ti % 2
            MT, XR, X2, SSG = mixT[i], xr[i], x2[i], ssg[i]
            pt_ = banks[0]
            ptv = bbf16(pt_).rearrange("p (c n) -> p c n", c=8)
            dma("sp", lambda e, XR=XR, ti=ti: e.dma_start(out=XR[:], in_=xall[(16 + ti) * 128:(17 + ti) * 128, :]), writes=[XR], track=XR)
            for c in range(8):
                op("pe", lambda e, c=c, ti=ti: e.transpose(out=ptv[:, c, :], in_=mix[:, ti, c * 128:(c + 1) * 128], identity=identb[:]), reads=[mix, identb], writes=[pt_])
            op("act", lambda e, MT=MT: e.activation(out=MT[:], in_=ptv, func=AF.Copy), reads=[pt_], writes=[MT])
            for half in range(2):
                po = banks[2 + half]
                for kc in range(8):
                    op("pe", lambda e, kc=kc, half=half, MT=MT, po=po: e.matmul(po[:, :], lhsT=MT[:, kc, :], rhs=wo[:, kc, half * 512:(half + 1) * 512], start=(kc == 0), stop=(kc == 7)),
                       reads=[MT, wo], writes=[po])
                op("dve", lambda e, half=half, po=po, XR=XR, ti=ti: e.tensor_tensor(out=acc[:, ti, half * 512:(half + 1) * 512], in0=po[:, :], in1=XR[:, half * 512:(half + 1) * 512], op=ALU.add),
                   reads=[po, XR], writes=[acc])
        def stageGB(ti):
            i = ti % 2
            MT, XR, X2, SSG = mixT[i], xr[i], x2[i], ssg[i]
            op("act", lambda e, SSG=SSG, ti=ti: e.activation(out=sqg[:], in_=acc[:, ti, :], func=AF.Square, accum_out=SSG[:]), reads=[acc], writes=[sqg, SSG])
            rstd_from_ss(SSG, 1e-6, 1.0 / D)
            op("dve", lambda e, SSG=SSG, X2=X2, ti=ti: e.scalar_tensor_tensor(out=X2[:], in0=acc[:, ti, :], scalar=SSG[:], in1=g2b[:], op0=ALU.mult, op1=ALU.mult),
               reads=[acc, SSG, g2b], writes=[X2])
            pt2 = banks[1]
            pt2v = bbf16(pt2).rearrange("p (c n) -> p c n", c=8)
            for c in range(8):
                op("pe", lambda e, c=c, X2=X2: e.transpose(out=pt2v[:, c, :], in_=X2[:, c * 128:(c + 1) * 128], identity=identb[:]), reads=[X2, identb], writes=[pt2])
            op("act", lambda e, ti=ti: e.activation(out=x2T[:, :, ti * 128:(ti + 1) * 128], in_=pt2v, func=AF.Copy), reads=[pt2], writes=[x2T])
            pl = banks[4 + i]
            for kc in range(8):
                op("pe", lambda e, kc=kc, ti=ti, pl=pl: e.matmul(pl[:, 0:20], lhsT=x2T[:, kc, ti * 128:(ti + 1) * 128], rhs=wrb[:, kc, :], start=(kc == 0), stop=(kc == 7)),
                   reads=[x2T, wrb], writes=[pl])
            op("dve", lambda e, pl=pl: e.tensor_tensor(out=lg[:, 0:4], in0=pl[:, 0:4], in1=brb[:, 0:4], op=ALU.add), reads=[pl, brb], writes=[lg])
            op("dve", lambda e, pl=pl: e.tensor_tensor(out=lg[:, 8:24], in0=pl[:, 4:20], in1=brb[:, 4:20], op=ALU.add), reads=[pl, brb], writes=[lg])
            op("pool", lambda e: e.memset(lg[:, 4:8], -1.0e30), writes=[lg])
            op("dve", lambda e: e.max(out=gm8[:], in_=lg[:, 0:8]), reads=[lg], writes=[gm8])
            op("dve", lambda e: e.tensor_scalar(out=ohg[:], in0=lg[:, 0:4], scalar1=gm8[:, 0:1], scalar2=None, op0=ALU.is_ge), reads=[lg, gm8], writes=[ohg])
            op("dve", lambda e: e.tensor_scalar(out=gm8[:, 1:2], in0=gm8[:, 0:1], scalar1=-1.0, scalar2=None, op0=ALU.mult), reads=[gm8], writes=[gm8])
            op("act", lambda e: e.activation(out=eg[:], in_=lg[:, 0:4], func=AF.Exp, bias=gm8[:, 1:2], accum_out=sume[:, 0:1]), reads=[lg, gm8], writes=[eg, sume])
            op("dve", lambda e: e.tensor_tensor(out=tmp44[:], in0=lg[:, 8:24].rearrange("p (g x) -> p g x", g=4), in1=ohg[:].unsqueeze(2).broadcast_to([128, 4, 4]), op=ALU.mult),
               reads=[lg, ohg], writes=[tmp44])
            op("dve", lambda e: e.tensor_reduce(out=ein[:, 0:4], in_=tmp44[:].rearrange("p g x -> p x g"), axis=AX.X, op=ALU.add), reads=[tmp44], writes=[ein])
            op("dve", lambda e: e.max(out=em8[:], in_=ein[:]), reads=[ein], writes=[em8])
            op("dve", lambda e: e.tensor_scalar(out=sel2[:], in0=ein[:, 0:4], scalar1=em8[:, 1:2], scalar2=None, op0=ALU.is_ge), reads=[ein, em8], writes=[sel2])
            op("dve", lambda e: e.tensor_scalar(out=em8[:, 2:3], in0=em8[:, 0:1], scalar1=-1.0, scalar2=None, op0=ALU.mult), reads=[em8], writes=[em8])
            op("act", lambda e: e.activation(out=ee[:], in_=ein[:, 0:4], func=AF.Exp, bias=em8[:, 2:3]), reads=[ein, em8], writes=[ee])
            op("dve", lambda e: e.tensor_tensor(out=ee[:], in0=ee[:], in1=sel2[:], op=ALU.mult), reads=[ee, sel2], writes=[ee])
            op("dve", lambda e: e.reduce_sum(out=sume[:, 1:2], in_=ee[:], axis=AX.X), reads=[ee], writes=[sume])
            op("dve", lambda e: e.tensor_tensor(out=sume[:, 0:1], in0=sume[:, 0:1], in1=sume[:, 1:2], op=ALU.mult), reads=[sume], writes=[sume])
            op("dve", lambda e: e.reciprocal(out=sume[:, 0:1], in_=sume[:, 0:1]), reads=[sume], writes=[sume])
            op("dve", lambda e: e.tensor_scalar(out=wts[:], in0=ee[:], scalar1=sume[:, 0:1], scalar2=None, op0=ALU.mult), reads=[ee, sume], writes=[wts])
            op("dve", lambda e, ti=ti: e.tensor_tensor(out=comb[:, ti, :].rearrange("p (g x) -> p g x", g=4), in0=ohg[:].unsqueeze(2).broadcast_to([128, 4, 4]),
                                                      in1=wts[:].unsqueeze(1).broadcast_to([128, 4, 4]), op=ALU.mult),
               reads=[ohg, wts], writes=[comb])
        stageGA(0)
        for ti in range(17):
            if ti + 1 < 17:
                stageGA(ti + 1)
            stageGB(ti)
        AR.release(*pG_tiles)
        AR.release(mix)
        if stop_after <= 5:
            if dbg:
                dbg_dump("acc", acc[:].rearrange("p a b -> p (a b)"), [128, 17 * 1024], F32, acc)
                dbg_dump("comb", comb[:].rearrange("p a b -> p (a b)"), [128, 17 * 16], F32, comb)
            fw.barrier()
            return nc

        wgb = [A(f"wgb{i}", [128, 8, 512], BF16) for i in range(2)]
        wub = [A(f"wub{i}", [128, 8, 512], BF16) for i in range(2)]
        wdb = [A(f"wdb{i}", [128, 4, D], BF16) for i in range(2)]
        stI = [A(f"stI{i}", [128, 2048], F32) for i in range(3)]
        sgt = [A(f"sgt{i}", [128, 512], F32) for i in range(2)]
        hT = [A(f"hT{i}", [128, 4, 512], BF16) for i in range(2)]
        pI_tiles = wgb + wub + wdb + stI + sgt + hT
        sti = [0]

        def load_expert(e_):
            i = e_ % 2
            srcs = []
            for hf in range(2):
                srcs.append((w_gate[e_, hf * 512:(hf + 1) * 512, :].rearrange("(k p) n -> p k n", p=128), wgb[i], lambda tl, hf=hf: tl[:, 4 * hf:4 * hf + 4, :], 4))
                srcs.append((w_up[e_, hf * 512:(hf + 1) * 512, :].rearrange("(k p) n -> p k n", p=128), wub[i], lambda tl, hf=hf: tl[:, 4 * hf:4 * hf + 4, :], 4))
            for hf in range(2):
                srcs.append((w_down[e_, hf * 256:(hf + 1) * 256, :].rearrange("(k p) n -> p k n", p=128), wdb[i], lambda tl, hf=hf: tl[:, 2 * hf:2 * hf + 2, :], 2))
            for (src, dst, view, k) in srcs:
                sg = stI[sti[0] % 3]
                sti[0] += 1
                dma("sp", lambda e, sg=sg, src=src, k=k: e.dma_start(out=sg[:].rearrange("p (k n) -> p k n", k=k), in_=src), writes=[sg], track=sg)
                op("pool", lambda e, sg=sg, dst=dst, view=view, k=k: e.tensor_copy(out=view(dst), in_=sg[:].rearrange("p (k n) -> p k n", k=k)), reads=[sg], writes=[dst])

        load_expert(0)
        gi = 0
        for e_ in range(16):
            if stop_after == 6 and e_ >= 2:
                break
            i = e_ % 2
            if e_ + 1 < 16:
                load_expert(e_ + 1)
            for tg in range(5):
                n = 512 if tg < 4 else 128
                tok0 = tg * 512
                H = hT[tg % 2]
                for fc in range(4):
                    pgt_ = banks[(gi % 2) * 2]
                    put_ = banks[(gi % 2) * 2 + 1]
                    gi += 1
                    for kc in range(8):
                        op("pe", lambda e, kc=kc, fc=fc, i=i, n=n, tok0=tok0, pgt_=pgt_: e.matmul(pgt_[:, 0:n], lhsT=wgb[i][:, kc, fc * 128:(fc + 1) * 128], rhs=x2T[:, kc, tok0:tok0 + n], start=(kc == 0), stop=(kc == 7)),
                           reads=[wgb[i], x2T], writes=[pgt_])
                    for kc in range(8):
                        op("pe", lambda e, kc=kc, fc=fc, i=i, n=n, tok0=tok0, put_=put_: e.matmul(put_[:, 0:n], lhsT=wub[i][:, kc, fc * 128:(fc + 1) * 128], rhs=x2T[:, kc, tok0:tok0 + n], start=(kc == 0), stop=(kc == 7)),
                           reads=[wub[i], x2T], writes=[put_])
                    SG = sgt[fc % 2]
                    op("act", lambda e, SG=SG, pgt_=pgt_, n=n: e.activation(out=SG[:, 0:n], in_=pgt_[:, 0:n], func=AF.Silu), reads=[pgt_], writes=[SG])
                    op("dve", lambda e, SG=SG, put_=put_, n=n, H=H, fc=fc: e.tensor_tensor(out=H[:, fc, 0:n], in0=put_[:, 0:n], in1=SG[:, 0:n], op=ALU.mult), reads=[put_, SG], writes=[H])
                for tt in range(n // 128):
                    ti = tg * 4 + tt
                    for half in range(2):
                        po = banks[4 + 2 * (ti % 2) + half]
                        for fc in range(4):
                            op("pe", lambda e, fc=fc, half=half, H=H, tt=tt, i=i, po=po: e.matmul(po[:, :], lhsT=H[:, fc, tt * 128:(tt + 1) * 128], rhs=wdb[i][:, fc, half * 512:(half + 1) * 512], start=(fc == 0), stop=(fc == 3)),
                               reads=[H, wdb[i]], writes=[po])
                        op("dve", lambda e, half=half, po=po, ti=ti, e_=e_: e.scalar_tensor_tensor(out=acc[:, ti, half * 512:(half + 1) * 512], in0=po[:, :], scalar=comb[:, ti, e_:e_ + 1],
                                                                                              in1=acc[:, ti, half * 512:(half + 1) * 512], op0=ALU.mult, op1=ALU.add),
                           reads=[po, comb, acc], writes=[acc])
        AR.release(*pI_tiles)
        AR.release(x2T)

        gfb = A("gfb", [128, D], F32)
        yt = [A(f"yt{i}", [128, D], F32) for i in range(2)]
        sqj = A("sqj", [128, D], BF16)
        ssj = [A(f"ssj{i}", [128, 1], F32) for i in range(2)]
        dma("sp", lambda e: e.dma_start(out=gfb[:], in_=gf.partition_broadcast(128)), writes=[gfb], track=gfb)
        for ti in range(17):
            i = ti % 2
            SSJ, Y = ssj[i], yt[i]
            op("act", lambda e, SSJ=SSJ, ti=ti: e.activation(out=sqj[:], in_=acc[:, ti, :], func=AF.Square, accum_out=SSJ[:]), reads=[acc], writes=[sqj, SSJ])
            rstd_from_ss(SSJ, 1e-6, 1.0 / D)
            op("dve", lambda e, SSJ=SSJ, Y=Y, ti=ti: e.scalar_tensor_tensor(out=Y[:], in0=acc[:, ti, :], scalar=SSJ[:], in1=gfb[:], op0=ALU.mult, op1=ALU.mult),
               reads=[acc, SSJ, gfb], writes=[Y])
            if ti < 16:
                dma("sp", lambda e, Y=Y, ti=ti: e.dma_start(out=o_y[ti * 128:(ti + 1) * 128, :], in_=Y[:]), reads=[Y], writes=[], track=Y)
            else:
                dma("sp", lambda e, Y=Y: e.dma_start(out=o_ys, in_=Y[:]), reads=[Y], writes=[], track=Y)
        fw.barrier()
    return nc


def build_sa():
    nc = bass.Bass("TRN2", target_bir_lowering=False)
    din = lambda n, s, d=F32: nc.dram_tensor(n, list(s), d, kind="ExternalInput").ap()
    xs = din("xs", [128, D])
    g1 = din("g1", [1, D])
    wq = din("wq", [D, 192])
    bq = din("bq", [1, 192])
    ropes = din("ropes", [128, 128])
    identf_d = din("identf", [128, 128])
    gsel_d = din("gsel", [96, 64])
    ck = din("ck", [5120, 8192])
    cv = din("cv", [5120, 8192])
    ptT_d = din("ptT", [128, 32], I32)
    pt_d = din("pt", [32, 128], I32)
    o_attn = nc.dram_tensor("o_attn", [32, 64], F32, kind="ExternalOutput").ap()
    scr_q = nc.dram_tensor("scr_q", [32, 64], F32, kind="Internal").ap()
    scr_p = nc.dram_tensor("scr_p", [32, 6], I32, kind="Internal").ap()

    with ExitStack() as st:
        fw = FW(nc, st)
        op, dma = fw.op, fw.dma
        arena_t = st.enter_context(nc.sbuf_tensor("arena", [128, ARENA_WORDS], F32))
        AR = Arena(fw, arena_t[:, :], ARENA_WORDS)
        A = AR.alloc
        banks = [Tl(st.enter_context(nc.psum_tensor(f"bank{i}", [128, 512], F32))[:, :], fw.buf(f"bank{i}")) for i in range(8)]
        out_b = fw.buf("outs")
        sq_b, sp_b = fw.buf("scrq"), fw.buf("scrp")

        identf = A("identf", [128, 128], F32)
        xt = A("xt", [128, D], F32)
        g1b = A("g1b", [128, D], F32)
        sqx = A("sqx", [128, D], F32)
        ssx = A("ssx", [128, 1], F32)
        xn = A("xn", [128, D], F32)
        xnT = A("xnT", [128, 8, 128], F32)
        wsb = A("wsb", [128, 8, 192], F32)
        bqb = A("bqb", [128, 192], F32)
        rp = A("rp", [128, 128], F32)
        z = A("z", [128, 192], F32)
        ta = A("ta", [128, 128], F32)
        tb = A("tb", [128, 128], F32)
        qk = A("qk", [128, 128], F32)
        ptT = A("ptT", [128, 32], I32)
        pti = A("pti", [32, 128], I32)
        ptf = A("ptf", [32, 128], F32)
        gsel = A("gsel", [96, 64], F32)
        for (t_, d_) in ((identf, identf_d), (xt, xs), (rp, ropes), (ptT, ptT_d), (pti, pt_d), (gsel, gsel_d)):
            dma("sp", lambda e, t_=t_, d_=d_: e.dma_start(out=t_[:], in_=d_), writes=[t_], track=t_)
        dma("sp", lambda e: e.dma_start(out=g1b[:], in_=g1.partition_broadcast(128)), writes=[g1b], track=g1b)
        dma("sp", lambda e: e.dma_start(out=bqb[:], in_=bq.partition_broadcast(128)), writes=[bqb], track=bqb)
        dma("sp", lambda e: e.dma_start(out=wsb[:], in_=wq.rearrange("(k p) n -> p k n", p=128)), writes=[wsb], track=wsb)
        op("dve", lambda e: e.tensor_copy(out=ptf[:], in_=pti[:]), reads=[pti], writes=[ptf])

        kbuf = [A(f"kbuf{i}", [128, 8192], F32) for i in range(2)]
        pagesum = A("pagesum", [128, 32, 64], F32)
        for b in range(32):
            KB = kbuf[b % 2]
            dma("pool", lambda e, KB=KB, b=b: e.indirect_dma_start(out=KB[:], out_offset=None, in_=ck, in_offset=bass.IndirectOffsetOnAxis(ap=ptT[:, b:b + 1], axis=0)),
                reads=[ptT], writes=[KB], track=KB)
            op("dve", lambda e, KB=KB, b=b: e.tensor_reduce(out=pagesum[:, b, :], in_=KB[:].rearrange("p (s d) -> p d s", d=64), axis=AX.X, op=ALU.add),
               reads=[KB], writes=[pagesum])

        op("act", lambda e: e.activation(out=sqx[:], in_=xt[:], func=AF.Square, accum_out=ssx[:]), reads=[xt], writes=[sqx, ssx])
        op("dve", lambda e: e.tensor_scalar(out=ssx[:], in0=ssx[:], scalar1=1.0 / D, scalar2=1e-6, op0=ALU.mult, op1=ALU.add), reads=[ssx], writes=[ssx])
        op("act", lambda e: e.activation(out=ssx[:], in_=ssx[:], func=AF.Sqrt), reads=[ssx], writes=[ssx])
        op("dve", lambda e: e.reciprocal(out=ssx[:], in_=ssx[:]), reads=[ssx], writes=[ssx])
        op("dve", lambda e: e.scalar_tensor_tensor(out=xn[:], in0=xt[:], scalar=ssx[:], in1=g1b[:], op0=ALU.mult, op1=ALU.mult), reads=[xt, ssx, g1b], writes=[xn])
        for c in range(8):
            bk = banks[c % 2]
            op("pe", lambda e, c=c, bk=bk: e.transpose(out=bk[:, 0:128], in_=xn[:, c * 128:(c + 1) * 128], identity=identf[:]), reads=[xn, identf], writes=[bk])
            op("dve", lambda e, c=c, bk=bk: e.tensor_copy(out=xnT[:, c, :], in_=bk[:, 0:128]), reads=[bk], writes=[xnT])
        pz = banks[2]
        for c in range(8):
            op("pe", lambda e, c=c: e.matmul(pz[:, 0:192], lhsT=xnT[:, c, :], rhs=wsb[:, c, :], start=(c == 0), stop=(c == 7)), reads=[xnT, wsb], writes=[pz])
        op("dve", lambda e: e.tensor_tensor(out=z[:], in0=pz[:, 0:192], in1=bqb[:], op=ALU.add), reads=[pz, bqb], writes=[z])
        zv = z[:, 0:128].rearrange("p (h d) -> p h d", h=2)
        tav = ta[:].rearrange("p (h d) -> p h d", h=2)
        tbv = tb[:].rearrange("p (h d) -> p h d", h=2)
        op("dve", lambda e: e.tensor_tensor(out=tav, in0=zv, in1=rp[:, 0:64].unsqueeze(1).broadcast_to([128, 2, 64]), op=ALU.mult), reads=[z, rp], writes=[ta])
        op("dve", lambda e: e.tensor_tensor(out=tbv[:, :, 0:32], in0=zv[:, :, 32:64], in1=rp[:, 64:96].unsqueeze(1).broadcast_to([128, 2, 32]), op=ALU.mult), reads=[z, rp], writes=[tb])
        op("dve", lambda e: e.tensor_tensor(out=tbv[:, :, 32:64], in0=zv[:, :, 0:32], in1=rp[:, 96:128].unsqueeze(1).broadcast_to([128, 2, 32]), op=ALU.mult), reads=[z, rp], writes=[tb])
        op("dve", lambda e: e.tensor_tensor(out=qk[:], in0=ta[:], in1=tb[:], op=ALU.add), reads=[ta, tb], writes=[qk])
        qbc = A("qbc", [128, 32, 64], F32)
        dma("sp", lambda e: e.dma_start(out=scr_q, in_=qk[0:32, 0:64]), reads=[qk], writes=[sq_b], track=sq_b)
        dma("sp", lambda e: e.dma_start(out=qbc[:].rearrange("p b d -> p (b d)"), in_=scr_q.rearrange("b d -> (b d)").unsqueeze(0).partition_broadcast(128)),
            reads=[sq_b], writes=[qbc], track=qbc)

        tmpg = A("tmpg", [128, 32, 64], F32)
        gp = A("gp", [128, 32], F32)
        gT = A("gT", [32, 128], F32)
        gate = A("gate", [32, 64], F32)
        m8 = A("m8", [32, 8], F32)
        eq = A("eq", [32, 3, 64], F32)
        tmp4 = A("tmp4", [32, 3, 2, 64], F32)
        psel = A("psel", [32, 6], F32)
        pseli = A("pseli", [32, 6], I32)
        op("dve", lambda e: e.tensor_tensor(out=tmpg[:], in0=pagesum[:], in1=qbc[:], op=ALU.mult), reads=[pagesum, qbc], writes=[tmpg])
        op("dve", lambda e: e.tensor_reduce(out=gp[:], in_=tmpg[:], axis=AX.X, op=ALU.add), reads=[tmpg], writes=[gp])
        pgT = banks[3]
        op("pe", lambda e: e.transpose(out=pgT[0:32, 0:128], in_=gp[:], identity=identf[:]), reads=[gp, identf], writes=[pgT])
        op("dve", lambda e: e.tensor_copy(out=gT[:], in_=pgT[0:32, 0:128]), reads=[pgT], writes=[gT])
        gTv = gT[:].rearrange("b (k t) -> b t k", t=2)
        op("dve", lambda e: e.tensor_tensor(out=gate[:], in0=gTv[:, 0, :], in1=gTv[:, 1, :], op=ALU.add), reads=[gT], writes=[gate])
        op("dve", lambda e: e.max(out=m8[:], in_=gate[:]), reads=[gate], writes=[m8])
        for j in range(3):
            op("dve", lambda e, j=j: e.tensor_scalar(out=eq[:, j, :], in0=gate[:], scalar1=m8[:, j:j + 1], scalar2=None, op0=ALU.is_equal), reads=[gate, m8], writes=[eq])
        ptv = ptf[:].rearrange("b (k t) -> b t k", t=2)
        op("dve", lambda e: e.tensor_tensor(out=tmp4[:], in0=eq[:].unsqueeze(2).broadcast_to([32, 3, 2, 64]), in1=ptv.unsqueeze(1).broadcast_to([32, 3, 2, 64]), op=ALU.mult),
           reads=[eq, ptf], writes=[tmp4])
        op("dve", lambda e: e.tensor_reduce(out=psel[:].rearrange("b (j t) -> b j t", j=3), in_=tmp4[:], axis=AX.X, op=ALU.add), reads=[tmp4], writes=[psel])
        op("dve", lambda e: e.tensor_copy(out=pseli[:], in_=psel[:]), reads=[psel], writes=[pseli])
        dma("sp", lambda e: e.dma_start(out=scr_p, in_=pseli[:]), reads=[pseli], writes=[sp_b], track=sp_b)

        AR.release(kbuf[0], kbuf[1], tmpg, qbc)
        ksel = A("ksel", [96, 8192], F32)
        vsel = A("vsel", [96, 8192], F32)
        prod = A("prod", [96, 8192], F32)
        idx = [A(f"idx{r}", [96, 1], I32) for r in range(2)]
        qrep = [A(f"qrep{r}", [96, 64], F32) for r in range(2)]
        S = A("S", [96, 128], F32)
        Pm = A("Pm", [96, 128], F32)
        pvr = A("pvr", [96, 65], F32)
        pacc = banks[4]
        for r in range(2):
            for s_ in range(6):
                dma("sp", lambda e, r=r, s_=s_: e.dma_start(out=idx[r][s_ * 16:(s_ + 1) * 16, :], in_=scr_p[16 * r:16 * r + 16, s_:s_ + 1], allow_slow_non_contiguous=True), reads=[sp_b], writes=[idx[r]], track=idx[r])
                dma("sp", lambda e, r=r, s_=s_: e.dma_start(out=qrep[r][s_ * 16:(s_ + 1) * 16, :], in_=scr_q[16 * r:16 * r + 16, :]), reads=[sq_b], writes=[qrep[r]], track=qrep[r])
            dma("pool", lambda e, r=r: e.indirect_dma_start(out=ksel[:], out_offset=None, in_=ck, in_offset=bass.IndirectOffsetOnAxis(ap=idx[r][:, 0:1], axis=0)),
                reads=[idx[r]], writes=[ksel], track=ksel)
            dma("pool", lambda e, r=r: e.indirect_dma_start(out=vsel[:], out_offset=None, in_=cv, in_offset=bass.IndirectOffsetOnAxis(ap=idx[r][:, 0:1], axis=0)),
                reads=[idx[r]], writes=[vsel], track=vsel)
            op("dve", lambda e, r=r: e.tensor_tensor(out=prod[:].rearrange("p (s d) -> p s d", d=64), in0=ksel[:].rearrange("p (s d) -> p s d", d=64),
                                                   in1=qrep[r][:].unsqueeze(1).broadcast_to([96, 128, 64]), op=ALU.mult), reads=[ksel, qrep[r]], writes=[prod])
            op("dve", lambda e: e.tensor_reduce(out=S[:], in_=prod[:].rearrange("p (s d) -> p s d", d=64), axis=AX.X, op=ALU.add), reads=[prod], writes=[S])
            op("act", lambda e: e.activation(out=Pm[:], in_=S[:], func=AF.Exp, scale=0.125, accum_out=pvr[:, 64:65]), reads=[S], writes=[Pm, pvr])
            op("dve", lambda e: e.tensor_tensor(out=prod[:].rearrange("p (s d) -> p s d", d=64), in0=vsel[:].rearrange("p (s d) -> p s d", d=64),
                                              in1=Pm[:].unsqueeze(2).broadcast_to([96, 128, 64]), op=ALU.mult), reads=[vsel, Pm], writes=[prod])
            op("dve", lambda e: e.tensor_reduce(out=pvr[:, 0:64], in_=prod[:].rearrange("p (s d) -> p d s", d=64), axis=AX.X, op=ALU.add), reads=[prod], writes=[pvr])
            op("pe", lambda e, r=r: e.matmul(pacc[0:32, 0:65], lhsT=gsel[:, 32 * r:32 * r + 32], rhs=pvr[:], start=(r == 0), stop=(r == 1)), reads=[gsel, pvr], writes=[pacc])
        ls = A("ls", [32, 1], F32)
        tq = A("tq", [32, 64], F32)
        num = A("num", [32, 65], F32)
        res = A("res", [32, 64], F32)
        op("dve", lambda e: e.tensor_tensor(out=tq[:], in0=qk[0:32, 0:64], in1=qk[0:32, 64:128], op=ALU.mult), reads=[qk], writes=[tq])
        op("dve", lambda e: e.reduce_sum(out=ls[:], in_=tq[:], axis=AX.X), reads=[tq], writes=[ls])
        op("act", lambda e: e.activation(out=ls[:], in_=ls[:], func=AF.Exp, scale=0.125), reads=[ls], writes=[ls])
        op("dve", lambda e: e.scalar_tensor_tensor(out=num[:, 0:64], in0=z[0:32, 128:192], scalar=ls[:], in1=pacc[0:32, 0:64], op0=ALU.mult, op1=ALU.add), reads=[z, ls, pacc], writes=[num])
        op("dve", lambda e: e.tensor_tensor(out=num[:, 64:65], in0=pacc[0:32, 64:65], in1=ls[:], op=ALU.add), reads=[pacc, ls], writes=[num])
        op("dve", lambda e: e.reciprocal(out=num[:, 64:65], in_=num[:, 64:65]), reads=[num], writes=[num])
        op("dve", lambda e: e.tensor_scalar(out=res[:], in0=num[:, 0:64], scalar1=num[:, 64:65], scalar2=None, op0=ALU.mult), reads=[num], writes=[res])
        dma("sp", lambda e: e.dma_start(out=o_attn, in_=res[:]), reads=[res], writes=[out_b], track=out_b)
        fw.barrier()
    return nc


def prep_sa(inp):
    f = lambda a: np.ascontiguousarray(np.asarray(a, dtype=np.float32))
    xs = f(inp["x_sample"])
    xsp = np.zeros((128, D), np.float32)
    xsp[:32] = xs[:, 0]
    w_in = f(inp["w_in"])[0]
    b_in = f(inp["b_in"])
    pt = np.ascontiguousarray(np.asarray(inp["page_table"], dtype=np.int32))
    ck = np.asarray(inp["cache_k"])[0]
    cv = np.asarray(inp["cache_v"])[0]
    gs = np.zeros((96, 64), np.float32)
    for r in range(2):
        for s_ in range(6):
            for b in range(16):
                gs[s_ * 16 + b, 32 * r + 16 * r + b] = 1.0
    maps = []
    for c in range(8):
        cols = np.concatenate([np.arange(64) + 64 * c, 512 + np.arange(64) + 64 * c, 1024 + np.arange(64) + 64 * c])
        maps.append(dict(
            xs=xsp, g1=f(inp["norm1_g"]), wq=np.ascontiguousarray(w_in[:, cols]), bq=np.ascontiguousarray(b_in[:, cols]),
            ropes=_rope_tables(np.full(128, 16384)), identf=np.eye(128, dtype=np.float32), gsel=gs,
            ck=np.ascontiguousarray(ck[:, :, c, :]).reshape(5120, 8192), cv=np.ascontiguousarray(cv[:, :, c, :]).reshape(5120, 8192),
            ptT=np.ascontiguousarray(pt.T), pt=pt))
    return maps


def _consts(s):
    c = {}
    c["identb"] = bf(np.eye(128))
    c["identf"] = np.eye(128, dtype=np.float32)
    es = np.zeros((128, 16, 16), np.float32)
    for j in range(16):
        es[:, j, j] = 1.0
    c["esel"] = bf(es.reshape(128, 256))
    sm = np.zeros((128, 8, 16), np.float32)
    oh = np.zeros((128, 8, 16), np.float32)
    for jb in range(8):
        sm[:, jb, 0:8] = 0.0 if s == 1 else NEG
        for sl in range(8):
            sm[:, jb, 8 + sl] = 0.0 if sl < jb else NEG
        oh[:, jb, 8 + jb] = 1.0
    c["selmask"] = sm.reshape(128, 128)
    c["ownhot"] = oh.reshape(128, 128)
    si = np.zeros((16, 4096), np.float32)
    for sl in range(16):
        si[sl, sl * 256:(sl + 1) * 256] = 1.0
    c["slotind"] = bf(si)
    key = np.arange(128)[:, None, None]
    jj = np.arange(4)[None, :, None]
    qq = np.arange(512)[None, None, :]
    c["causal4"] = bf((128 * jj + key <= qq).astype(np.float32).reshape(128, 2048))
    c["hflag"] = np.full((128, 1), float(s), np.float32)
    return c


def prep(inp, ncores=8):
    f = lambda a: np.ascontiguousarray(np.asarray(a, dtype=np.float32))
    xp = f(inp["x_prompt"])
    xs = f(inp["x_sample"])
    xsp = np.zeros((128, D), np.float32)
    xsp[:32] = xs[:, 0]
    cw = f(inp["conv_w"])[0]
    shared = dict(
        w_in=f(inp["w_in"])[0], b_in=f(inp["b_in"]), g1=f(inp["norm1_g"]), g2=f(inp["norm2_g"]),
        gf=f(inp["norm_f_g"]).reshape(1, D),
        convw=np.ascontiguousarray(cw.T.reshape(4, 128, 31).transpose(1, 0, 2)).reshape(128, 124),
        convb=np.ascontiguousarray(f(inp["conv_b"])[0].reshape(4, 128).T),
        lng=f(inp["conv_ln_g"]), lnb=f(inp["conv_ln_b"]),
        w_out=f(inp["w_out"])[0],
        wr=np.ascontiguousarray(np.concatenate([f(inp["w_group"])[0], f(inp["w_router"])[0]], axis=1)),
        br=np.ascontiguousarray(np.concatenate([f(inp["b_group"]), f(inp["b_router"])], axis=1)),
        w_gate=f(inp["w_gate"])[0], w_up=f(inp["w_up"])[0], w_down=f(inp["w_down"])[0],
        stc=f(inp["state_conv"])[0].reshape(32 * 30, 512),
    )
    maps = []
    for c in range(ncores):
        b, s = c // 2, c % 2
        xo = xp[b, 2048 * s:2048 * s + 2048]
        xh = xp[b, 2048 * (1 - s):2048 * (1 - s) + 2048]
        pos = np.concatenate([np.arange(2048) + 2048 * (1 - s), np.arange(2048) + 2048 * s, np.full(128, 16384)])
        m = dict(shared)
        m.update(_consts(s))
        m["xall"] = np.concatenate([xh, xo, xsp], axis=0)
        m["rope"] = _rope_tables(pos)
        maps.append(m)
    return maps


_NC_CACHE = {}


def kernel(**inputs):
    if "nca" not in _NC_CACHE:
        _NC_CACHE["nca"] = build_sa()
    resa = run_bass_kernel_spmd(_NC_CACHE["nca"], prep_sa(inputs), core_ids=list(range(8))).results
    attn_s = np.zeros((128, 512), np.float32)
    for c in range(8):
        attn_s[:32, 64 * c:64 * c + 64] = resa[c]["o_attn"]
    del resa
    maps = prep(inputs, 8)
    for m in maps:
        m["attn_s"] = attn_s
    if "nc" not in _NC_CACHE:
        _NC_CACHE["nc"] = build()
    nc = _NC_CACHE["nc"]
    res = run_bass_kernel_spmd(nc, maps, core_ids=list(range(8))).results
    y_prompt = np.zeros((4, 4096, D), np.float32)
    kp = np.zeros((1, 4, 4096, 8, 64), np.float32)
    vp = np.zeros((1, 4, 4096, 8, 64), np.float32)
    cp = np.zeros((1, 4, 30, 512), np.float32)
    for c in range(8):
        b, s = c // 2, c % 2
        sl = slice(2048 * s, 2048 * s + 2048)
        y_prompt[b, sl] = res[c]["o_y"]
        kp[0, b, sl] = res[c]["o_k"].reshape(2048, 8, 64)
        vp[0, b, sl] = res[c]["o_v"].reshape(2048, 8, 64)
        if s == 1:
            cp[0, b] = res[c]["o_convp"]
    y_sample = np.ascontiguousarray(res[0]["o_ys"][:32]).reshape(32, 1, D)
    ks = np.ascontiguousarray(res[0]["o_ks"][:32]).reshape(1, 32, 1, 8, 64)
    vs = np.ascontiguousarray(res[0]["o_vs"][:32]).reshape(1, 32, 1, 8, 64)
    cs = np.ascontiguousarray(res[0]["o_convs"]).reshape(1, 32, 30, 512)
    return (y_prompt, y_sample, kp, vp, cp, ks, vs, cs)
```
